# Optimizing a Trainium2 kernel written in Bass

```python
import jax, jax.numpy as jnp
from jax import lax
import numpy as np

D_MODEL = 4096
BATCH = 2
SEQ = 8192
DEPTH = 2

CTX_LEN = 256
GRID_W = 64
MIX_WIDTH = D_MODEL
HEAD_DIM = 128
NA_HEADS = (MIX_WIDTH // 2) // HEAD_DIM
NA_WIDTH = NA_HEADS * HEAD_DIM
CONV_WIDTH = MIX_WIDTH // 4
SG_WIDTH = MIX_WIDTH - NA_WIDTH - CONV_WIDTH
SG_GROUPS = 8
SG_GROUP_DIM = SG_WIDTH // SG_GROUPS
CHUNK = 128
NA_KH_MAX = 8
NA_KW = 16
CONV_K = 3
N_EXPERTS = 16
EXPERT_FF = D_MODEL // 4
CAPACITY_FACTOR = 2
N_MOD = 6
EPS = 1e-6
IN_COLS = 3 * NA_WIDTH + 3 * CONV_WIDTH + 2 * SG_WIDTH
SPLITS = (NA_WIDTH, 2 * NA_WIDTH, 3 * NA_WIDTH,
          3 * NA_WIDTH + CONV_WIDTH, 3 * NA_WIDTH + 2 * CONV_WIDTH, 3 * NA_WIDTH + 3 * CONV_WIDTH,
          3 * NA_WIDTH + 3 * CONV_WIDTH + SG_WIDTH)

kernel_name = 'hybrid_natten_conv_gmlp_ec_moe_dit'


def rms_norm(x, g):
    xf = x.astype(jnp.float32)
    y = xf * lax.rsqrt(jnp.mean(xf * xf, axis=-1, keepdims=True) + EPS)
    return (y * g.astype(jnp.float32)).astype(x.dtype)


def adaln(cvec, w, b):
    return jax.nn.silu(cvec) @ w + b


def modulate(h, shift, scale):
    return h * (1 + scale) + shift


def to_heads(t, gain=None):
    t = t.reshape(*t.shape[:-1], NA_HEADS, HEAD_DIM)
    return t if gain is None else rms_norm(t, gain)


def neighbourhood_attention(q, k, v, k_ctx, v_ctx, rpb):
    B, N, H, dh = q.shape
    rows = N // GRID_W
    kh = min(NA_KH_MAX, rows)
    scale = dh ** -0.5
    qg = q.reshape(B, rows, GRID_W, H, dh)
    kg = k.reshape(B, rows, GRID_W, H, dh)
    vg = v.reshape(B, rows, GRID_W, H, dh)
    cols = jnp.arange(GRID_W)
    col_start = jnp.clip(cols - NA_KW // 2, 0, GRID_W - NA_KW)
    col_valid = (cols[None, :] >= col_start[:, None]) & (cols[None, :] < col_start[:, None] + NA_KW)
    mask = jnp.broadcast_to(col_valid[:, None, :], (GRID_W, kh, GRID_W)).reshape(GRID_W, kh * GRID_W)
    dc_idx = jnp.clip(cols[None, :] - cols[:, None] + NA_KW - 1, 0, 2 * NA_KW - 2)
    rpb32 = rpb.astype(jnp.float32)

    def row_block(r):
        start = jnp.clip(r - kh // 2, 0, rows - kh)
        q_r = lax.dynamic_index_in_dim(qg, r, axis=1, keepdims=False)
        k_r = lax.dynamic_slice_in_dim(kg, start, kh, axis=1).reshape(B, kh * GRID_W, H, dh)
        v_r = lax.dynamic_slice_in_dim(vg, start, kh, axis=1).reshape(B, kh * GRID_W, H, dh)
        dr_idx = start + jnp.arange(kh) - r + NA_KH_MAX - 1
        bias = rpb32[:, dr_idx[:, None, None], dc_idx[None, :, :]]
        bias = bias.transpose(0, 2, 1, 3).reshape(H, GRID_W, kh * GRID_W)
        s_loc = jnp.einsum('bqhd,bkhd->bhqk', q_r, k_r).astype(jnp.float32) * scale + bias[None]
        s_loc = jnp.where(mask, s_loc, -jnp.inf)
        s_ctx = jnp.einsum('bqhd,bkhd->bhqk', q_r, k_ctx).astype(jnp.float32) * scale
        p = jax.nn.softmax(jnp.concatenate([s_loc, s_ctx], axis=-1), axis=-1).astype(v.dtype)
        n_loc = kh * GRID_W
        return (jnp.einsum('bhqk,bkhd->bqhd', p[..., :n_loc], v_r)
                + jnp.einsum('bhqk,bkhd->bqhd', p[..., n_loc:], v_ctx))

    out = lax.map(row_block, jnp.arange(rows))
    return out.transpose(1, 0, 2, 3, 4).reshape(B, N, H * dh)


def context_attention(q, k, v):
    B, L, H, dh = q.shape
    s = jnp.einsum('bqhd,bkhd->bhqk', q, k).astype(jnp.float32) * (dh ** -0.5)
    p = jax.nn.softmax(s, axis=-1).astype(v.dtype)
    return jnp.einsum('bhqk,bkhd->bqhd', p, v).reshape(B, L, H * dh)


def gated_short_conv(xin, gate_b, gate_c, conv_w):
    ch = xin.shape[-1]
    z = gate_c * xin
    z = lax.conv_general_dilated(z, conv_w[:, None, :].astype(z.dtype), window_strides=(1,),
                                 padding=((CONV_K // 2, CONV_K // 2),),
                                 dimension_numbers=('NWC', 'WIO', 'NWC'), feature_group_count=ch)
    return gate_b * z


def spatial_gating(u, v, sg_norm, sg_w, sg_b):
    B, L, _ = u.shape
    vn = rms_norm(v, sg_norm).reshape(B, L // CHUNK, CHUNK, SG_GROUPS, SG_GROUP_DIM)
    mixed = jnp.einsum('gts,bnsgd->bntgd', sg_w, vn) + sg_b.T[None, None, :, :, None]
    return u * mixed.reshape(B, L, SG_WIDTH)


def expert_choice_ffn(h, w_router, w_gate, w_up, w_down):
    B, N, D = h.shape
    cap = CAPACITY_FACTOR * N // N_EXPERTS
    aff = jax.nn.softmax((h @ w_router).astype(jnp.float32), axis=-1)
    gate, idx = lax.top_k(aff.transpose(0, 2, 1), cap)
    bidx = jnp.arange(B)[:, None, None]
    hs = h[bidx, idx]
    a = jax.nn.silu(jnp.einsum('becd,edf->becf', hs, w_gate)) * jnp.einsum('becd,edf->becf', hs, w_up)
    y = jnp.einsum('becf,efd->becd', a, w_down) * gate[..., None].astype(h.dtype)
    return jnp.zeros_like(h).at[bidx, idx].add(y)


def mixer_concat(attn_out, xin, gb, gc, su, sv, conv_w, sg_norm, sg_w, sg_b):
    return jnp.concatenate([attn_out, gated_short_conv(xin, gb, gc, conv_w),
                            spatial_gating(su, sv, sg_norm, sg_w, sg_b)], axis=-1)


def hybrid_layer(x, xc, c, c_ctx, w_ada, b_ada, norm1, norm2, w_in, q_norm, k_norm, rpb, conv_w,
                 sg_norm, sg_w, sg_b, w_out, w_router, w_gate, w_up, w_down, update_ctx):
    D = x.shape[-1]
    sh1, sc1, g1, sh2, sc2, g2 = jnp.split(adaln(c, w_ada, b_ada)[:, None, :], N_MOD, axis=-1)

    if update_ctx:
        csh1, csc1, cg1, csh2, csc2, cg2 = jnp.split(adaln(c_ctx, w_ada, b_ada), N_MOD, axis=-1)
        hc = modulate(rms_norm(xc, norm1), csh1, csc1)
        qc_, kc_, vc_, xin_c, gb_c, gc_c, su_c, sv_c = jnp.split(hc @ w_in, SPLITS, axis=-1)
    else:
        csh1, csc1 = jnp.split(adaln(c_ctx, w_ada[:, :2 * D], b_ada[:2 * D]), 2, axis=-1)
        hc = modulate(rms_norm(xc, norm1), csh1, csc1)
        kc_, vc_ = jnp.split(hc @ w_in[:, NA_WIDTH:3 * NA_WIDTH], 2, axis=-1)
    k_ctx = to_heads(kc_, k_norm)
    v_ctx = to_heads(vc_)

    h = modulate(rms_norm(x, norm1), sh1, sc1)
    q_, k_, v_, xin, gb, gc, su, sv = jnp.split(h @ w_in, SPLITS, axis=-1)
    a = neighbourhood_attention(to_heads(q_, q_norm), to_heads(k_, k_norm), to_heads(v_), k_ctx, v_ctx, rpb)
    m = mixer_concat(a, xin, gb, gc, su, sv, conv_w, sg_norm, sg_w, sg_b)
    x = x + g1 * (m @ w_out)
    h2 = modulate(rms_norm(x, norm2), sh2, sc2)
    x = x + g2 * expert_choice_ffn(h2, w_router, w_gate, w_up, w_down)

    if update_ctx:
        a_c = context_attention(to_heads(qc_, q_norm), k_ctx, v_ctx)
        m_c = mixer_concat(a_c, xin_c, gb_c, gc_c, su_c, sv_c, conv_w, sg_norm, sg_w, sg_b)
        xc = xc + cg1 * (m_c @ w_out)
        hc2 = modulate(rms_norm(xc, norm2), csh2, csc2)
        xc = xc + cg2 * expert_choice_ffn(hc2, w_router, w_gate, w_up, w_down)
    return x, xc


def setup_inputs(seed: int = 0) -> dict:
    key = jax.random.key(seed)
    ks = jax.random.split(key, 24)
    D = D_MODEL
    nrm = lambda k, shape, s: jax.random.normal(k, shape, jnp.float32) * s
    return {
        'x': nrm(ks[0], (BATCH, SEQ, D), 1.0),
        'c': nrm(ks[1], (BATCH, D), 1.0),
        'ctx': nrm(ks[2], (BATCH, CTX_LEN, D), 1.0),
        'c_ctx': nrm(ks[3], (D,), 1.0),
        'w_ada': nrm(ks[4], (DEPTH, D, N_MOD * D), 0.5 * D ** -0.5),
        'b_ada': nrm(ks[5], (DEPTH, N_MOD * D), 0.01),
        'norm1': 1.0 + nrm(ks[6], (DEPTH, D), 0.02),
        'norm2': 1.0 + nrm(ks[7], (DEPTH, D), 0.02),
        'w_in': nrm(ks[8], (DEPTH, D, IN_COLS), D ** -0.5),
        'q_norm': 1.0 + nrm(ks[9], (DEPTH, HEAD_DIM), 0.02),
        'k_norm': 1.0 + nrm(ks[10], (DEPTH, HEAD_DIM), 0.02),
        'rpb': nrm(ks[11], (DEPTH, NA_HEADS, 2 * NA_KH_MAX - 1, 2 * NA_KW - 1), 0.1),
        'conv_w': nrm(ks[12], (DEPTH, CONV_K, CONV_WIDTH), CONV_K ** -0.5),
        'sg_norm': 1.0 + nrm(ks[13], (DEPTH, SG_WIDTH), 0.02),
        'sg_w': nrm(ks[14], (DEPTH, SG_GROUPS, CHUNK, CHUNK), CHUNK ** -0.5),
        'sg_b': nrm(ks[15], (DEPTH, SG_GROUPS, CHUNK), 0.1),
        'w_out': nrm(ks[16], (DEPTH, MIX_WIDTH, D), MIX_WIDTH ** -0.5),
        'w_router': nrm(ks[17], (DEPTH, D, N_EXPERTS), D ** -0.5),
        'w_gate': nrm(ks[18], (DEPTH, N_EXPERTS, D, EXPERT_FF), D ** -0.5),
        'w_up': nrm(ks[19], (DEPTH, N_EXPERTS, D, EXPERT_FF), D ** -0.5),
        'w_down': nrm(ks[20], (DEPTH, N_EXPERTS, EXPERT_FF, D), EXPERT_FF ** -0.5),
    }


def reference(x, c, ctx, c_ctx, w_ada, b_ada, norm1, norm2, w_in, q_norm, k_norm, rpb, conv_w,
              sg_norm, sg_w, sg_b, w_out, w_router, w_gate, w_up, w_down):
    xc = ctx
    for layer in range(DEPTH):
        x, xc = hybrid_layer(x, xc, c, c_ctx, w_ada[layer], b_ada[layer], norm1[layer], norm2[layer],
                             w_in[layer], q_norm[layer], k_norm[layer], rpb[layer], conv_w[layer],
                             sg_norm[layer], sg_w[layer], sg_b[layer], w_out[layer], w_router[layer],
                             w_gate[layer], w_up[layer], w_down[layer], update_ctx=layer < DEPTH - 1)
    return x
```

```python
import numpy as np
from contextlib import ExitStack, contextmanager
import concourse.bass as bass
import concourse.mybir as mybir
from concourse.bass_utils import run_bass_kernel_spmd

F32 = mybir.dt.float32
BF16 = mybir.dt.bfloat16
I32 = mybir.dt.int32
AF = mybir.ActivationFunctionType
ALU = mybir.AluOpType
AX = mybir.AxisListType

SEM_WRAP = 1 << 30


class Sync:
    def __init__(self, nc):
        self.nc = nc
        self.stack = ExitStack()
        self.eng = {"pe": nc.tensor, "act": nc.scalar, "dve": nc.vector, "pool": nc.gpsimd, "sp": nc.sync}
        self.cnt = {}
        self.esem = {}
        self.seen = {e: {} for e in self.eng}
        self.rec = {}
        self.dsem = {}
        self.semname = {}
        self.out_tokens = []
        self.nwait = 0
        self.ninst = 0

    @contextmanager
    def ctx(self):
        with self.stack:
            for e in ("pe", "act", "dve", "pool"):
                self.esem[e] = self.stack.enter_context(self.nc.semaphore(f"c_{e}"))
                self.cnt[e] = 0
            yield self

    def sb(self, name, shape, dt):
        return self.stack.enter_context(self.nc.sbuf_tensor(name, list(shape), dt))

    def ps(self, name, shape, dt):
        return self.stack.enter_context(self.nc.psum_tensor(name, list(shape), dt))

    @staticmethod
    def _split(k):
        if isinstance(k, tuple):
            return (k[0] if isinstance(k[0], str) else id(k[0])), k[1]
        return (k if isinstance(k, str) else id(k)), None

    def _recs(self, k, create):
        oid, sub = self._split(k)
        d = self.rec.setdefault(oid, {})
        if sub is None:
            if create and None not in d:
                d[None] = [None, []]
            return list(d.values()) if not create else list(d.values())
        out = []
        if None in d:
            out.append(d[None])
        if sub not in d and create:
            d[sub] = [None, []]
        if sub in d:
            out.append(d[sub])
        return out

    def _tok(self, t):
        sem, val, isdma, key = t
        if isdma and key in self.dsem and self.dsem[key][0] is sem:
            val = max(val, self.dsem[key][1])
        return sem, val

    def _deps(self, r, w):
        deps = {}
        def add(t):
            if t is None:
                return
            sem, val = self._tok(t)
            sid = id(sem)
            if sid not in deps or deps[sid][1] < val:
                deps[sid] = (sem, val)
        for k in r:
            for rc in self._recs(k, False):
                add(rc[0])
        for k in w:
            for rc in self._recs(k, False):
                add(rc[0])
                for t in rc[1]:
                    add(t)
        return deps

    def _commit(self, r, w, tok):
        for k in w:
            oid, sub = self._split(k)
            d = self.rec.setdefault(oid, {})
            if sub is None:
                d.clear()
                d[None] = [tok, []]
            else:
                d[sub] = [tok, []]
        for k in r:
            oid, sub = self._split(k)
            d = self.rec.setdefault(oid, {})
            if sub not in d:
                d[sub] = [None, []]
            rd = d[sub][1]
            rd[:] = [t for t in rd if t[0] is not tok[0]]
            rd.append(tok)

    def _emit_waits(self, e, deps, skip_sem=None):
        eng = self.eng[e]
        for sid, (sem, val) in deps.items():
            if skip_sem is not None and sem is skip_sem:
                continue
            if self.seen[e].get(sid, 0) >= val:
                continue
            eng.wait_ge(sem, val)
            self.nwait += 1
            self.seen[e][sid] = val

    def op(self, e, fn, r=(), w=(), acc=False, sig=True):
        deps = self._deps(r, w)
        self._emit_waits(e, deps, skip_sem=self.esem["pe"] if e == "pe" else None)
        ins = fn(self.eng[e])
        self.ninst += 1
        if sig:
            self.cnt[e] += 1
            ins.then_inc(self.esem[e], 1)
            tok = (self.esem[e], self.cnt[e], False, None)
        else:
            tok = (self.esem[e], self.cnt[e] + 1, False, None)
        self._commit(r, w, tok)
        return ins

    def dma(self, q, out, in_, r=(), w_=(), sem_key=None, out_dram=False, **kw):
        if sem_key is None:
            ks = list(w_) + list(r)
            nk = [k for k in ks if not isinstance(k[0] if isinstance(k, tuple) else k, str)]
            k0 = (nk or ks)[0]
            sem_key = self._split(k0)[0]
        self._get_dsem(sem_key)
        deps = self._deps(r, w_)
        self._emit_waits(q, deps)
        ent = self.dsem[sem_key]
        ent[1] += 16
        ins = self.eng[q].dma_start(out=out, in_=in_, **kw)
        ins.then_inc(ent[0], 16)
        self.ninst += 1
        tok = (ent[0], ent[1], True, sem_key)
        self._commit(r, w_, tok)
        if out_dram:
            self.out_tokens.append(tok)
        return ins

    def _get_dsem(self, sem_key):
        if sem_key not in self.dsem:
            fl = self.__dict__.setdefault("free_sems", [])
            if fl:
                sem, cntv = fl.pop()
                self.dsem[sem_key] = [sem, cntv]
            else:
                self.nsem = getattr(self, "nsem", 0) + 1
                sem = self.stack.enter_context(self.nc.semaphore(f"d{self.nsem}"))
                self.dsem[sem_key] = [sem, 0]

    def finish(self):
        deps = {}
        for t in self.out_tokens:
            sem, val = self._tok(t)
            sid = id(sem)
            if sid not in deps or deps[sid][1] < val:
                deps[sid] = (sem, val)
        for e in ("pe", "act", "dve", "pool"):
            if self.cnt[e] > 0:
                deps[id(self.esem[e])] = (self.esem[e], self.cnt[e])
        for k, (sem, issued) in self.dsem.items():
            if issued > 0:
                deps[id(sem)] = (sem, issued)
        for (sem, issued) in self.__dict__.get("free_sems", []):
            if issued > 0 and id(sem) not in deps:
                deps[id(sem)] = (sem, issued)
        self._emit_waits("sp", deps)


def make_identity(S, ident):
    nc = S.nc
    n = ident.shape[-1]
    def f(e):
        return e.memset(ident[:], 1.0)
    S.op("pool", f, w=[ident])
    def g(e):
        return e.affine_select(out=ident[:], in_=ident[:], pattern=[[1, n]], compare_op=ALU.is_equal,
                               fill=0.0, base=0, channel_multiplier=-1)
    S.op("pool", g, r=[ident], w=[ident])


def _coll(self, kind, op, ins_ap, outs_ap, r=(), w_=(), groups=None, sem_key=None):
    if sem_key is None:
        sem_key = ("coll", len(self.dsem))
    if sem_key not in self.dsem:
        sem = self.stack.enter_context(self.nc.semaphore(f"d{len(self.dsem)}"))
        self.dsem[sem_key] = [sem, 0]
    deps = self._deps(r, w_)
    self._emit_waits("pool", deps)
    ent = self.dsem[sem_key]
    ent[1] += 16
    ins = self.nc.gpsimd.collective_compute(kind, op, replica_groups=groups, ins=[ins_ap], outs=[outs_ap])
    ins.then_inc(ent[0], 16)
    tok = (ent[0], ent[1], True, sem_key)
    self._commit(r, w_, tok)
    return ins

Sync.coll = _coll


def _dma_custom(self, q, fn, r=(), w_=(), sem_key=None, out_dram=False):
    if sem_key is None:
        ks = list(w_) + list(r)
        nk = [k for k in ks if not isinstance(k[0] if isinstance(k, tuple) else k, str)]
        k0 = (nk or ks)[0]
        sem_key = self._split(k0)[0]
    self._get_dsem(sem_key)
    deps = self._deps(r, w_)
    self._emit_waits(q, deps)
    ent = self.dsem[sem_key]
    ent[1] += 16
    ins = fn(self.eng[q])
    ins.then_inc(ent[0], 16)
    self.ninst += 1
    tok = (ent[0], ent[1], True, sem_key)
    self._commit(r, w_, tok)
    if out_dram:
        self.out_tokens.append(tok)
    return ins

Sync.dma_custom = _dma_custom


U32 = mybir.dt.uint32
EPS = 1e-6


def _scope_push(self):
    st = ExitStack()
    self.tstacks.append(st)


def _scope_pop(self):
    self.barrier()
    self.tstacks.pop().close()
    fl = self.__dict__.setdefault("free_sems", [])
    for k, (sem, issued) in self.dsem.items():
        fl.append((sem, issued))
    self.dsem.clear()
    self.rec.clear()


def _barrier(self):
    deps = {}
    for e in ("pe", "act", "dve", "pool"):
        if self.cnt[e] > 0:
            deps[id(self.esem[e])] = (self.esem[e], self.cnt[e])
    for k, (sem, issued) in self.dsem.items():
        if issued > 0:
            deps[id(sem)] = (sem, issued)
    for e in ("pe", "act", "dve", "pool", "sp"):
        self._emit_waits(e, dict(deps))


def _sb2(self, name, shape, dt):
    self.uid = getattr(self, "uid", 0) + 1
    st = self.tstacks[-1] if getattr(self, "tstacks", None) else self.stack
    return st.enter_context(self.nc.sbuf_tensor(f"{name}_{self.uid}", list(shape), dt))


def _ps2(self, name, shape, dt):
    self.uid = getattr(self, "uid", 0) + 1
    st = self.tstacks[-1] if getattr(self, "tstacks", None) else self.stack
    return st.enter_context(self.nc.psum_tensor(f"{name}_{self.uid}", list(shape), dt))


Sync.push = _scope_push
Sync.pop = _scope_pop
Sync.barrier = _barrier
Sync.sb = _sb2
Sync.ps = _ps2


def make_cfg(D=4096, SEQ=8192, CTX=256, E=16, FF=1024, DEPTH=2):
    c = dict(D=D, SEQ=SEQ, CTX=CTX, E=E, FF=FF, DEPTH=DEPTH)
    c["H"] = (D // 2) // 128
    c["NG"] = c["H"] // 2
    c["CW"] = D // 4
    c["SGW"] = D // 4
    assert c["CW"] == c["NG"] * 128
    c["KD"] = D // 128
    c["NTL"] = SEQ // 128
    c["NTC"] = CTX // 128
    c["NT"] = c["NTL"] + c["NTC"]
    c["NTOK"] = SEQ + CTX
    c["ROWS"] = SEQ // 64
    c["capL"] = 2 * SEQ // E
    c["capC"] = 2 * CTX // E
    c["SLABC"] = 11 * 128
    return c


def build(cfg):
    D, SEQ, CTX, E, FF, DEPTH = (cfg[k] for k in ("D", "SEQ", "CTX", "E", "FF", "DEPTH"))
    H, NG, CW, SGW, KD, NTL, NTC, NT, NTOK, ROWS = (cfg[k] for k in ("H", "NG", "CW", "SGW", "KD", "NTL", "NTC", "NT", "NTOK", "ROWS"))
    capL, capC, SLABC = cfg["capL"], cfg["capC"], cfg["SLABC"]
    KM = KD
    FC = FF // 128
    P = 128
    nc = bass.Bass("TRN2", target_bir_lowering=False)

    def din(name, shape, dt=F32):
        return nc.dram_tensor(name, list(shape), dt, kind="ExternalInput").ap()

    x_in = din("x", [SEQ, D]); ctx_in = din("ctx", [CTX, D]); cvec = din("cvec", [2 * KD, 128])
    w_ada = din("w_ada", [DEPTH, D, 6 * D]); b_ada = din("b_ada", [DEPTH, 1, 6 * D])
    norm1 = din("norm1", [DEPTH, D]); norm2 = din("norm2", [DEPTH, D])
    w_in = din("w_in", [DEPTH, D, NG * SLABC])
    q_norm = din("q_norm", [DEPTH, 128]); k_norm = din("k_norm", [DEPTH, 128])
    rpbT = din("rpbT", [DEPTH, H, 128, 15 * 64]); maskT = din("maskT", [128, 15 * 64])
    conv_w = din("conv_w", [DEPTH, NG, 128, 3])
    sg_norm = din("sg_norm", [DEPTH, SGW]); sg_wT = din("sg_wT", [DEPTH, 128, NG * 128]); sg_b = din("sg_b", [DEPTH, NG * 128])
    w_out = din("w_out", [DEPTH, D, D]); w_router = din("w_router", [DEPTH, D, E])
    w_gate = din("w_gate", [DEPTH, E, D, FF]); w_up = din("w_up", [DEPTH, E, D, FF]); w_down = din("w_down", [DEPTH, E, FF, D])
    out = nc.dram_tensor("out", [SEQ, D], F32, kind="ExternalOutput").ap()

    def dsc(name, shape, dt):
        return nc.dram_tensor(name, list(shape), dt).ap()

    SLOTS_MAX = ((capL + capC + 127) // 128) * 128
    mods_d = dsc("mods_d", [2 * 6 * KD, 128], F32)
    x_d = dsc("x_d", [NTOK, D], F32)
    hT_d = dsc("hT_d", [128, KD, NTOK], BF16)
    htm_d = dsc("htm_d", [NTOK + 128, D], BF16)
    M_d = dsc("M_d", [NTOK + 128, D], F32)
    qT_d = dsc("qT_d", [H, 128, NTOK], BF16); kT_d = dsc("kT_d", [H, 128, NTOK], BF16)
    v_d = dsc("v_d", [NTOK, H * 128], BF16)
    zT_d = dsc("zT_d", [NG, 128, NTOK], BF16); gbT_d = dsc("gbT_d", [NG, 128, NTOK], BF16)
    suT_d = dsc("suT_d", [NG, 128, NTOK], BF16); sv_d = dsc("sv_d", [NTOK, SGW], BF16)
    mT_d = dsc("mT_d", [128, KM, NTOK], BF16)
    Y_d = dsc("Y_d", [E * SLOTS_MAX, D], BF16)

    S = Sync(nc)
    S.tstacks = []
    mods_v = mods_d.rearrange("(v m k) p -> v m (k p)", v=2, m=6)

    def bc_row(ap1d, n):
        return ap1d.rearrange("(o d) -> o d", o=1).to_broadcast([P, n])

    with S.ctx():
        ident_b = S.sb("identb", [P, P], BF16); make_identity(S, ident_b)
        ident_f = S.sb("identf", [P, P], F32); make_identity(S, ident_f)
        ones_b = S.sb("onesb", [P, P], BF16); S.op("pool", lambda e: e.memset(ones_b[:], 1.0), w=[ones_b])
        ones_f = S.sb("onesf", [P, P], F32); S.op("pool", lambda e: e.memset(ones_f[:], 1.0), w=[ones_f])
        epsc = S.sb("epsc", [P, 1], F32); S.op("pool", lambda e: e.memset(epsc[:], EPS), w=[epsc])
        ustr = S.sb("ustr", [P, P], F32); S.op("pool", lambda e: e.memset(ustr[:], 1.0), w=[ustr])
        S.op("pool", lambda e: e.affine_select(out=ustr[:], in_=ustr[:], pattern=[[1, P]], compare_op=ALU.is_ge, fill=0.0,
                                               base=-1, channel_multiplier=-1), r=[ustr], w=[ustr])
        aff_e = S.sb("aff_e", [P, E, NT], F32)
        msk = S.sb("msk", [P, E, NT], F32)
        gate = S.sb("gate", [P, E, NT], F32)
        posm = S.sb("posm", [P, E, NT], F32)

        S.push()
        cp = [S.sb("cp", [P, D], F32) for _ in range(2)]
        for ti in range(NT):
            src = x_in[ti * P:(ti + 1) * P, :] if ti < NTL else ctx_in[(ti - NTL) * P:(ti - NTL + 1) * P, :]
            t = cp[ti % 2]
            S.dma("sp", t[:], src, w_=[t])
            S.dma("act", x_d[ti * P:(ti + 1) * P, :], t[:], r=[t], w_=[("x_d", ti)])
        S.pop()

        def rstd_of(ss, n, tag):
            sd = S.sb("sd" + tag, [P, 1], F32); rs = S.sb("rs" + tag, [P, 1], F32)
            return sd, rs

        def phase_ada(l):
            S.push()
            cv = S.sb("cv", [2 * KD, P], F32)
            S.dma("sp", cv[:], cvec, w_=[cv])
            S.op("act", lambda e: e.activation(cv[:], cv[:], AF.Silu), r=[cv], w=[cv])
            pc = S.ps("pc", [P, 2 * KD], F32)
            S.op("pe", lambda e: e.transpose(pc[:], cv[:], ident_f[:2 * KD, :2 * KD]), r=[cv, ident_f], w=[pc])
            cT = S.sb("cT", [P, KD, 2], F32)
            for v in range(2):
                S.op("dve", lambda e: e.tensor_copy(cT[:, :, v], pc[:, v * KD:(v + 1) * KD]), r=[pc], w=[(cT, v)])
            brow = S.sb("brow", [1, 6 * D], F32)
            S.dma("sp", brow[:], b_ada[l], w_=[brow])
            modsT = S.sb("modsT", [P, 2, 6 * KD], F32)
            CG = 256
            wa = [S.sb("wa", [P, KD, CG], F32) for _ in range(2)]
            pm = [S.ps("pm", [P, 2], F32) for _ in range(2)]
            for cg in range(6 * D // CG):
                w = wa[cg % 2]
                S.dma("sp" if cg % 2 == 0 else "act", w[:], w_ada[l][:, cg * CG:(cg + 1) * CG].rearrange("(k p) c -> p k c", p=P), w_=[w])
                for sub in range(CG // P):
                    j = cg * (CG // P) + sub
                    pp = pm[j % 2]
                    for k in range(KD):
                        S.op("pe", lambda e: e.matmul(pp[:], w[:, k, sub * P:(sub + 1) * P], cT[:, k, :], start=(k == 0), stop=False),
                             r=[w, cT], w=[pp], sig=False)
                    S.op("pe", lambda e: e.matmul(pp[:], brow[0:1, j * P:(j + 1) * P], ones_f[0:1, 0:2], start=False, stop=True),
                         r=[brow, ones_f], w=[pp])
                    S.op("dve", lambda e: e.tensor_copy(modsT[:, :, j], pp[:]), r=[pp], w=[(modsT, j)])
            mflat = modsT[:].rearrange("p v j -> p (v j)")
            nrow = 2 * 6 * KD
            for r0 in range(0, nrow, P):
                n = min(P, nrow - r0)
                pt = S.ps("ptm", [P, P], F32)
                S.op("pe", lambda e: e.transpose(pt[:n, :], mflat[:, r0:r0 + n], ident_f[:]), r=[modsT, ident_f], w=[pt])
                mt = S.sb("mtm", [P, P], F32)
                S.op("dve", lambda e: e.tensor_copy(mt[:n, :], pt[:n, :]), r=[pt], w=[mt])
                S.dma("sp", mods_d[r0:r0 + n, :], mt[:n, :], r=[mt], w_=["mods_d"])
            S.pop()

        def phase_norm(l, tiles, normw, m_sh, m_sc, router):
            S.push()
            gam = {}; bet = {}
            nw = S.sb("nw", [P, D], F32)
            S.dma("sp", nw[:], bc_row(normw[l], D), w_=[nw])
            for v in sorted(set(0 if ti < NTL else 1 for ti in tiles)):
                g = S.sb("gam", [P, D], F32); b = S.sb("bet", [P, D], F32)
                S.dma("sp", g[:], bc_row(mods_v[v, m_sc], D), r=["mods_d"], w_=[g])
                S.dma("act", b[:], bc_row(mods_v[v, m_sh], D), r=["mods_d"], w_=[b])
                S.op("dve", lambda e: e.scalar_tensor_tensor(out=g[:], in0=g[:], scalar=1.0, in1=nw[:], op0=ALU.add, op1=ALU.mult), r=[g, nw], w=[g])
                gam[v] = g; bet[v] = b
            if router:
                wrf = S.sb("wrf", [P, KD, E], F32); wr = S.sb("wr", [P, KD, E], BF16)
                S.dma("sp", wrf[:], w_router[l].rearrange("(k p) e -> p k e", p=P), w_=[wrf])
                S.op("dve", lambda e: e.tensor_copy(wr[:], wrf[:]), r=[wrf], w=[wr])
            xts = [S.sb("xt", [P, D], F32) for _ in range(2)]
            junk = S.sb("junk", [P, D], BF16)
            hbs = [S.sb("hb", [P, D], BF16) for _ in range(2)]
            GN = 2
            hTs = [S.sb("hTt", [P, KD, GN * P], BF16) for _ in range(2)]
            groups = []
            for ti in tiles:
                if groups and len(groups[-1]) < GN and groups[-1][-1] == ti - 1 and (ti < NTL) == (groups[-1][0] < NTL):
                    groups[-1].append(ti)
                else:
                    groups.append([ti])
            gidx = {}
            for gi, grp in enumerate(groups):
                for j, ti in enumerate(grp):
                    gidx[ti] = (gi, j, len(grp))
            pts = [S.ps("ptn", [P, P], BF16) for _ in range(4)]
            plg = S.ps("plg", [P, E], F32)
            ss = S.sb("ss", [P, 1], F32); sd = S.sb("sd", [P, 1], F32); rs = S.sb("rs", [P, 1], F32)
            mx = S.sb("mx", [P, 1], F32); sm = S.sb("sm", [P, 1], F32); ex = S.sb("ex", [P, E], F32)
            for i, ti in enumerate(tiles):
                v = 0 if ti < NTL else 1
                gi, gj, gn = gidx[ti]
                xt = xts[i % 2]; hb = hbs[i % 2]; hTg = hTs[gi % 2]
                hTt = hTg[:, :, gj * P:(gj + 1) * P]
                S.dma("sp", xt[:], x_d[ti * P:(ti + 1) * P, :], r=[("x_d", ti)], w_=[xt])
                S.op("act", lambda e: e.activation(junk[:], xt[:], AF.Square, accum_out=ss[:]), r=[xt], w=[junk, ss])
                S.op("act", lambda e: e.activation(sd[:], ss[:], AF.Sqrt, bias=epsc[:], scale=1.0 / D), r=[ss, epsc], w=[sd])
                S.op("dve", lambda e: e.reciprocal(rs[:], sd[:]), r=[sd], w=[rs])
                S.op("dve", lambda e: e.scalar_tensor_tensor(out=xt[:], in0=xt[:], scalar=rs[:, 0:1], in1=gam[v][:], op0=ALU.mult, op1=ALU.mult),
                     r=[xt, rs, gam[v]], w=[xt])
                S.op("dve", lambda e: e.tensor_tensor(out=hb[:], in0=xt[:], in1=bet[v][:], op=ALU.add), r=[xt, bet[v]], w=[hb])
                if router:
                    S.dma("act", htm_d[ti * P:(ti + 1) * P, :], hb[:], r=[hb], w_=[("htm_d", ti)])
                for k in range(KD):
                    pt = pts[k % 4]
                    S.op("pe", lambda e: e.transpose(pt[:], hb[:, k * P:(k + 1) * P], ident_b[:]), r=[hb, ident_b], w=[pt])
                    if k % 2 == 0:
                        S.op("act", lambda e: e.activation(hTt[:, k, :], pt[:], AF.Copy), r=[pt], w=[(hTg, (gj, k))])
                    else:
                        S.op("dve", lambda e: e.tensor_copy(hTt[:, k, :], pt[:]), r=[pt], w=[(hTg, (gj, k))])
                if gj == gn - 1:
                    t0g = (ti - gn + 1) * P
                    S.dma("sp", hT_d[:, :, t0g:t0g + gn * P], hTg[:, :, 0:gn * P], r=[hTg], w_=[("hT_d", ti)])
                if router:
                    for k in range(KD):
                        S.op("pe", lambda e: e.matmul(plg[:], hTt[:, k, :], wr[:, k, :], start=(k == 0), stop=(k == KD - 1)),
                             r=[hTg, wr], w=[plg], sig=(k == KD - 1))
                    S.op("dve", lambda e: e.tensor_reduce(out=mx[:], in_=plg[:], axis=AX.X, op=ALU.max), r=[plg], w=[mx])
                    S.op("dve", lambda e: e.tensor_scalar(mx[:], mx[:], -1.0, None, op0=ALU.mult), r=[mx], w=[mx])
                    S.op("act", lambda e: e.activation(ex[:], plg[:], AF.Exp, bias=mx[:], scale=1.0, accum_out=sm[:]), r=[plg, mx], w=[ex, sm])
                    S.op("dve", lambda e: e.reciprocal(sm[:], sm[:]), r=[sm], w=[sm])
                    S.op("dve", lambda e: e.tensor_scalar(aff_e[:, :, ti], ex[:], sm[:, 0:1], None, op0=ALU.mult), r=[ex, sm], w=[(aff_e, ti)])
            S.pop()

        def phase_inproj(l):
            S.push()
            qg = S.sb("qg", [P, 1], F32); kg = S.sb("kg", [P, 1], F32)
            S.dma("sp", qg[:], q_norm[l].rearrange("(p o) -> p o", o=1), w_=[qg])
            S.dma("sp", kg[:], k_norm[l].rearrange("(p o) -> p o", o=1), w_=[kg])
            S.op("dve", lambda e: e.tensor_scalar(qg[:], qg[:], float(128 ** -0.5), None, op0=ALU.mult), r=[qg], w=[qg])
            wsl = S.sb("wsl", [P, KD, SLABC], BF16)
            stg = [S.sb("stg", [P, 1, SLABC], F32) for _ in range(2)]
            hTb = [S.sb("hTb", [P, KD, 512], BF16) for _ in range(2)]
            pf = [S.ps("pf", [P, 512], F32) for _ in range(3)]
            pss = S.ps("pss", [P, 512], F32)
            ptm = [S.ps("ptmm", [P, 512], F32) for _ in range(2)]
            qf = S.sb("qf", [P, 512], F32); sq = S.sb("sq", [P, 512], BF16); rq = S.sb("rq", [P, 512], F32)
            ob = [S.sb("ob", [P, 512], BF16) for _ in range(3)]
            xin_sb = S.sb("xin_sb", [P, 512], F32)
            ot = [S.sb("ot", [P, 384], BF16) for _ in range(2)]
            nblk = (NTOK + 511) // 512
            cnt = 0
            for g in range(NG):
                for kk in range(0, KD):
                    st = stg[kk % 2]
                    S.dma("sp" if kk % 2 == 0 else "act", st[:],
                          w_in[l][kk * P:(kk + 1) * P, g * SLABC:(g + 1) * SLABC].rearrange("(k p) c -> p k c", p=P), w_=[st])
                    S.op("pool", lambda e: e.tensor_copy(wsl[:, kk:kk + 1, :], st[:]), r=[st], w=[(wsl, kk)])
                for tb in range(nblk):
                    t0 = tb * 512; nb = min(512, NTOK - t0)
                    hb = hTb[tb % 2]
                    S.dma("sp", hb[:, :, :nb], hT_d[:, :, t0:t0 + nb], r=["hT_d"], w_=[hb])
                    for j in range(8):
                        pp = pf[j % 3]
                        for k in range(KD):
                            S.op("pe", lambda e: e.matmul(pp[:, :nb], wsl[:, k, j * P:(j + 1) * P], hb[:, k, :nb], start=(k == 0), stop=(k == KD - 1)),
                                 r=[wsl, hb], w=[pp], sig=(k == KD - 1))
                        o = ob[cnt % 3]; cnt += 1
                        if j < 4:
                            gn = qg if j < 2 else kg
                            head = 2 * g + (j % 2)
                            dst = (qT_d if j < 2 else kT_d)[head][:, t0:t0 + nb]
                            S.op("act", lambda e: e.activation(qf[:, :nb], pp[:, :nb], AF.Copy), r=[pp], w=[qf])
                            S.op("act", lambda e: e.activation(sq[:, :nb], pp[:, :nb], AF.Square), r=[pp], w=[sq])
                            S.op("pe", lambda e: e.matmul(pss[:, :nb], ones_b[:], sq[:, :nb], start=True, stop=True), r=[ones_b, sq], w=[pss])
                            S.op("act", lambda e: e.activation(rq[:, :nb], pss[:, :nb], AF.Sqrt, bias=epsc[:], scale=1.0 / 128), r=[pss, epsc], w=[rq])
                            S.op("dve", lambda e: e.reciprocal(rq[:, :nb], rq[:, :nb]), r=[rq], w=[rq])
                            S.op("dve", lambda e: e.scalar_tensor_tensor(out=o[:, :nb], in0=qf[:, :nb], scalar=gn[:, 0:1], in1=rq[:, :nb], op0=ALU.mult, op1=ALU.mult),
                                 r=[qf, gn, rq], w=[o])
                            S.dma("act", dst, o[:, :nb], r=[o], w_=[("qT_d" if j < 2 else "kT_d", head)])
                        elif j == 4:
                            S.op("act", lambda e: e.activation(xin_sb[:, :nb], pp[:, :nb], AF.Copy), r=[pp], w=[xin_sb])
                        elif j == 6:
                            S.op("dve", lambda e: e.tensor_tensor(out=o[:, :nb], in0=pp[:, :nb], in1=xin_sb[:, :nb], op=ALU.mult), r=[pp, xin_sb], w=[o])
                            S.dma("act", zT_d[g][:, t0:t0 + nb], o[:, :nb], r=[o], w_=[("zT_d", g)])
                        else:
                            S.op("act", lambda e: e.activation(o[:, :nb], pp[:, :nb], AF.Copy), r=[pp], w=[o])
                            dd = gbT_d if j == 5 else suT_d
                            S.dma("act", dd[g][:, t0:t0 + nb], o[:, :nb], r=[o], w_=[("dd", g)])
                    for sub in range(nb // P):
                        pt = ptm[sub % 2]; o2 = ot[sub % 2]
                        for k in range(KD):
                            S.op("pe", lambda e: e.matmul(pt[:, 0:384], hb[:, k, sub * P:(sub + 1) * P], wsl[:, k, 1024:1408], start=(k == 0), stop=(k == KD - 1)),
                                 r=[hb, wsl], w=[pt], sig=(k == KD - 1))
                        S.op("dve", lambda e: e.tensor_copy(o2[:], pt[:, 0:384]), r=[pt], w=[o2])
                        r0 = t0 + sub * P
                        S.dma("act", v_d[r0:r0 + P, 2 * g * P:(2 * g + 2) * P], o2[:, 0:256], r=[o2], w_=[("v_d", g)])
                        S.dma("act", sv_d[r0:r0 + P, g * P:(g + 1) * P], o2[:, 256:384], r=[o2], w_=[("sv_d", g)])
            S.pop()

        def phase_attn(l, upd):
            S.push()
            NCH = 4 + NTC
            mk_ = S.sb("mk_", [P, 15 * 64], F32)
            S.dma("sp", mk_[:], maskT, w_=[mk_])
            kT = S.sb("kT", [P, NTOK], BF16); qT = S.sb("qT", [P, NTOK], BF16)
            V = S.sb("V", [P, NT, P], BF16); Vs = S.sb("Vs", [P, NTL - 1, P], BF16)
            TB = S.sb("TB", [P, 15, 64], F32)
            aT = S.sb("aT", [P, NTOK], BF16)
            sp_ = [S.ps("sp_", [P, 8, 64], F32) for _ in range(4)]
            popr = [S.ps("popr", [P, 2, P], F32) for _ in range(4)]
            po = [t[:, 0, :] for t in popr]
            pr = [t[:, 1, :] for t in popr]
            pokey = popr
            sbb = [S.sb("sbb", [P, 4, 64], F32) for _ in range(4)]
            pT = [S.sb("pT", [P, NCH, 64], BF16) for _ in range(4)]
            pT2 = S.sb("pT2", [P, NTC, P], BF16)
            rinv = [S.sb("rinv", [P, P], F32) for _ in range(4)]
            TBf = TB[:].rearrange("p a b -> p (a b)")
            for h in range(H):
                S.dma("sp", kT[:], kT_d[h], r=["kT_d"], w_=[kT])
                S.dma("act", qT[:], qT_d[h], r=["qT_d"], w_=[qT])
                S.dma("sp", V[:], v_d[:, h * P:(h + 1) * P].rearrange("(t p) c -> p t c", p=P), r=["v_d"], w_=[V])
                S.dma("act", Vs[:], v_d[64:64 + (NTL - 1) * P, h * P:(h + 1) * P].rearrange("(t p) c -> p t c", p=P), r=["v_d"], w_=[Vs])
                S.dma("sp", TBf, rpbT[l, h], w_=[TB])
                S.op("dve", lambda e: e.tensor_tensor(out=TBf, in0=TBf, in1=mk_[:], op=ALU.add), r=[TB, mk_], w=[TB])
                def stage_a(r):
                    s = min(max(r - 4, 0), ROWS - 8); off = s - r + 7; tok0 = 64 * s
                    ps_ = sp_[r % 4]; pp = pT[r % 4]; sb_ = sbb[r % 4]
                    qs = qT[:, 64 * r:64 * r + 64]
                    for c in range(NCH):
                        ks = kT[:, tok0 + P * c: tok0 + P * (c + 1)] if c < 4 else kT[:, SEQ + P * (c - 4): SEQ + P * (c - 3)]
                        S.op("pe", lambda e: e.matmul(ps_[:, c, :], ks, qs, start=True, stop=True), r=[kT, qT], w=[ps_], sig=(c == NCH - 1))
                    S.op("dve", lambda e: e.tensor_tensor(out=sb_[:], in0=ps_[:, 0:4, :], in1=TB[:, off:off + 8:2, :], op=ALU.add), r=[ps_, TB], w=[sb_])
                    S.op("act", lambda e: e.activation(pp[:, 0:4, :], sb_[:], AF.Exp), r=[sb_], w=[(pp, 0)])
                    S.op("act", lambda e: e.activation(pp[:, 4:, :], ps_[:, 4:NCH, :], AF.Exp), r=[ps_], w=[(pp, 1)])

                def stage_b(r):
                    s = min(max(r - 4, 0), ROWS - 8)
                    pp = pT[r % 4]; pov = po[r % 4]; prv = pr[r % 4]; ri = rinv[r % 4]
                    for c in range(NCH):
                        if c < 4:
                            vt = V[:, s // 2 + c, :] if s % 2 == 0 else Vs[:, (s - 1) // 2 + c, :]
                        else:
                            vt = V[:, NTL + (c - 4), :]
                        S.op("pe", lambda e: e.matmul(pov[:, 0:64], vt, pp[:, c, :], start=(c == 0), stop=(c == NCH - 1)), r=[V, Vs, pp], w=[pov], sig=(c == NCH - 1))
                    for c in range(NCH):
                        S.op("pe", lambda e: e.matmul(prv[:, 0:64], ones_b[:], pp[:, c, :], start=(c == 0), stop=(c == NCH - 1)), r=[ones_b, pp], w=[prv], sig=(c == NCH - 1))
                    S.op("dve", lambda e: e.reciprocal(ri[:, 0:64], prv[:, 0:64]), r=[prv], w=[ri])
                    S.op("dve", lambda e: e.tensor_tensor(out=aT[:, 64 * r:64 * r + 64], in0=pov[:, 0:64], in1=ri[:, 0:64], op=ALU.mult), r=[pov, ri], w=[(aT, r)])

                LAG = 2
                for step in range(ROWS + LAG):
                    if step < ROWS:
                        stage_a(step)
                    if step >= LAG:
                        stage_b(step - LAG)
                nq = SEQ
                if upd:
                    nq = NTOK
                    for qb in range(NTC):
                        ps_ = sp_[qb % 2]; pov = po[qb % 2]; prv = pr[qb % 2]; ri = rinv[qb % 2]
                        psv = ps_[:].rearrange("p a b -> p (a b)")
                        qs = qT[:, SEQ + P * qb: SEQ + P * (qb + 1)]
                        for c in range(NTC):
                            S.op("pe", lambda e: e.matmul(psv[:, c * P:(c + 1) * P], kT[:, SEQ + P * c: SEQ + P * (c + 1)], qs, start=True, stop=True),
                                 r=[kT, qT], w=[ps_], sig=(c == NTC - 1))
                        S.op("act", lambda e: e.activation(pT2[:].rearrange("p a b -> p (a b)"), psv[:, 0:NTC * P], AF.Exp), r=[ps_], w=[pT2])
                        for c in range(NTC):
                            S.op("pe", lambda e: e.matmul(pov[:], V[:, NTL + c, :], pT2[:, c, :], start=(c == 0), stop=(c == NTC - 1)), r=[V, pT2], w=[pov], sig=(c == NTC - 1))
                        for c in range(NTC):
                            S.op("pe", lambda e: e.matmul(prv[:], ones_b[:], pT2[:, c, :], start=(c == 0), stop=(c == NTC - 1)), r=[ones_b, pT2], w=[prv], sig=(c == NTC - 1))
                        S.op("dve", lambda e: e.reciprocal(ri[:], prv[:]), r=[prv], w=[ri])
                        S.op("dve", lambda e: e.tensor_tensor(out=aT[:, SEQ + P * qb: SEQ + P * (qb + 1)], in0=pov[:], in1=ri[:], op=ALU.mult), r=[pov, ri], w=[(aT, 1000 + qb)])
                S.dma("sp", mT_d[:, h, 0:nq], aT[:, 0:nq], r=[aT], w_=[("mT_d", h)])
            S.pop()

        def phase_conv_sg(l, upd):
            S.push()
            z = S.sb("z", [P, NTOK], BF16); gb = S.sb("gb", [P, NTOK], BF16)
            acc = S.sb("acc", [P, NTOK], F32); y = S.sb("y", [P, NTOK], BF16)
            cw = S.sb("cw", [P, 3], F32)
            seqs = [(0, SEQ)] + ([(SEQ, CTX)] if upd else [])
            nq = NTOK if upd else SEQ
            for g in range(NG):
                S.dma("sp", z[:], zT_d[g], r=["zT_d"], w_=[z])
                S.dma("act", gb[:], gbT_d[g], r=["gbT_d"], w_=[gb])
                S.dma("sp", cw[:], conv_w[l, g], w_=[cw])
                for (a, n) in seqs:
                    S.op("dve", lambda e: e.tensor_scalar(acc[:, a:a + n], z[:, a:a + n], cw[:, 1:2], None, op0=ALU.mult), r=[z, cw], w=[acc])
                    S.op("dve", lambda e: e.scalar_tensor_tensor(out=acc[:, a + 1:a + n], in0=z[:, a:a + n - 1], scalar=cw[:, 0:1], in1=acc[:, a + 1:a + n], op0=ALU.mult, op1=ALU.add),
                         r=[z, cw, acc], w=[acc])
                    S.op("dve", lambda e: e.scalar_tensor_tensor(out=acc[:, a:a + n - 1], in0=z[:, a + 1:a + n], scalar=cw[:, 2:3], in1=acc[:, a:a + n - 1], op0=ALU.mult, op1=ALU.add),
                         r=[z, cw, acc], w=[acc])
                    S.op("dve", lambda e: e.tensor_tensor(out=y[:, a:a + n], in0=acc[:, a:a + n], in1=gb[:, a:a + n], op=ALU.mult), r=[acc, gb], w=[y])
                S.dma("sp", mT_d[:, H + g, 0:nq], y[:, 0:nq], r=[y], w_=[("mT_d", H + g)])
            S.pop()
            S.push()
            sgn = S.sb("sgn", [P, SGW], F32)
            S.dma("sp", sgn[:], bc_row(sg_norm[l], SGW), w_=[sgn])
            swf = S.sb("swf", [P, NG * P], F32); sw = S.sb("sw", [P, NG * P], BF16)
            S.dma("sp", swf[:], sg_wT[l], w_=[swf])
            S.op("dve", lambda e: e.tensor_copy(sw[:], swf[:]), r=[swf], w=[sw])
            sgb = S.sb("sgb", [P, NG * P], F32)
            S.dma("sp", sgb[:], bc_row(sg_b[l], NG * P), w_=[sgb])
            svt = [S.sb("svt", [P, SGW], BF16) for _ in range(2)]
            sut = [S.sb("sut", [P, NG, P], BF16) for _ in range(2)]
            vn = [S.sb("vn", [P, SGW], BF16) for _ in range(2)]
            junk = S.sb("junk2", [P, SGW], BF16)
            ss = S.sb("ss2", [P, 1], F32); sd = S.sb("sd2", [P, 1], F32); rs = S.sb("rs2", [P, 1], F32)
            pmx = [S.ps("pmx", [P, P], F32) for _ in range(2)]
            tmp = [S.sb("tmp", [P, P], F32) for _ in range(2)]
            og = [S.sb("og", [P, NG, P], BF16) for _ in range(2)]
            tiles = list(range(NTL)) + (list(range(NTL, NT)) if upd else [])
            for i, ti in enumerate(tiles):
                sv = svt[i % 2]; su = sut[i % 2]; vv = vn[i % 2]; o = og[i % 2]
                S.dma("sp", sv[:], sv_d[ti * P:(ti + 1) * P, :], r=["sv_d"], w_=[sv])
                S.dma("act", su[:], suT_d[:, :, ti * P:(ti + 1) * P].rearrange("g p t -> p g t"), r=["suT_d"], w_=[su])
                S.op("act", lambda e: e.activation(junk[:], sv[:], AF.Square, accum_out=ss[:]), r=[sv], w=[junk, ss])
                S.op("act", lambda e: e.activation(sd[:], ss[:], AF.Sqrt, bias=epsc[:], scale=1.0 / SGW), r=[ss, epsc], w=[sd])
                S.op("dve", lambda e: e.reciprocal(rs[:], sd[:]), r=[sd], w=[rs])
                S.op("dve", lambda e: e.scalar_tensor_tensor(out=vv[:], in0=sv[:], scalar=rs[:, 0:1], in1=sgn[:], op0=ALU.mult, op1=ALU.mult), r=[sv, rs, sgn], w=[vv])
                for g in range(NG):
                    pm = pmx[g % 2]; tp = tmp[g % 2]
                    S.op("pe", lambda e: e.matmul(pm[:], vv[:, g * P:(g + 1) * P], sw[:, g * P:(g + 1) * P], start=True, stop=True), r=[vv, sw], w=[pm])
                    S.op("dve", lambda e: e.tensor_tensor(out=tp[:], in0=pm[:], in1=sgb[:, g * P:(g + 1) * P], op=ALU.add), r=[pm, sgb], w=[tp])
                    S.op("dve", lambda e: e.tensor_tensor(out=o[:, g, :], in0=tp[:], in1=su[:, g, :], op=ALU.mult), r=[tp, su], w=[(o, g)])
                S.dma("sp", mT_d[:, H + NG:H + 2 * NG, ti * P:(ti + 1) * P], o[:], r=[o], w_=[("mT_d", 5000 + ti)])
            S.pop()

        def phase_outproj(l, upd):
            S.push()
            OC = min(1024, D)
            wo = S.sb("wo", [P, KM, OC], BF16)
            stg = [S.sb("stgo", [P, 2, OC], F32) for _ in range(2)]
            g1 = {v: S.sb("g1", [P, OC], F32) for v in ((0, 1) if upd else (0,))}
            mts = [S.sb("mt", [P, KM, 4 * P], BF16) for _ in range(2)]
            xts = [S.sb("xto", [P, OC], F32) for _ in range(2)]
            xos = [S.sb("xoo", [P, OC], F32) for _ in range(2)]
            pps = [S.ps("ppo", [P, 512], F32) for _ in range(2)]
            tiles = list(range(NTL)) + (list(range(NTL, NT)) if upd else [])
            for cs in range(D // OC):
                for kk in range(0, KM, 2):
                    st = stg[(kk // 2) % 2]
                    S.dma("sp" if (kk // 2) % 2 == 0 else "act", st[:], w_out[l][kk * P:(kk + 2) * P, cs * OC:(cs + 1) * OC].rearrange("(k p) c -> p k c", p=P), w_=[st])
                    S.op("pool", lambda e: e.tensor_copy(wo[:, kk:kk + 2, :], st[:]), r=[st], w=[(wo, kk)])
                for v in g1:
                    S.dma("sp", g1[v][:], bc_row(mods_v[v, 2, cs * OC:(cs + 1) * OC], OC), r=["mods_d"], w_=[g1[v]])
                for i, ti in enumerate(tiles):
                    v = 0 if ti < NTL else 1
                    xt = xts[i % 2]; xo = xos[i % 2]
                    if ti < NTL:
                        g0 = (ti // 4) * 4; gn = min(4, NTL - g0)
                    else:
                        g0 = NTL + ((ti - NTL) // 4) * 4; gn = min(4, NT - g0)
                    mtg = mts[(g0 // 4) % 2] if ti < NTL else mts[((NTL + 3) // 4 + (ti - NTL) // 4) % 2]
                    if ti == g0:
                        S.dma("sp", mtg[:, :, 0:gn * P], mT_d[:, :, g0 * P:(g0 + gn) * P], r=["mT_d"], w_=[mtg])
                    mt = mtg[:, :, (ti - g0) * P:(ti - g0 + 1) * P]
                    S.dma("act", xt[:], x_d[ti * P:(ti + 1) * P, cs * OC:(cs + 1) * OC], r=[("x_d", ti)], w_=[xt])
                    for hf in range(OC // 512):
                        pp = pps[hf % 2]
                        for k in range(KM):
                            S.op("pe", lambda e: e.matmul(pp[:], mt[:, k, :], wo[:, k, hf * 512:(hf + 1) * 512], start=(k == 0), stop=(k == KM - 1)), r=[mtg, wo], w=[pp], sig=(k == KM - 1))
                        S.op("dve", lambda e: e.tensor_tensor(out=xo[:, hf * 512:(hf + 1) * 512], in0=pp[:], in1=g1[v][:, hf * 512:(hf + 1) * 512], op=ALU.mult), r=[pp, g1[v]], w=[(xo, hf)])
                    S.op("dve", lambda e: e.tensor_tensor(out=xo[:], in0=xo[:], in1=xt[:], op=ALU.add), r=[xo, xt], w=[xo])
                    S.dma("sp", x_d[ti * P:(ti + 1) * P, cs * OC:(cs + 1) * OC], xo[:], r=[xo], w_=[("x_d", ti)])
            S.pop()

        def phase_topk(upd):
            S.push()
            sets = [(0, NTL, capL, 0)] + ([(NTL, NT, capC, capL)] if upd else [])
            zt = S.sb("zt", [P, D], F32)
            S.op("pool", lambda e: e.memset(zt[:], 0.0), w=[zt])
            for ti in range(NT + 1):
                S.dma("sp" if ti % 2 == 0 else "act", M_d[ti * P:(ti + 1) * P, :], zt[:], r=[zt], w_=["M_d"])
            ztb = S.sb("ztb", [P, D], BF16)
            S.op("pool", lambda e: e.memset(ztb[:], 0.0), w=[ztb])
            S.dma("sp", htm_d[NTOK:NTOK + P, :], ztb[:], r=[ztb], w_=["htm_d"])
            lo = S.sb("lo", [P, E], F32); hi = S.sb("hi", [P, E], F32); mid = S.sb("mid", [P, E], F32)
            cnp = S.sb("cnp", [P, E], F32); ge = S.sb("ge", [P, E], F32); d1 = S.sb("d1", [P, E], F32); d2 = S.sb("d2", [P, E], F32)
            cmpj = S.sb("cmpj", [P, NT], F32)
            pq = [S.ps("pq", [P, 512], F32) for _ in range(2)]
            pct = S.ps("pct", [P, E], F32)
            for (a, b, cap, base) in sets:
                S.op("dve", lambda e: e.memset(lo[:], 0.0), w=[lo])
                S.op("dve", lambda e: e.memset(hi[:], 1.0), w=[hi])
                for it in range(30):
                    S.op("dve", lambda e: e.tensor_tensor(out=mid[:], in0=lo[:], in1=hi[:], op=ALU.add), r=[lo, hi], w=[mid])
                    S.op("dve", lambda e: e.tensor_scalar(mid[:], mid[:], 0.5, None, op0=ALU.mult), r=[mid], w=[mid])
                    for ex in range(E):
                        S.op("dve", lambda e: e.tensor_scalar(cmpj[:, a:b], aff_e[:, ex, a:b], mid[:, ex:ex + 1], 0.0, op0=ALU.is_ge, op1=ALU.add, accum_out=cnp[:, ex:ex + 1]),
                             r=[aff_e, mid], w=[cmpj, (cnp, ex)])
                    S.op("pe", lambda e: e.matmul(pct[:], ones_f[:], cnp[:], start=True, stop=True), r=[ones_f, cnp], w=[pct])
                    S.op("dve", lambda e: e.tensor_scalar(ge[:], pct[:], float(cap) - 0.5, None, op0=ALU.is_ge), r=[pct], w=[ge])
                    S.op("dve", lambda e: e.tensor_tensor(out=d1[:], in0=mid[:], in1=lo[:], op=ALU.subtract), r=[mid, lo], w=[d1])
                    S.op("dve", lambda e: e.tensor_tensor(out=d2[:], in0=hi[:], in1=mid[:], op=ALU.subtract), r=[mid, hi], w=[d2])
                    S.op("dve", lambda e: e.tensor_tensor(out=d1[:], in0=d1[:], in1=ge[:], op=ALU.mult), r=[d1, ge], w=[d1])
                    S.op("dve", lambda e: e.tensor_tensor(out=d2[:], in0=d2[:], in1=ge[:], op=ALU.mult), r=[d2, ge], w=[d2])
                    S.op("dve", lambda e: e.tensor_tensor(out=lo[:], in0=lo[:], in1=d1[:], op=ALU.add), r=[lo, d1], w=[lo])
                    S.op("dve", lambda e: e.tensor_tensor(out=hi[:], in0=mid[:], in1=d2[:], op=ALU.add), r=[mid, d2], w=[hi])
                for ex in range(E):
                    S.op("dve", lambda e: e.tensor_scalar(msk[:, ex, a:b], aff_e[:, ex, a:b], lo[:, ex:ex + 1], None, op0=ALU.is_ge), r=[aff_e, lo], w=[(msk, (ex, a))])
            S.op("dve", lambda e: e.tensor_tensor(out=gate[:], in0=msk[:], in1=aff_e[:], op=ALU.mult), r=[msk, aff_e], w=[gate])
            mflat = msk[:].rearrange("p e t -> p (e t)")
            pw = S.sb("pw", [P, E, NT], F32); tot = S.sb("tot", [P, E, NT], F32); tb2 = S.sb("tb2", [P, E, NT], F32)
            pwf = pw[:].rearrange("p e t -> p (e t)"); totf = tot[:].rearrange("p e t -> p (e t)")
            n = E * NT
            for c0 in range(0, n, 512):
                m = min(512, n - c0)
                S.op("pe", lambda e: e.matmul(pq[0][:, :m], ustr[:], mflat[:, c0:c0 + m], start=True, stop=True), r=[ustr, msk], w=[pq[0]])
                S.op("dve", lambda e: e.tensor_copy(pwf[:, c0:c0 + m], pq[0][:, :m]), r=[pq[0]], w=[pw])
                S.op("pe", lambda e: e.matmul(pq[1][:, :m], ones_f[:], mflat[:, c0:c0 + m], start=True, stop=True), r=[ones_f, msk], w=[pq[1]])
                S.op("dve", lambda e: e.tensor_copy(totf[:, c0:c0 + m], pq[1][:, :m]), r=[pq[1]], w=[tot])
            for (a, b, cap, base) in sets:
                nxt = tb2
                src0 = S.sb("src0", [P, E, NT], F32)
                S.op("dve", lambda e: e.tensor_copy(src0[:, :, a:b], tot[:, :, a:b]), r=[tot], w=[src0])
                cur = src0
                s = 1
                while s < (b - a):
                    S.op("dve", lambda e: e.tensor_tensor(out=nxt[:, :, a + s:b], in0=cur[:, :, a + s:b], in1=cur[:, :, a:b - s], op=ALU.add), r=[cur], w=[nxt])
                    S.op("dve", lambda e: e.tensor_copy(nxt[:, :, a:a + s], cur[:, :, a:a + s]), r=[cur], w=[nxt])
                    cur, nxt = nxt, cur
                    s *= 2
                S.op("dve", lambda e: e.tensor_tensor(out=cur[:, :, a:b], in0=cur[:, :, a:b], in1=tot[:, :, a:b], op=ALU.subtract), r=[cur, tot], w=[cur])
                S.op("dve", lambda e: e.tensor_tensor(out=pw[:, :, a:b], in0=pw[:, :, a:b], in1=cur[:, :, a:b], op=ALU.add), r=[pw, cur], w=[pw])
                if base:
                    S.op("dve", lambda e: e.tensor_scalar(pw[:, :, a:b], pw[:, :, a:b], float(base), None, op0=ALU.add), r=[pw], w=[pw])
            hiT = NT if upd else NTL
            S.op("dve", lambda e: e.scalar_tensor_tensor(out=posm[:, :, 0:hiT], in0=pw[:, :, 0:hiT], scalar=1.0, in1=msk[:, :, 0:hiT], op0=ALU.add, op1=ALU.mult), r=[pw, msk], w=[posm])
            S.op("dve", lambda e: e.tensor_scalar(posm[:, :, 0:hiT], posm[:, :, 0:hiT], -1.0, None, op0=ALU.add), r=[posm], w=[posm])
            S.pop()

        def phase_moe(l, upd):
            S.push()
            nslots = capL + (capC if upd else 0)
            NSB = (nslots + P - 1) // P
            NS = NSB * P
            tiles = list(range(NTL)) + (list(range(NTL, NT)) if upd else [])
            DH = min(2048, D)
            NDH = D // DH
            M2 = M_d.rearrange("t (h d) -> (t h) d", d=DH)
            iot = S.sb("iot", [P, NS], F32)
            S.op("pool", lambda e: e.iota(iot[:], pattern=[[1, NS]], base=0, channel_multiplier=0, allow_small_or_imprecise_dtypes=True), w=[iot])
            pg = S.ps("pg", [P, 512], F32); pu = S.ps("pu", [P, 512], F32)
            py = [S.ps("py", [P, 512], F32) for _ in range(2)]
            ptr = [S.ps("ptr", [P, P], BF16) for _ in range(2)]
            pi = S.ps("pi", [P, 8], F32)
            pis = S.sb("pis", [P, 8], F32)
            pcol = S.sb("pcol", [P, 1], F32); pbase = S.sb("pbase", [P, 1], F32)
            S.op("pe", lambda e: e.matmul(pi[:, 0:1], ustr[:], ones_f[:, 0:1], start=True, stop=True), r=[ustr, ones_f], w=[pi])
            S.op("dve", lambda e: e.tensor_copy(pcol[:], pi[:, 0:1]), r=[pi], w=[pcol])
            S.op("dve", lambda e: e.tensor_scalar(pbase[:], pcol[:], float(NTOK), None, op0=ALU.add), r=[pcol], w=[pbase])
            rv5 = S.sb("rv5", [P, E, NT, 5], BF16)
            r3 = S.sb("r3", [P, NT, 3], F32)
            gtmp = S.sb("gtmp", [P, E, NT], F32)
            for ti in range(NT):
                S.op("pool", lambda e: e.memset(r3[:, ti, 0:1], float(ti)), w=[(r3, (ti, 0))])
                S.op("dve", lambda e: e.tensor_copy(r3[:, ti, 1:2], pcol[:]), r=[pcol], w=[(r3, (ti, 1))])
                S.op("pool", lambda e: e.memset(r3[:, ti, 2:3], 1.0), w=[(r3, (ti, 2))])
            for ex in range(E):
                S.op("dve", lambda e: e.tensor_copy(rv5[:, ex, :, 0:3], r3[:]), r=[r3], w=[(rv5, ("c", ex))])
            S.op("dve", lambda e: e.tensor_copy(rv5[:, :, :, 3], gate[:]), r=[gate], w=[(rv5, "hi")])
            S.op("dve", lambda e: e.tensor_copy(gtmp[:], rv5[:, :, :, 3]), r=[rv5], w=[gtmp])
            S.op("dve", lambda e: e.tensor_tensor(out=gtmp[:], in0=gate[:], in1=gtmp[:], op=ALU.subtract), r=[gate, gtmp], w=[gtmp])
            S.op("dve", lambda e: e.tensor_copy(rv5[:, :, :, 4], gtmp[:]), r=[gtmp], w=[(rv5, "lo")])
            oh = [S.sb("oh", [P, P], BF16) for _ in range(3)]
            sif = S.sb("sif", [P, 1], F32); dif = S.sb("dif", [P, 1], F32)
            sidx = [S.sb("sidx", [P, 1], I32) for _ in range(2)]
            si2 = [[S.sb("si2", [P, 1], I32) for _ in range(NDH)] for _ in range(NSB)]
            gs = [S.sb("gs", [P, 1], F32) for _ in range(NSB)]
            hg = [S.sb("hg", [P, D], BF16) for _ in range(1)]
            hsT = S.sb("hsT", [P, KD, NS], BF16)
            FH = 128
            SZ = KD * 2 * FH
            wbuf = S.sb("wbuf", [P, max(2 * SZ, FC * DH)], BF16)
            wgus = [wbuf[:, sl * SZ:(sl + 1) * SZ].rearrange("p (k w f) -> p k w f", k=KD, w=2) for sl in range(2)]
            wd = wbuf[:, 0:FC * DH].rearrange("p (f d) -> p f d", f=FC)
            wd_slots = lambda f: list(range((f * DH) // SZ, min(1, ((f + 1) * DH - 1) // SZ) + 1))
            KS = min(4, KD)
            stg = [S.sb("stgm", [P, KS, FH], F32) for _ in range(4)]
            gc = 0
            aTt = S.sb("aTt", [P, FC, NS], BF16)
            sgt = S.sb("sgt", [P, 512], F32)
            DS = min(1024, DH)
            stgd = [S.sb("stgd", [P, DS], F32) for _ in range(2)]
            xg = [S.sb("xg", [P, DH], F32) for _ in range(2)]
            nst = 0; nxg = 0
            for ex in range(E):
                for sb in range(NSB):
                    for i, ti in enumerate(tiles):
                        o = oh[i % 3]
                        S.op("dve", lambda e: e.tensor_scalar(o[:], iot[:, sb * P:(sb + 1) * P], posm[:, ex, ti:ti + 1], None, op0=ALU.is_equal), r=[iot, posm], w=[o])
                        S.op("pe", lambda e: e.matmul(pi[:, 0:5], o[:], rv5[:, ex, ti, :], start=(i == 0), stop=(i == len(tiles) - 1)), r=[o, rv5], w=[pi])
                    si = sidx[sb % 2]; hgt = hg[0]
                    S.op("dve", lambda e: e.tensor_copy(pis[:, 0:5], pi[:, 0:5]), r=[pi], w=[pis])
                    S.op("dve", lambda e: e.scalar_tensor_tensor(out=sif[:], in0=pis[:, 0:1], scalar=128.0, in1=pis[:, 1:2], op0=ALU.mult, op1=ALU.add), r=[pis], w=[sif])
                    S.op("dve", lambda e: e.tensor_tensor(out=dif[:], in0=sif[:], in1=pbase[:], op=ALU.subtract), r=[sif, pbase], w=[dif])
                    S.op("dve", lambda e: e.scalar_tensor_tensor(out=sif[:], in0=dif[:], scalar=pis[:, 2:3], in1=pbase[:], op0=ALU.mult, op1=ALU.add), r=[dif, pis, pbase], w=[sif])
                    S.op("dve", lambda e: e.tensor_copy(si[:], sif[:]), r=[sif], w=[si])
                    for dh in range(NDH):
                        S.op("dve", lambda e: e.tensor_scalar(si2[sb][dh][:], sif[:], float(NDH), float(dh), op0=ALU.mult, op1=ALU.add), r=[sif], w=[si2[sb][dh]])
                    S.op("dve", lambda e: e.tensor_tensor(out=gs[sb][:], in0=pis[:, 3:4], in1=pis[:, 4:5], op=ALU.add), r=[pis], w=[gs[sb]])
                    S.dma_custom("pool", lambda e: e.indirect_dma_start(out=hgt[:], out_offset=None, in_=htm_d, in_offset=bass.IndirectOffsetOnAxis(ap=si[:, 0:1], axis=0)),
                                 r=[si, "htm_d"], w_=[hgt])
                    for k in range(KD):
                        pt = ptr[k % 2]
                        S.op("pe", lambda e: e.transpose(pt[:], hgt[:, k * P:(k + 1) * P], ident_b[:]), r=[hgt, ident_b], w=[pt])
                        if k % 2 == 0:
                            S.op("act", lambda e: e.activation(hsT[:, k, sb * P:(sb + 1) * P], pt[:], AF.Copy), r=[pt], w=[(hsT, (k, sb))])
                        else:
                            S.op("dve", lambda e: e.tensor_copy(hsT[:, k, sb * P:(sb + 1) * P], pt[:]), r=[pt], w=[(hsT, (k, sb))])
                for fc in range(FF // FH):
                    sl = gc % 2; gc += 1
                    wgu = wgus[sl]
                    for which, wsrc in ((0, w_gate), (1, w_up)):
                        for kk in range(0, KD, KS):
                            st = stg[nst % 4]; nst += 1
                            S.dma("sp", st[:], wsrc[l, ex][kk * P:(kk + KS) * P, fc * FH:(fc + 1) * FH].rearrange("(k p) c -> p k c", p=P), w_=[st])
                            S.op("pool", lambda e: e.tensor_copy(wgu[:, kk:kk + KS, which, :], st[:]), r=[st], w=[(wbuf, sl)])
                    for s0 in range(0, NS, 512):
                        ns = min(512, NS - s0)
                        for k in range(KD):
                            S.op("pe", lambda e: e.matmul(pg[:, :ns], wgu[:, k, 0, :], hsT[:, k, s0:s0 + ns], start=(k == 0), stop=(k == KD - 1)), r=[(wbuf, sl), hsT], w=[pg], sig=(k == KD - 1))
                        for k in range(KD):
                            S.op("pe", lambda e: e.matmul(pu[:, :ns], wgu[:, k, 1, :], hsT[:, k, s0:s0 + ns], start=(k == 0), stop=(k == KD - 1)), r=[(wbuf, sl), hsT], w=[pu], sig=(k == KD - 1))
                        S.op("act", lambda e: e.activation(sgt[:, :ns], pg[:, :ns], AF.Silu), r=[pg], w=[sgt])
                        S.op("dve", lambda e: e.tensor_tensor(out=aTt[:, fc, s0:s0 + ns], in0=sgt[:, :ns], in1=pu[:, :ns], op=ALU.mult), r=[sgt, pu], w=[(aTt, (fc, s0))])
                for dh in range(NDH):
                    for f in range(FC):
                        for d0 in range(0, DH, DS):
                            st = stgd[nst % 2]; nst += 1
                            S.dma("sp", st[:], w_down[l, ex][f * P:(f + 1) * P, dh * DH + d0: dh * DH + d0 + DS], w_=[st])
                            S.op("pool", lambda e: e.tensor_copy(wd[:, f, d0:d0 + DS], st[:]), r=[st], w=[(wbuf, sl_) for sl_ in wd_slots(f)])
                    prev_keys = [("M_d", (dh, (ex - 1) % 2, sbp)) for sbp in range(NSB)]

                    def gather_x(sb):
                        x_ = xg[sb % 2]; ixt = si2[sb][dh]
                        S.dma_custom("pool", lambda e: e.indirect_dma_start(out=x_[:], out_offset=None, in_=M2, in_offset=bass.IndirectOffsetOnAxis(ap=ixt[:, 0:1], axis=0)),
                                     r=[ixt] + prev_keys, w_=[x_])

                    gather_x(0)
                    for sb in range(NSB):
                        if sb + 1 < NSB:
                            gather_x(sb + 1)
                        x_ = xg[sb % 2]; ixt = si2[sb][dh]
                        for cb in range(DH // 512):
                            pp = py[cb % 2]
                            for f in range(FC):
                                S.op("pe", lambda e: e.matmul(pp[:], aTt[:, f, sb * P:(sb + 1) * P], wd[:, f, cb * 512:(cb + 1) * 512], start=(f == 0), stop=(f == FC - 1)), r=[aTt, (wbuf, 0), (wbuf, 1)], w=[pp], sig=(f == FC - 1))
                            S.op("dve", lambda e: e.scalar_tensor_tensor(out=x_[:, cb * 512:(cb + 1) * 512], in0=pp[:], scalar=gs[sb][:, 0:1], in1=x_[:, cb * 512:(cb + 1) * 512], op0=ALU.mult, op1=ALU.add),
                                 r=[pp, gs[sb], x_], w=[x_])
                        S.dma_custom("pool", lambda e: e.indirect_dma_start(out=M2, out_offset=bass.IndirectOffsetOnAxis(ap=ixt[:, 0:1], axis=0), in_=x_[:], in_offset=None),
                                     r=[ixt, x_], w_=[("M_d", (dh, ex % 2, sb))])
            S.pop()

        def phase_combine(l, upd, last):
            S.push()
            tiles = list(range(NTL)) + (list(range(NTL, NT)) if upd else [])
            g2 = {v: S.sb("g2", [P, D], F32) for v in ((0, 1) if upd else (0,))}
            for v in g2:
                S.dma("sp", g2[v][:], bc_row(mods_v[v, 5], D), r=["mods_d"], w_=[g2[v]])
            xts = [S.sb("xtc", [P, D], F32) for _ in range(2)]
            mts = [S.sb("mtc", [P, D], F32) for _ in range(2)]
            for i, ti in enumerate(tiles):
                v = 0 if ti < NTL else 1
                xt = xts[i % 2]; mt = mts[i % 2]
                S.dma("sp", xt[:], x_d[ti * P:(ti + 1) * P, :], r=[("x_d", ti)], w_=[xt])
                S.dma("act", mt[:], M_d[ti * P:(ti + 1) * P, :], r=["M_d"], w_=[mt])
                S.op("dve", lambda e: e.tensor_tensor(out=mt[:], in0=mt[:], in1=g2[v][:], op=ALU.mult), r=[mt, g2[v]], w=[mt])
                S.op("pool", lambda e: e.tensor_tensor(out=mt[:], in0=mt[:], in1=xt[:], op=ALU.add), r=[mt, xt], w=[mt])
                if last:
                    S.dma("act", out[ti * P:(ti + 1) * P, :], mt[:], r=[mt], w_=[("out", ti)], out_dram=True)
                else:
                    S.dma("act", x_d[ti * P:(ti + 1) * P, :], mt[:], r=[mt], w_=[("x_d", ti)])
            S.pop()

        all_tiles = list(range(NT))
        lat_tiles = list(range(NTL))
        stop_after = cfg.get("stop_after")
        plist = []
        for l in range(DEPTH):
            upd = l < DEPTH - 1
            plist += [lambda l=l: phase_ada(l),
                      lambda l=l: phase_norm(l, all_tiles, norm1, 0, 1, router=False),
                      lambda l=l: phase_inproj(l),
                      lambda l=l, upd=upd: phase_attn(l, upd),
                      lambda l=l, upd=upd: phase_conv_sg(l, upd),
                      lambda l=l, upd=upd: phase_outproj(l, upd),
                      lambda l=l, upd=upd: phase_norm(l, all_tiles if upd else lat_tiles, norm2, 3, 4, router=True),
                      lambda l=l, upd=upd: phase_topk(upd),
                      lambda l=l, upd=upd: phase_moe(l, upd),
                      lambda l=l, upd=upd: phase_combine(l, upd, last=(l == DEPTH - 1))]
        import os as _os
        nstop = int(_os.environ.get("MK_STOP", "1000"))
        for i, ph in enumerate(plist):
            if i >= nstop:
                break
            ph()
        S.finish()
    cfg["ninst"] = S.ninst; cfg["nwait"] = S.nwait
    return nc


def host_prep(cfg, inputs, b):
    D, SEQ, CTX, E, FF, DEPTH, H, NG, KD = (cfg[k] for k in ("D", "SEQ", "CTX", "E", "FF", "DEPTH", "H", "NG", "KD"))
    f = lambda a: np.ascontiguousarray(np.asarray(a, dtype=np.float32))
    NAW = H * 128; CW = NG * 128
    m = {}
    m["x"] = f(inputs["x"][b]); m["ctx"] = f(inputs["ctx"][b])
    m["cvec"] = f(np.stack([np.asarray(inputs["c"])[b], np.asarray(inputs["c_ctx"])], 0).reshape(2 * KD, 128))
    m["w_ada"] = f(inputs["w_ada"]); m["b_ada"] = f(np.asarray(inputs["b_ada"]).reshape(DEPTH, 1, 6 * D))
    m["norm1"] = f(inputs["norm1"]); m["norm2"] = f(inputs["norm2"])
    w_in = np.asarray(inputs["w_in"])
    cols = []
    for g in range(NG):
        for base in (0, NAW):
            for hh in (2 * g, 2 * g + 1):
                cols.append(np.arange(base + hh * 128, base + (hh + 1) * 128))
        o = 3 * NAW
        for j in range(3):
            cols.append(np.arange(o + j * CW + g * 128, o + j * CW + (g + 1) * 128))
        o2 = 3 * NAW + 3 * CW
        cols.append(np.arange(o2 + g * 128, o2 + (g + 1) * 128))
        for hh in (2 * g, 2 * g + 1):
            cols.append(np.arange(2 * NAW + hh * 128, 2 * NAW + (hh + 1) * 128))
        cols.append(np.arange(o2 + CW + g * 128, o2 + CW + (g + 1) * 128))
    cols = np.concatenate(cols)
    m["w_in"] = f(w_in[:, :, cols])
    m["q_norm"] = f(inputs["q_norm"]); m["k_norm"] = f(inputs["k_norm"])
    rpb = np.asarray(inputs["rpb"])
    ck = np.arange(64)[:, None]; cq = np.arange(64)[None, :]
    dc = np.clip(ck - cq + 15, 0, 30)
    t = rpb[:, :, :, dc]
    top = t.transpose(0, 1, 3, 2, 4)
    bot = np.concatenate([top[:, :, :, 1:, :], top[:, :, :, 14:15, :]], axis=3)
    m["rpbT"] = f(np.concatenate([top, bot], axis=2).reshape(DEPTH, H, 128, 15 * 64))
    cs = np.clip(np.arange(64) - 8, 0, 48)
    valid = (ck >= cs[None, :]) & (ck < cs[None, :] + 16)
    mk = np.where(valid, 0.0, -30000.0).astype(np.float32)
    mk = np.broadcast_to(np.concatenate([mk, mk], 0)[:, None, :], (128, 15, 64))
    m["maskT"] = f(mk.reshape(128, 15 * 64))
    cwv = np.asarray(inputs["conv_w"])
    m["conv_w"] = f(cwv.transpose(0, 2, 1).reshape(DEPTH, NG, 128, 3))
    m["sg_norm"] = f(inputs["sg_norm"])
    sgw = np.asarray(inputs["sg_w"])
    m["sg_wT"] = f(sgw.transpose(0, 3, 1, 2).reshape(DEPTH, 128, NG * 128))
    m["sg_b"] = f(np.asarray(inputs["sg_b"]).reshape(DEPTH, NG * 128))
    m["w_out"] = f(inputs["w_out"]); m["w_router"] = f(inputs["w_router"])
    m["w_gate"] = f(inputs["w_gate"]); m["w_up"] = f(inputs["w_up"]); m["w_down"] = f(inputs["w_down"])
    return m


_CACHE = {}


def kernel(**inputs):
    x = np.asarray(inputs["x"])
    B, SEQ, D = x.shape
    cfg = make_cfg(D=D, SEQ=SEQ, CTX=np.asarray(inputs["ctx"]).shape[1], E=np.asarray(inputs["w_router"]).shape[-1],
                   FF=np.asarray(inputs["w_gate"]).shape[-1], DEPTH=np.asarray(inputs["w_ada"]).shape[0])
    key = tuple(sorted((k, v) for k, v in cfg.items() if isinstance(v, int)))
    if key not in _CACHE:
        _CACHE[key] = build(cfg)
    nc = _CACHE[key]
    in_maps = [host_prep(cfg, inputs, b) for b in range(B)]
    res = run_bass_kernel_spmd(nc, in_maps, core_ids=list(range(B)))
    return np.stack([res.results[b]["out"] for b in range(B)], 0).astype(np.float32)
```

```python
import numpy as np
from contextlib import ExitStack, contextmanager
import concourse.bass as bass
import concourse.mybir as mybir
from concourse.bass_utils import run_bass_kernel_spmd

F32 = mybir.dt.float32
BF16 = mybir.dt.bfloat16
I32 = mybir.dt.int32
AF = mybir.ActivationFunctionType
ALU = mybir.AluOpType
AX = mybir.AxisListType

SEM_WRAP = 1 << 30


class Sync:
    def __init__(self, nc):
        self.nc = nc
        self.stack = ExitStack()
        self.eng = {"pe": nc.tensor, "act": nc.scalar, "dve": nc.vector, "pool": nc.gpsimd, "sp": nc.sync}
        self.cnt = {}
        self.esem = {}
        self.seen = {e: {} for e in self.eng}
        self.rec = {}
        self.dsem = {}
        self.semname = {}
        self.out_tokens = []
        self.nwait = 0
        self.ninst = 0

    @contextmanager
    def ctx(self):
        with self.stack:
            for e in ("pe", "act", "dve", "pool"):
                self.esem[e] = self.stack.enter_context(self.nc.semaphore(f"c_{e}"))
                self.cnt[e] = 0
            yield self

    def sb(self, name, shape, dt):
        return self.stack.enter_context(self.nc.sbuf_tensor(name, list(shape), dt))

    def ps(self, name, shape, dt):
        return self.stack.enter_context(self.nc.psum_tensor(name, list(shape), dt))

    @staticmethod
    def _split(k):
        if isinstance(k, tuple):
            return (k[0] if isinstance(k[0], str) else id(k[0])), k[1]
        return (k if isinstance(k, str) else id(k)), None

    def _recs(self, k, create):
        oid, sub = self._split(k)
        d = self.rec.setdefault(oid, {})
        if sub is None:
            if create and None not in d:
                d[None] = [None, []]
            return list(d.values()) if not create else list(d.values())
        out = []
        if None in d:
            out.append(d[None])
        if sub not in d and create:
            d[sub] = [None, []]
        if sub in d:
            out.append(d[sub])
        return out

    def _tok(self, t):
        sem, val, isdma, key = t
        if isdma and key in self.dsem and self.dsem[key][0] is sem:
            val = max(val, self.dsem[key][1])
        return sem, val

    def _deps(self, r, w):
        deps = {}
        def add(t):
            if t is None:
                return
            sem, val = self._tok(t)
            sid = id(sem)
            if sid not in deps or deps[sid][1] < val:
                deps[sid] = (sem, val)
        for k in r:
            for rc in self._recs(k, False):
                add(rc[0])
        for k in w:
            for rc in self._recs(k, False):
                add(rc[0])
                for t in rc[1]:
                    add(t)
        return deps

    def _commit(self, r, w, tok):
        for k in w:
            oid, sub = self._split(k)
            d = self.rec.setdefault(oid, {})
            if sub is None:
                d.clear()
                d[None] = [tok, []]
            else:
                d[sub] = [tok, []]
        for k in r:
            oid, sub = self._split(k)
            d = self.rec.setdefault(oid, {})
            if sub not in d:
                d[sub] = [None, []]
            rd = d[sub][1]
            rd[:] = [t for t in rd if t[0] is not tok[0]]
            rd.append(tok)

    def _emit_waits(self, e, deps, skip_sem=None):
        eng = self.eng[e]
        for sid, (sem, val) in deps.items():
            if skip_sem is not None and sem is skip_sem:
                continue
            if self.seen[e].get(sid, 0) >= val:
                continue
            eng.wait_ge(sem, val)
            self.nwait += 1
            self.seen[e][sid] = val

    def op(self, e, fn, r=(), w=(), acc=False, sig=True):
        deps = self._deps(r, w)
        self._emit_waits(e, deps, skip_sem=self.esem["pe"] if e == "pe" else None)
        ins = fn(self.eng[e])
        self.ninst += 1
        if sig:
            self.cnt[e] += 1
            ins.then_inc(self.esem[e], 1)
            tok = (self.esem[e], self.cnt[e], False, None)
        else:
            tok = (self.esem[e], self.cnt[e] + 1, False, None)
        self._commit(r, w, tok)
        return ins

    def dma(self, q, out, in_, r=(), w_=(), sem_key=None, out_dram=False, **kw):
        if sem_key is None:
            ks = list(w_) + list(r)
            nk = [k for k in ks if not isinstance(k[0] if isinstance(k, tuple) else k, str)]
            k0 = (nk or ks)[0]
            sem_key = self._split(k0)[0]
        self._get_dsem(sem_key)
        deps = self._deps(r, w_)
        self._emit_waits(q, deps)
        ent = self.dsem[sem_key]
        ent[1] += 16
        ins = self.eng[q].dma_start(out=out, in_=in_, **kw)
        ins.then_inc(ent[0], 16)
        self.ninst += 1
        tok = (ent[0], ent[1], True, sem_key)
        self._commit(r, w_, tok)
        if out_dram:
            self.out_tokens.append(tok)
        return ins

    def _get_dsem(self, sem_key):
        if sem_key not in self.dsem:
            fl = self.__dict__.setdefault("free_sems", [])
            if fl:
                sem, cntv = fl.pop()
                self.dsem[sem_key] = [sem, cntv]
            else:
                self.nsem = getattr(self, "nsem", 0) + 1
                sem = self.stack.enter_context(self.nc.semaphore(f"d{self.nsem}"))
                self.dsem[sem_key] = [sem, 0]

    def finish(self):
        deps = {}
        for t in self.out_tokens:
            sem, val = self._tok(t)
            sid = id(sem)
            if sid not in deps or deps[sid][1] < val:
                deps[sid] = (sem, val)
        for e in ("pe", "act", "dve", "pool"):
            if self.cnt[e] > 0:
                deps[id(self.esem[e])] = (self.esem[e], self.cnt[e])
        for k, (sem, issued) in self.dsem.items():
            if issued > 0:
                deps[id(sem)] = (sem, issued)
        for (sem, issued) in self.__dict__.get("free_sems", []):
            if issued > 0 and id(sem) not in deps:
                deps[id(sem)] = (sem, issued)
        self._emit_waits("sp", deps)


def make_identity(S, ident):
    nc = S.nc
    n = ident.shape[-1]
    def f(e):
        return e.memset(ident[:], 1.0)
    S.op("pool", f, w=[ident])
    def g(e):
        return e.affine_select(out=ident[:], in_=ident[:], pattern=[[1, n]], compare_op=ALU.is_equal,
                               fill=0.0, base=0, channel_multiplier=-1)
    S.op("pool", g, r=[ident], w=[ident])


def _coll(self, kind, op, ins_ap, outs_ap, r=(), w_=(), groups=None, sem_key=None):
    if sem_key is None:
        sem_key = ("coll", len(self.dsem))
    if sem_key not in self.dsem:
        sem = self.stack.enter_context(self.nc.semaphore(f"d{len(self.dsem)}"))
        self.dsem[sem_key] = [sem, 0]
    deps = self._deps(r, w_)
    self._emit_waits("pool", deps)
    ent = self.dsem[sem_key]
    ent[1] += 16
    ins = self.nc.gpsimd.collective_compute(kind, op, replica_groups=groups, ins=[ins_ap], outs=[outs_ap])
    ins.then_inc(ent[0], 16)
    tok = (ent[0], ent[1], True, sem_key)
    self._commit(r, w_, tok)
    return ins

Sync.coll = _coll


def _dma_custom(self, q, fn, r=(), w_=(), sem_key=None, out_dram=False):
    if sem_key is None:
        ks = list(w_) + list(r)
        nk = [k for k in ks if not isinstance(k[0] if isinstance(k, tuple) else k, str)]
        k0 = (nk or ks)[0]
        sem_key = self._split(k0)[0]
    self._get_dsem(sem_key)
    deps = self._deps(r, w_)
    self._emit_waits(q, deps)
    ent = self.dsem[sem_key]
    ent[1] += 16
    ins = fn(self.eng[q])
    ins.then_inc(ent[0], 16)
    self.ninst += 1
    tok = (ent[0], ent[1], True, sem_key)
    self._commit(r, w_, tok)
    if out_dram:
        self.out_tokens.append(tok)
    return ins

Sync.dma_custom = _dma_custom


U32 = mybir.dt.uint32
EPS = 1e-6


def _scope_push(self):
    st = ExitStack()
    self.tstacks.append(st)


def _scope_pop(self):
    self.barrier()
    self.tstacks.pop().close()
    fl = self.__dict__.setdefault("free_sems", [])
    for k, (sem, issued) in self.dsem.items():
        fl.append((sem, issued))
    self.dsem.clear()
    self.rec.clear()


def _barrier(self):
    deps = {}
    for e in ("pe", "act", "dve", "pool"):
        if self.cnt[e] > 0:
            deps[id(self.esem[e])] = (self.esem[e], self.cnt[e])
    for k, (sem, issued) in self.dsem.items():
        if issued > 0:
            deps[id(sem)] = (sem, issued)
    for e in ("pe", "act", "dve", "pool", "sp"):
        self._emit_waits(e, dict(deps))


def _sb2(self, name, shape, dt):
    self.uid = getattr(self, "uid", 0) + 1
    st = self.tstacks[-1] if getattr(self, "tstacks", None) else self.stack
    return st.enter_context(self.nc.sbuf_tensor(f"{name}_{self.uid}", list(shape), dt))


def _ps2(self, name, shape, dt):
    self.uid = getattr(self, "uid", 0) + 1
    st = self.tstacks[-1] if getattr(self, "tstacks", None) else self.stack
    return st.enter_context(self.nc.psum_tensor(f"{name}_{self.uid}", list(shape), dt))


Sync.push = _scope_push
Sync.pop = _scope_pop
Sync.barrier = _barrier
Sync.sb = _sb2
Sync.ps = _ps2


def make_cfg(D=4096, SEQ=8192, CTX=256, E=16, FF=1024, DEPTH=2):
    c = dict(D=D, SEQ=SEQ, CTX=CTX, E=E, FF=FF, DEPTH=DEPTH)
    c["H"] = (D // 2) // 128
    c["NG"] = c["H"] // 2
    c["CW"] = D // 4
    c["SGW"] = D // 4
    assert c["CW"] == c["NG"] * 128
    c["KD"] = D // 128
    c["NTL"] = SEQ // 128
    c["NTC"] = CTX // 128
    c["NT"] = c["NTL"] + c["NTC"]
    c["NTOK"] = SEQ + CTX
    c["ROWS"] = SEQ // 64
    c["capL"] = 2 * SEQ // E
    c["capC"] = 2 * CTX // E
    c["SLABC"] = 11 * 128
    return c


def build(cfg):
    D, SEQ, CTX, E, FF, DEPTH = (cfg[k] for k in ("D", "SEQ", "CTX", "E", "FF", "DEPTH"))
    H, NG, CW, SGW, KD, NTL, NTC, NT, NTOK, ROWS = (cfg[k] for k in ("H", "NG", "CW", "SGW", "KD", "NTL", "NTC", "NT", "NTOK", "ROWS"))
    capL, capC, SLABC = cfg["capL"], cfg["capC"], cfg["SLABC"]
    KM = KD
    FC = FF // 128
    P = 128
    nc = bass.Bass("TRN2", target_bir_lowering=False)

    def din(name, shape, dt=F32):
        return nc.dram_tensor(name, list(shape), dt, kind="ExternalInput").ap()

    x_in = din("x", [SEQ, D]); ctx_in = din("ctx", [CTX, D]); cvec = din("cvec", [2 * KD, 128])
    w_ada = din("w_ada", [DEPTH, D, 6 * D]); b_ada = din("b_ada", [DEPTH, 1, 6 * D])
    norm1 = din("norm1", [DEPTH, D]); norm2 = din("norm2", [DEPTH, D])
    w_in = din("w_in", [DEPTH, D, NG * SLABC])
    q_norm = din("q_norm", [DEPTH, 128]); k_norm = din("k_norm", [DEPTH, 128])
    rpbT = din("rpbT", [DEPTH, H, 128, 15 * 64]); maskT = din("maskT", [128, 15 * 64])
    conv_w = din("conv_w", [DEPTH, NG, 128, 3])
    sg_norm = din("sg_norm", [DEPTH, SGW]); sg_wT = din("sg_wT", [DEPTH, 128, NG * 128]); sg_b = din("sg_b", [DEPTH, NG * 128])
    w_out = din("w_out", [DEPTH, D, D]); w_router = din("w_router", [DEPTH, D, E])
    w_gate = din("w_gate", [DEPTH, E, D, FF]); w_up = din("w_up", [DEPTH, E, D, FF]); w_down = din("w_down", [DEPTH, E, FF, D])
    out = nc.dram_tensor("out", [SEQ, D], F32, kind="ExternalOutput").ap()

    def dsc(name, shape, dt):
        return nc.dram_tensor(name, list(shape), dt).ap()

    SLOTS_MAX = ((capL + capC + 127) // 128) * 128
    mods_d = dsc("mods_d", [2 * 6 * KD, 128], F32)
    x_d = dsc("x_d", [NTOK, D], F32)
    hT_d = dsc("hT_d", [128, KD, NTOK], BF16)
    htm_d = dsc("htm_d", [NTOK + 128, D], BF16)
    M_d = dsc("M_d", [NTOK + 128, D], F32)
    qT_d = dsc("qT_d", [H, 128, NTOK], BF16); kT_d = dsc("kT_d", [H, 128, NTOK], BF16)
    v_d = dsc("v_d", [NTOK, H * 128], BF16)
    zT_d = dsc("zT_d", [NG, 128, NTOK], BF16); gbT_d = dsc("gbT_d", [NG, 128, NTOK], BF16)
    suT_d = dsc("suT_d", [NG, 128, NTOK], BF16); sv_d = dsc("sv_d", [NTOK, SGW], BF16)
    mT_d = dsc("mT_d", [128, KM, NTOK], BF16)
    Y_d = dsc("Y_d", [E * SLOTS_MAX, D], BF16)

    S = Sync(nc)
    S.tstacks = []
    mods_v = mods_d.rearrange("(v m k) p -> v m (k p)", v=2, m=6)

    def bc_row(ap1d, n):
        return ap1d.rearrange("(o d) -> o d", o=1).to_broadcast([P, n])

    with S.ctx():
        ident_b = S.sb("identb", [P, P], BF16); make_identity(S, ident_b)
        ident_f = S.sb("identf", [P, P], F32); make_identity(S, ident_f)
        ones_b = S.sb("onesb", [P, P], BF16); S.op("pool", lambda e: e.memset(ones_b[:], 1.0), w=[ones_b])
        ones_f = S.sb("onesf", [P, P], F32); S.op("pool", lambda e: e.memset(ones_f[:], 1.0), w=[ones_f])
        epsc = S.sb("epsc", [P, 1], F32); S.op("pool", lambda e: e.memset(epsc[:], EPS), w=[epsc])
        ustr = S.sb("ustr", [P, P], F32); S.op("pool", lambda e: e.memset(ustr[:], 1.0), w=[ustr])
        S.op("pool", lambda e: e.affine_select(out=ustr[:], in_=ustr[:], pattern=[[1, P]], compare_op=ALU.is_ge, fill=0.0,
                                               base=-1, channel_multiplier=-1), r=[ustr], w=[ustr])
        aff_e = S.sb("aff_e", [P, E, NT], F32)
        msk = S.sb("msk", [P, E, NT], F32)
        gate = S.sb("gate", [P, E, NT], F32)
        posm = S.sb("posm", [P, E, NT], F32)

        S.push()
        cp = [S.sb("cp", [P, D], F32) for _ in range(2)]
        for ti in range(NT):
            src = x_in[ti * P:(ti + 1) * P, :] if ti < NTL else ctx_in[(ti - NTL) * P:(ti - NTL + 1) * P, :]
            t = cp[ti % 2]
            S.dma("sp", t[:], src, w_=[t])
            S.dma("act", x_d[ti * P:(ti + 1) * P, :], t[:], r=[t], w_=[("x_d", ti)])
        S.pop()

        def rstd_of(ss, n, tag):
            sd = S.sb("sd" + tag, [P, 1], F32); rs = S.sb("rs" + tag, [P, 1], F32)
            return sd, rs

        def phase_ada(l):
            S.push()
            cv = S.sb("cv", [2 * KD, P], F32)
            S.dma("sp", cv[:], cvec, w_=[cv])
            S.op("act", lambda e: e.activation(cv[:], cv[:], AF.Silu), r=[cv], w=[cv])
            pc = S.ps("pc", [P, 2 * KD], F32)
            S.op("pe", lambda e: e.transpose(pc[:], cv[:], ident_f[:2 * KD, :2 * KD]), r=[cv, ident_f], w=[pc])
            cT = S.sb("cT", [P, KD, 2], F32)
            for v in range(2):
                S.op("dve", lambda e: e.tensor_copy(cT[:, :, v], pc[:, v * KD:(v + 1) * KD]), r=[pc], w=[(cT, v)])
            brow = S.sb("brow", [1, 6 * D], F32)
            S.dma("sp", brow[:], b_ada[l], w_=[brow])
            modsT = S.sb("modsT", [P, 2, 6 * KD], F32)
            CG = 256
            wa = [S.sb("wa", [P, KD, CG], F32) for _ in range(2)]
            pm = [S.ps("pm", [P, 2], F32) for _ in range(2)]
            for cg in range(6 * D // CG):
                w = wa[cg % 2]
                S.dma("sp" if cg % 2 == 0 else "act", w[:], w_ada[l][:, cg * CG:(cg + 1) * CG].rearrange("(k p) c -> p k c", p=P), w_=[w])
                for sub in range(CG // P):
                    j = cg * (CG // P) + sub
                    pp = pm[j % 2]
                    for k in range(KD):
                        S.op("pe", lambda e: e.matmul(pp[:], w[:, k, sub * P:(sub + 1) * P], cT[:, k, :], start=(k == 0), stop=False),
                             r=[w, cT], w=[pp], sig=False)
                    S.op("pe", lambda e: e.matmul(pp[:], brow[0:1, j * P:(j + 1) * P], ones_f[0:1, 0:2], start=False, stop=True),
                         r=[brow, ones_f], w=[pp])
                    S.op("dve", lambda e: e.tensor_copy(modsT[:, :, j], pp[:]), r=[pp], w=[(modsT, j)])
            mflat = modsT[:].rearrange("p v j -> p (v j)")
            nrow = 2 * 6 * KD
            for r0 in range(0, nrow, P):
                n = min(P, nrow - r0)
                pt = S.ps("ptm", [P, P], F32)
                S.op("pe", lambda e: e.transpose(pt[:n, :], mflat[:, r0:r0 + n], ident_f[:]), r=[modsT, ident_f], w=[pt])
                mt = S.sb("mtm", [P, P], F32)
                S.op("dve", lambda e: e.tensor_copy(mt[:n, :], pt[:n, :]), r=[pt], w=[mt])
                S.dma("sp", mods_d[r0:r0 + n, :], mt[:n, :], r=[mt], w_=["mods_d"])
            S.pop()

        def phase_norm(l, tiles, normw, m_sh, m_sc, router):
            S.push()
            gam = {}; bet = {}
            nw = S.sb("nw", [P, D], F32)
            S.dma("sp", nw[:], bc_row(normw[l], D), w_=[nw])
            for v in sorted(set(0 if ti < NTL else 1 for ti in tiles)):
                g = S.sb("gam", [P, D], F32); b = S.sb("bet", [P, D], F32)
                S.dma("sp", g[:], bc_row(mods_v[v, m_sc], D), r=["mods_d"], w_=[g])
                S.dma("act", b[:], bc_row(mods_v[v, m_sh], D), r=["mods_d"], w_=[b])
                S.op("dve", lambda e: e.scalar_tensor_tensor(out=g[:], in0=g[:], scalar=1.0, in1=nw[:], op0=ALU.add, op1=ALU.mult), r=[g, nw], w=[g])
                gam[v] = g; bet[v] = b
            if router:
                wrf = S.sb("wrf", [P, KD, E], F32); wr = S.sb("wr", [P, KD, E], BF16)
                S.dma("sp", wrf[:], w_router[l].rearrange("(k p) e -> p k e", p=P), w_=[wrf])
                S.op("dve", lambda e: e.tensor_copy(wr[:], wrf[:]), r=[wrf], w=[wr])
            xts = [S.sb("xt", [P, D], F32) for _ in range(2)]
            junk = S.sb("junk", [P, D], BF16)
            hbs = [S.sb("hb", [P, D], BF16) for _ in range(2)]
            GN = 2
            hTs = [S.sb("hTt", [P, KD, GN * P], BF16) for _ in range(2)]
            groups = []
            for ti in tiles:
                if groups and len(groups[-1]) < GN and groups[-1][-1] == ti - 1 and (ti < NTL) == (groups[-1][0] < NTL):
                    groups[-1].append(ti)
                else:
                    groups.append([ti])
            gidx = {}
            for gi, grp in enumerate(groups):
                for j, ti in enumerate(grp):
                    gidx[ti] = (gi, j, len(grp))
            pts = [S.ps("ptn", [P, P], BF16) for _ in range(4)]
            plg = S.ps("plg", [P, E], F32)
            ss = S.sb("ss", [P, 1], F32); sd = S.sb("sd", [P, 1], F32); rs = S.sb("rs", [P, 1], F32)
            mx = S.sb("mx", [P, 1], F32); sm = S.sb("sm", [P, 1], F32); ex = S.sb("ex", [P, E], F32)
            for i, ti in enumerate(tiles):
                v = 0 if ti < NTL else 1
                gi, gj, gn = gidx[ti]
                xt = xts[i % 2]; hb = hbs[i % 2]; hTg = hTs[gi % 2]
                hTt = hTg[:, :, gj * P:(gj + 1) * P]
                S.dma("sp", xt[:], x_d[ti * P:(ti + 1) * P, :], r=[("x_d", ti)], w_=[xt])
                S.op("act", lambda e: e.activation(junk[:], xt[:], AF.Square, accum_out=ss[:]), r=[xt], w=[junk, ss])
                S.op("act", lambda e: e.activation(sd[:], ss[:], AF.Sqrt, bias=epsc[:], scale=1.0 / D), r=[ss, epsc], w=[sd])
                S.op("dve", lambda e: e.reciprocal(rs[:], sd[:]), r=[sd], w=[rs])
                S.op("dve", lambda e: e.scalar_tensor_tensor(out=xt[:], in0=xt[:], scalar=rs[:, 0:1], in1=gam[v][:], op0=ALU.mult, op1=ALU.mult),
                     r=[xt, rs, gam[v]], w=[xt])
                S.op("dve", lambda e: e.tensor_tensor(out=hb[:], in0=xt[:], in1=bet[v][:], op=ALU.add), r=[xt, bet[v]], w=[hb])
                if router:
                    S.dma("act", htm_d[ti * P:(ti + 1) * P, :], hb[:], r=[hb], w_=[("htm_d", ti)])
                for k in range(KD):
                    pt = pts[k % 4]
                    S.op("pe", lambda e: e.transpose(pt[:], hb[:, k * P:(k + 1) * P], ident_b[:]), r=[hb, ident_b], w=[pt])
                    if k % 2 == 0:
                        S.op("act", lambda e: e.activation(hTt[:, k, :], pt[:], AF.Copy), r=[pt], w=[(hTg, (gj, k))])
                    else:
                        S.op("dve", lambda e: e.tensor_copy(hTt[:, k, :], pt[:]), r=[pt], w=[(hTg, (gj, k))])
                if gj == gn - 1:
                    t0g = (ti - gn + 1) * P
                    S.dma("sp", hT_d[:, :, t0g:t0g + gn * P], hTg[:, :, 0:gn * P], r=[hTg], w_=[("hT_d", ti)])
                if router:
                    for k in range(KD):
                        S.op("pe", lambda e: e.matmul(plg[:], hTt[:, k, :], wr[:, k, :], start=(k == 0), stop=(k == KD - 1)),
                             r=[hTg, wr], w=[plg], sig=(k == KD - 1))
                    S.op("dve", lambda e: e.tensor_reduce(out=mx[:], in_=plg[:], axis=AX.X, op=ALU.max), r=[plg], w=[mx])
                    S.op("dve", lambda e: e.tensor_scalar(mx[:], mx[:], -1.0, None, op0=ALU.mult), r=[mx], w=[mx])
                    S.op("act", lambda e: e.activation(ex[:], plg[:], AF.Exp, bias=mx[:], scale=1.0, accum_out=sm[:]), r=[plg, mx], w=[ex, sm])
                    S.op("dve", lambda e: e.reciprocal(sm[:], sm[:]), r=[sm], w=[sm])
                    S.op("dve", lambda e: e.tensor_scalar(aff_e[:, :, ti], ex[:], sm[:, 0:1], None, op0=ALU.mult), r=[ex, sm], w=[(aff_e, ti)])
            S.pop()

        def phase_inproj(l):
            S.push()
            qg = S.sb("qg", [P, 1], F32); kg = S.sb("kg", [P, 1], F32)
            S.dma("sp", qg[:], q_norm[l].rearrange("(p o) -> p o", o=1), w_=[qg])
            S.dma("sp", kg[:], k_norm[l].rearrange("(p o) -> p o", o=1), w_=[kg])
            S.op("dve", lambda e: e.tensor_scalar(qg[:], qg[:], float(128 ** -0.5), None, op0=ALU.mult), r=[qg], w=[qg])
            wsl = S.sb("wsl", [P, KD, SLABC], BF16)
            stg = [S.sb("stg", [P, 1, SLABC], F32) for _ in range(2)]
            hTb = [S.sb("hTb", [P, KD, 512], BF16) for _ in range(2)]
            pf = [S.ps("pf", [P, 512], F32) for _ in range(3)]
            pss = S.ps("pss", [P, 512], F32)
            ptm = [S.ps("ptmm", [P, 512], F32) for _ in range(2)]
            qf = S.sb("qf", [P, 512], F32); sq = S.sb("sq", [P, 512], BF16); rq = S.sb("rq", [P, 512], F32)
            ob = [S.sb("ob", [P, 512], BF16) for _ in range(3)]
            xin_sb = S.sb("xin_sb", [P, 512], F32)
            ot = [S.sb("ot", [P, 384], BF16) for _ in range(2)]
            nblk = (NTOK + 511) // 512
            cnt = 0
            for g in range(NG):
                for kk in range(0, KD):
                    st = stg[kk % 2]
                    S.dma("sp" if kk % 2 == 0 else "act", st[:],
                          w_in[l][kk * P:(kk + 1) * P, g * SLABC:(g + 1) * SLABC].rearrange("(k p) c -> p k c", p=P), w_=[st])
                    S.op("pool", lambda e: e.tensor_copy(wsl[:, kk:kk + 1, :], st[:]), r=[st], w=[(wsl, kk)])
                for tb in range(nblk):
                    t0 = tb * 512; nb = min(512, NTOK - t0)
                    hb = hTb[tb % 2]
                    S.dma("sp", hb[:, :, :nb], hT_d[:, :, t0:t0 + nb], r=["hT_d"], w_=[hb])
                    for j in range(8):
                        pp = pf[j % 3]
                        for k in range(KD):
                            S.op("pe", lambda e: e.matmul(pp[:, :nb], wsl[:, k, j * P:(j + 1) * P], hb[:, k, :nb], start=(k == 0), stop=(k == KD - 1)),
                                 r=[wsl, hb], w=[pp], sig=(k == KD - 1))
                        o = ob[cnt % 3]; cnt += 1
                        if j < 4:
                            gn = qg if j < 2 else kg
                            head = 2 * g + (j % 2)
                            dst = (qT_d if j < 2 else kT_d)[head][:, t0:t0 + nb]
                            S.op("act", lambda e: e.activation(qf[:, :nb], pp[:, :nb], AF.Copy), r=[pp], w=[qf])
                            S.op("act", lambda e: e.activation(sq[:, :nb], pp[:, :nb], AF.Square), r=[pp], w=[sq])
                            S.op("pe", lambda e: e.matmul(pss[:, :nb], ones_b[:], sq[:, :nb], start=True, stop=True), r=[ones_b, sq], w=[pss])
                            S.op("act", lambda e: e.activation(rq[:, :nb], pss[:, :nb], AF.Sqrt, bias=epsc[:], scale=1.0 / 128), r=[pss, epsc], w=[rq])
                            S.op("dve", lambda e: e.reciprocal(rq[:, :nb], rq[:, :nb]), r=[rq], w=[rq])
                            S.op("dve", lambda e: e.scalar_tensor_tensor(out=o[:, :nb], in0=qf[:, :nb], scalar=gn[:, 0:1], in1=rq[:, :nb], op0=ALU.mult, op1=ALU.mult),
                                 r=[qf, gn, rq], w=[o])
                            S.dma("act", dst, o[:, :nb], r=[o], w_=[("qT_d" if j < 2 else "kT_d", head)])
                        elif j == 4:
                            S.op("act", lambda e: e.activation(xin_sb[:, :nb], pp[:, :nb], AF.Copy), r=[pp], w=[xin_sb])
                        elif j == 6:
                            S.op("dve", lambda e: e.tensor_tensor(out=o[:, :nb], in0=pp[:, :nb], in1=xin_sb[:, :nb], op=ALU.mult), r=[pp, xin_sb], w=[o])
                            S.dma("act", zT_d[g][:, t0:t0 + nb], o[:, :nb], r=[o], w_=[("zT_d", g)])
                        else:
                            S.op("act", lambda e: e.activation(o[:, :nb], pp[:, :nb], AF.Copy), r=[pp], w=[o])
                            dd = gbT_d if j == 5 else suT_d
                            S.dma("act", dd[g][:, t0:t0 + nb], o[:, :nb], r=[o], w_=[("dd", g)])
                    for sub in range(nb // P):
                        pt = ptm[sub % 2]; o2 = ot[sub % 2]
                        for k in range(KD):
                            S.op("pe", lambda e: e.matmul(pt[:, 0:384], hb[:, k, sub * P:(sub + 1) * P], wsl[:, k, 1024:1408], start=(k == 0), stop=(k == KD - 1)),
                                 r=[hb, wsl], w=[pt], sig=(k == KD - 1))
                        S.op("dve", lambda e: e.tensor_copy(o2[:], pt[:, 0:384]), r=[pt], w=[o2])
                        r0 = t0 + sub * P
                        S.dma("act", v_d[r0:r0 + P, 2 * g * P:(2 * g + 2) * P], o2[:, 0:256], r=[o2], w_=[("v_d", g)])
                        S.dma("act", sv_d[r0:r0 + P, g * P:(g + 1) * P], o2[:, 256:384], r=[o2], w_=[("sv_d", g)])
            S.pop()

        def phase_attn(l, upd):
            S.push()
            NCH = 4 + NTC
            mk_ = S.sb("mk_", [P, 15 * 64], F32)
            S.dma("sp", mk_[:], maskT, w_=[mk_])
            kT = S.sb("kT", [P, NTOK], BF16); qT = S.sb("qT", [P, NTOK], BF16)
            V = S.sb("V", [P, NT, P], BF16); Vs = S.sb("Vs", [P, NTL - 1, P], BF16)
            TB = S.sb("TB", [P, 15, 64], F32)
            aT = S.sb("aT", [P, NTOK], BF16)
            sp_ = [S.ps("sp_", [P, 8, 64], F32) for _ in range(4)]
            popr = [S.ps("popr", [P, 2, P], F32) for _ in range(4)]
            po = [t[:, 0, :] for t in popr]
            pr = [t[:, 1, :] for t in popr]
            pokey = popr
            sbb = [S.sb("sbb", [P, 4, 64], F32) for _ in range(4)]
            pT = [S.sb("pT", [P, NCH, 64], BF16) for _ in range(4)]
            pT2 = S.sb("pT2", [P, NTC, P], BF16)
            rinv = [S.sb("rinv", [P, P], F32) for _ in range(4)]
            TBf = TB[:].rearrange("p a b -> p (a b)")
            for h in range(H):
                S.dma("sp", kT[:], kT_d[h], r=["kT_d"], w_=[kT])
                S.dma("act", qT[:], qT_d[h], r=["qT_d"], w_=[qT])
                S.dma("sp", V[:], v_d[:, h * P:(h + 1) * P].rearrange("(t p) c -> p t c", p=P), r=["v_d"], w_=[V])
                S.dma("act", Vs[:], v_d[64:64 + (NTL - 1) * P, h * P:(h + 1) * P].rearrange("(t p) c -> p t c", p=P), r=["v_d"], w_=[Vs])
                S.dma("sp", TBf, rpbT[l, h], w_=[TB])
                S.op("dve", lambda e: e.tensor_tensor(out=TBf, in0=TBf, in1=mk_[:], op=ALU.add), r=[TB, mk_], w=[TB])
                def stage_a(r):
                    s = min(max(r - 4, 0), ROWS - 8); off = s - r + 7; tok0 = 64 * s
                    ps_ = sp_[r % 4]; pp = pT[r % 4]; sb_ = sbb[r % 4]
                    qs = qT[:, 64 * r:64 * r + 64]
                    for c in range(NCH):
                        ks = kT[:, tok0 + P * c: tok0 + P * (c + 1)] if c < 4 else kT[:, SEQ + P * (c - 4): SEQ + P * (c - 3)]
                        S.op("pe", lambda e: e.matmul(ps_[:, c, :], ks, qs, start=True, stop=True), r=[kT, qT], w=[ps_], sig=(c == NCH - 1))
                    S.op("dve", lambda e: e.tensor_tensor(out=sb_[:], in0=ps_[:, 0:4, :], in1=TB[:, off:off + 8:2, :], op=ALU.add), r=[ps_, TB], w=[sb_])
                    S.op("act", lambda e: e.activation(pp[:, 0:4, :], sb_[:], AF.Exp), r=[sb_], w=[(pp, 0)])
                    S.op("act", lambda e: e.activation(pp[:, 4:, :], ps_[:, 4:NCH, :], AF.Exp), r=[ps_], w=[(pp, 1)])

                def stage_b(r):
                    s = min(max(r - 4, 0), ROWS - 8)
                    pp = pT[r % 4]; pov = po[r % 4]; prv = pr[r % 4]; ri = rinv[r % 4]
                    for c in range(NCH):
                        if c < 4:
                            vt = V[:, s // 2 + c, :] if s % 2 == 0 else Vs[:, (s - 1) // 2 + c, :]
                        else:
                            vt = V[:, NTL + (c - 4), :]
                        S.op("pe", lambda e: e.matmul(pov[:, 0:64], vt, pp[:, c, :], start=(c == 0), stop=(c == NCH - 1)), r=[V, Vs, pp], w=[pov], sig=(c == NCH - 1))
                    for c in range(NCH):
                        S.op("pe", lambda e: e.matmul(prv[:, 0:64], ones_b[:], pp[:, c, :], start=(c == 0), stop=(c == NCH - 1)), r=[ones_b, pp], w=[prv], sig=(c == NCH - 1))
                    S.op("dve", lambda e: e.reciprocal(ri[:, 0:64], prv[:, 0:64]), r=[prv], w=[ri])
                    S.op("dve", lambda e: e.tensor_tensor(out=aT[:, 64 * r:64 * r + 64], in0=pov[:, 0:64], in1=ri[:, 0:64], op=ALU.mult), r=[pov, ri], w=[(aT, r)])

                LAG = 2
                for step in range(ROWS + LAG):
                    if step < ROWS:
                        stage_a(step)
                    if step >= LAG:
                        stage_b(step - LAG)
                nq = SEQ
                if upd:
                    nq = NTOK
                    for qb in range(NTC):
                        ps_ = sp_[qb % 2]; pov = po[qb % 2]; prv = pr[qb % 2]; ri = rinv[qb % 2]
                        psv = ps_[:].rearrange("p a b -> p (a b)")
                        qs = qT[:, SEQ + P * qb: SEQ + P * (qb + 1)]
                        for c in range(NTC):
                            S.op("pe", lambda e: e.matmul(psv[:, c * P:(c + 1) * P], kT[:, SEQ + P * c: SEQ + P * (c + 1)], qs, start=True, stop=True),
                                 r=[kT, qT], w=[ps_], sig=(c == NTC - 1))
                        S.op("act", lambda e: e.activation(pT2[:].rearrange("p a b -> p (a b)"), psv[:, 0:NTC * P], AF.Exp), r=[ps_], w=[pT2])
                        for c in range(NTC):
                            S.op("pe", lambda e: e.matmul(pov[:], V[:, NTL + c, :], pT2[:, c, :], start=(c == 0), stop=(c == NTC - 1)), r=[V, pT2], w=[pov], sig=(c == NTC - 1))
                        for c in range(NTC):
                            S.op("pe", lambda e: e.matmul(prv[:], ones_b[:], pT2[:, c, :], start=(c == 0), stop=(c == NTC - 1)), r=[ones_b, pT2], w=[prv], sig=(c == NTC - 1))
                        S.op("dve", lambda e: e.reciprocal(ri[:], prv[:]), r=[prv], w=[ri])
                        S.op("dve", lambda e: e.tensor_tensor(out=aT[:, SEQ + P * qb: SEQ + P * (qb + 1)], in0=pov[:], in1=ri[:], op=ALU.mult), r=[pov, ri], w=[(aT, 1000 + qb)])
                S.dma("sp", mT_d[:, h, 0:nq], aT[:, 0:nq], r=[aT], w_=[("mT_d", h)])
            S.pop()

        def phase_conv_sg(l, upd):
            S.push()
            z = S.sb("z", [P, NTOK], BF16); gb = S.sb("gb", [P, NTOK], BF16)
            acc = S.sb("acc", [P, NTOK], F32); y = S.sb("y", [P, NTOK], BF16)
            cw = S.sb("cw", [P, 3], F32)
            seqs = [(0, SEQ)] + ([(SEQ, CTX)] if upd else [])
            nq = NTOK if upd else SEQ
            for g in range(NG):
                S.dma("sp", z[:], zT_d[g], r=["zT_d"], w_=[z])
                S.dma("act", gb[:], gbT_d[g], r=["gbT_d"], w_=[gb])
                S.dma("sp", cw[:], conv_w[l, g], w_=[cw])
                for (a, n) in seqs:
                    S.op("dve", lambda e: e.tensor_scalar(acc[:, a:a + n], z[:, a:a + n], cw[:, 1:2], None, op0=ALU.mult), r=[z, cw], w=[acc])
                    S.op("dve", lambda e: e.scalar_tensor_tensor(out=acc[:, a + 1:a + n], in0=z[:, a:a + n - 1], scalar=cw[:, 0:1], in1=acc[:, a + 1:a + n], op0=ALU.mult, op1=ALU.add),
                         r=[z, cw, acc], w=[acc])
                    S.op("dve", lambda e: e.scalar_tensor_tensor(out=acc[:, a:a + n - 1], in0=z[:, a + 1:a + n], scalar=cw[:, 2:3], in1=acc[:, a:a + n - 1], op0=ALU.mult, op1=ALU.add),
                         r=[z, cw, acc], w=[acc])
                    S.op("dve", lambda e: e.tensor_tensor(out=y[:, a:a + n], in0=acc[:, a:a + n], in1=gb[:, a:a + n], op=ALU.mult), r=[acc, gb], w=[y])
                S.dma("sp", mT_d[:, H + g, 0:nq], y[:, 0:nq], r=[y], w_=[("mT_d", H + g)])
            S.pop()
            S.push()
            sgn = S.sb("sgn", [P, SGW], F32)
            S.dma("sp", sgn[:], bc_row(sg_norm[l], SGW), w_=[sgn])
            swf = S.sb("swf", [P, NG * P], F32); sw = S.sb("sw", [P, NG * P], BF16)
            S.dma("sp", swf[:], sg_wT[l], w_=[swf])
            S.op("dve", lambda e: e.tensor_copy(sw[:], swf[:]), r=[swf], w=[sw])
            sgb = S.sb("sgb", [P, NG * P], F32)
            S.dma("sp", sgb[:], bc_row(sg_b[l], NG * P), w_=[sgb])
            svt = [S.sb("svt", [P, SGW], BF16) for _ in range(2)]
            sut = [S.sb("sut", [P, NG, P], BF16) for _ in range(2)]
            vn = [S.sb("vn", [P, SGW], BF16) for _ in range(2)]
            junk = S.sb("junk2", [P, SGW], BF16)
            ss = S.sb("ss2", [P, 1], F32); sd = S.sb("sd2", [P, 1], F32); rs = S.sb("rs2", [P, 1], F32)
            pmx = [S.ps("pmx", [P, P], F32) for _ in range(2)]
            tmp = [S.sb("tmp", [P, P], F32) for _ in range(2)]
            og = [S.sb("og", [P, NG, P], BF16) for _ in range(2)]
            tiles = list(range(NTL)) + (list(range(NTL, NT)) if upd else [])
            for i, ti in enumerate(tiles):
                sv = svt[i % 2]; su = sut[i % 2]; vv = vn[i % 2]; o = og[i % 2]
                S.dma("sp", sv[:], sv_d[ti * P:(ti + 1) * P, :], r=["sv_d"], w_=[sv])
                S.dma("act", su[:], suT_d[:, :, ti * P:(ti + 1) * P].rearrange("g p t -> p g t"), r=["suT_d"], w_=[su])
                S.op("act", lambda e: e.activation(junk[:], sv[:], AF.Square, accum_out=ss[:]), r=[sv], w=[junk, ss])
                S.op("act", lambda e: e.activation(sd[:], ss[:], AF.Sqrt, bias=epsc[:], scale=1.0 / SGW), r=[ss, epsc], w=[sd])
                S.op("dve", lambda e: e.reciprocal(rs[:], sd[:]), r=[sd], w=[rs])
                S.op("dve", lambda e: e.scalar_tensor_tensor(out=vv[:], in0=sv[:], scalar=rs[:, 0:1], in1=sgn[:], op0=ALU.mult, op1=ALU.mult), r=[sv, rs, sgn], w=[vv])
                for g in range(NG):
                    pm = pmx[g % 2]; tp = tmp[g % 2]
                    S.op("pe", lambda e: e.matmul(pm[:], vv[:, g * P:(g + 1) * P], sw[:, g * P:(g + 1) * P], start=True, stop=True), r=[vv, sw], w=[pm])
                    S.op("dve", lambda e: e.tensor_tensor(out=tp[:], in0=pm[:], in1=sgb[:, g * P:(g + 1) * P], op=ALU.add), r=[pm, sgb], w=[tp])
                    S.op("dve", lambda e: e.tensor_tensor(out=o[:, g, :], in0=tp[:], in1=su[:, g, :], op=ALU.mult), r=[tp, su], w=[(o, g)])
                S.dma("sp", mT_d[:, H + NG:H + 2 * NG, ti * P:(ti + 1) * P], o[:], r=[o], w_=[("mT_d", 5000 + ti)])
            S.pop()

        def phase_outproj(l, upd):
            S.push()
            OC = min(1024, D)
            wo = S.sb("wo", [P, KM, OC], BF16)
            stg = [S.sb("stgo", [P, 2, OC], F32) for _ in range(2)]
            g1 = {v: S.sb("g1", [P, OC], F32) for v in ((0, 1) if upd else (0,))}
            mts = [S.sb("mt", [P, KM, 4 * P], BF16) for _ in range(2)]
            xts = [S.sb("xto", [P, OC], F32) for _ in range(2)]
            xos = [S.sb("xoo", [P, OC], F32) for _ in range(2)]
            pps = [S.ps("ppo", [P, 512], F32) for _ in range(2)]
            tiles = list(range(NTL)) + (list(range(NTL, NT)) if upd else [])
            for cs in range(D // OC):
                for kk in range(0, KM, 2):
                    st = stg[(kk // 2) % 2]
                    S.dma("sp" if (kk // 2) % 2 == 0 else "act", st[:], w_out[l][kk * P:(kk + 2) * P, cs * OC:(cs + 1) * OC].rearrange("(k p) c -> p k c", p=P), w_=[st])
                    S.op("pool", lambda e: e.tensor_copy(wo[:, kk:kk + 2, :], st[:]), r=[st], w=[(wo, kk)])
                for v in g1:
                    S.dma("sp", g1[v][:], bc_row(mods_v[v, 2, cs * OC:(cs + 1) * OC], OC), r=["mods_d"], w_=[g1[v]])
                for i, ti in enumerate(tiles):
                    v = 0 if ti < NTL else 1
                    xt = xts[i % 2]; xo = xos[i % 2]
                    if ti < NTL:
                        g0 = (ti // 4) * 4; gn = min(4, NTL - g0)
                    else:
                        g0 = NTL + ((ti - NTL) // 4) * 4; gn = min(4, NT - g0)
                    mtg = mts[(g0 // 4) % 2] if ti < NTL else mts[((NTL + 3) // 4 + (ti - NTL) // 4) % 2]
                    if ti == g0:
                        S.dma("sp", mtg[:, :, 0:gn * P], mT_d[:, :, g0 * P:(g0 + gn) * P], r=["mT_d"], w_=[mtg])
                    mt = mtg[:, :, (ti - g0) * P:(ti - g0 + 1) * P]
                    S.dma("act", xt[:], x_d[ti * P:(ti + 1) * P, cs * OC:(cs + 1) * OC], r=[("x_d", ti)], w_=[xt])
                    for hf in range(OC // 512):
                        pp = pps[hf % 2]
                        for k in range(KM):
                            S.op("pe", lambda e: e.matmul(pp[:], mt[:, k, :], wo[:, k, hf * 512:(hf + 1) * 512], start=(k == 0), stop=(k == KM - 1)), r=[mtg, wo], w=[pp], sig=(k == KM - 1))
                        S.op("dve", lambda e: e.tensor_tensor(out=xo[:, hf * 512:(hf + 1) * 512], in0=pp[:], in1=g1[v][:, hf * 512:(hf + 1) * 512], op=ALU.mult), r=[pp, g1[v]], w=[(xo, hf)])
                    S.op("dve", lambda e: e.tensor_tensor(out=xo[:], in0=xo[:], in1=xt[:], op=ALU.add), r=[xo, xt], w=[xo])
                    S.dma("sp", x_d[ti * P:(ti + 1) * P, cs * OC:(cs + 1) * OC], xo[:], r=[xo], w_=[("x_d", ti)])
            S.pop()

        def phase_topk(upd):
            S.push()
            sets = [(0, NTL, capL, 0)] + ([(NTL, NT, capC, capL)] if upd else [])
            zt = S.sb("zt", [P, D], F32)
            S.op("pool", lambda e: e.memset(zt[:], 0.0), w=[zt])
            for ti in range(NT + 1):
                S.dma("sp" if ti % 2 == 0 else "act", M_d[ti * P:(ti + 1) * P, :], zt[:], r=[zt], w_=["M_d"])
            ztb = S.sb("ztb", [P, D], BF16)
            S.op("pool", lambda e: e.memset(ztb[:], 0.0), w=[ztb])
            S.dma("sp", htm_d[NTOK:NTOK + P, :], ztb[:], r=[ztb], w_=["htm_d"])
            lo = S.sb("lo", [P, E], F32); hi = S.sb("hi", [P, E], F32); mid = S.sb("mid", [P, E], F32)
            cnp = S.sb("cnp", [P, E], F32); ge = S.sb("ge", [P, E], F32); d1 = S.sb("d1", [P, E], F32); d2 = S.sb("d2", [P, E], F32)
            cmpj = S.sb("cmpj", [P, NT], F32)
            pq = [S.ps("pq", [P, 512], F32) for _ in range(2)]
            pct = S.ps("pct", [P, E], F32)
            for (a, b, cap, base) in sets:
                S.op("dve", lambda e: e.memset(lo[:], 0.0), w=[lo])
                S.op("dve", lambda e: e.memset(hi[:], 1.0), w=[hi])
                for it in range(30):
                    S.op("dve", lambda e: e.tensor_tensor(out=mid[:], in0=lo[:], in1=hi[:], op=ALU.add), r=[lo, hi], w=[mid])
                    S.op("dve", lambda e: e.tensor_scalar(mid[:], mid[:], 0.5, None, op0=ALU.mult), r=[mid], w=[mid])
                    for ex in range(E):
                        S.op("dve", lambda e: e.tensor_scalar(cmpj[:, a:b], aff_e[:, ex, a:b], mid[:, ex:ex + 1], 0.0, op0=ALU.is_ge, op1=ALU.add, accum_out=cnp[:, ex:ex + 1]),
                             r=[aff_e, mid], w=[cmpj, (cnp, ex)])
                    S.op("pe", lambda e: e.matmul(pct[:], ones_f[:], cnp[:], start=True, stop=True), r=[ones_f, cnp], w=[pct])
                    S.op("dve", lambda e: e.tensor_scalar(ge[:], pct[:], float(cap) - 0.5, None, op0=ALU.is_ge), r=[pct], w=[ge])
                    S.op("dve", lambda e: e.tensor_tensor(out=d1[:], in0=mid[:], in1=lo[:], op=ALU.subtract), r=[mid, lo], w=[d1])
                    S.op("dve", lambda e: e.tensor_tensor(out=d2[:], in0=hi[:], in1=mid[:], op=ALU.subtract), r=[mid, hi], w=[d2])
                    S.op("dve", lambda e: e.tensor_tensor(out=d1[:], in0=d1[:], in1=ge[:], op=ALU.mult), r=[d1, ge], w=[d1])
                    S.op("dve", lambda e: e.tensor_tensor(out=d2[:], in0=d2[:], in1=ge[:], op=ALU.mult), r=[d2, ge], w=[d2])
                    S.op("dve", lambda e: e.tensor_tensor(out=lo[:], in0=lo[:], in1=d1[:], op=ALU.add), r=[lo, d1], w=[lo])
                    S.op("dve", lambda e: e.tensor_tensor(out=hi[:], in0=mid[:], in1=d2[:], op=ALU.add), r=[mid, d2], w=[hi])
                for ex in range(E):
                    S.op("dve", lambda e: e.tensor_scalar(msk[:, ex, a:b], aff_e[:, ex, a:b], lo[:, ex:ex + 1], None, op0=ALU.is_ge), r=[aff_e, lo], w=[(msk, (ex, a))])
            S.op("dve", lambda e: e.tensor_tensor(out=gate[:], in0=msk[:], in1=aff_e[:], op=ALU.mult), r=[msk, aff_e], w=[gate])
            mflat = msk[:].rearrange("p e t -> p (e t)")
            pw = S.sb("pw", [P, E, NT], F32); tot = S.sb("tot", [P, E, NT], F32); tb2 = S.sb("tb2", [P, E, NT], F32)
            pwf = pw[:].rearrange("p e t -> p (e t)"); totf = tot[:].rearrange("p e t -> p (e t)")
            n = E * NT
            for c0 in range(0, n, 512):
                m = min(512, n - c0)
                S.op("pe", lambda e: e.matmul(pq[0][:, :m], ustr[:], mflat[:, c0:c0 + m], start=True, stop=True), r=[ustr, msk], w=[pq[0]])
                S.op("dve", lambda e: e.tensor_copy(pwf[:, c0:c0 + m], pq[0][:, :m]), r=[pq[0]], w=[pw])
                S.op("pe", lambda e: e.matmul(pq[1][:, :m], ones_f[:], mflat[:, c0:c0 + m], start=True, stop=True), r=[ones_f, msk], w=[pq[1]])
                S.op("dve", lambda e: e.tensor_copy(totf[:, c0:c0 + m], pq[1][:, :m]), r=[pq[1]], w=[tot])
            for (a, b, cap, base) in sets:
                nxt = tb2
                src0 = S.sb("src0", [P, E, NT], F32)
                S.op("dve", lambda e: e.tensor_copy(src0[:, :, a:b], tot[:, :, a:b]), r=[tot], w=[src0])
                cur = src0
                s = 1
                while s < (b - a):
                    S.op("dve", lambda e: e.tensor_tensor(out=nxt[:, :, a + s:b], in0=cur[:, :, a + s:b], in1=cur[:, :, a:b - s], op=ALU.add), r=[cur], w=[nxt])
                    S.op("dve", lambda e: e.tensor_copy(nxt[:, :, a:a + s], cur[:, :, a:a + s]), r=[cur], w=[nxt])
                    cur, nxt = nxt, cur
                    s *= 2
                S.op("dve", lambda e: e.tensor_tensor(out=cur[:, :, a:b], in0=cur[:, :, a:b], in1=tot[:, :, a:b], op=ALU.subtract), r=[cur, tot], w=[cur])
                S.op("dve", lambda e: e.tensor_tensor(out=pw[:, :, a:b], in0=pw[:, :, a:b], in1=cur[:, :, a:b], op=ALU.add), r=[pw, cur], w=[pw])
                if base:
                    S.op("dve", lambda e: e.tensor_scalar(pw[:, :, a:b], pw[:, :, a:b], float(base), None, op0=ALU.add), r=[pw], w=[pw])
            hiT = NT if upd else NTL
            S.op("dve", lambda e: e.scalar_tensor_tensor(out=posm[:, :, 0:hiT], in0=pw[:, :, 0:hiT], scalar=1.0, in1=msk[:, :, 0:hiT], op0=ALU.add, op1=ALU.mult), r=[pw, msk], w=[posm])
            S.op("dve", lambda e: e.tensor_scalar(posm[:, :, 0:hiT], posm[:, :, 0:hiT], -1.0, None, op0=ALU.add), r=[posm], w=[posm])
            S.pop()

        def phase_moe(l, upd):
            S.push()
            nslots = capL + (capC if upd else 0)
            NSB = (nslots + P - 1) // P
            NS = NSB * P
            tiles = list(range(NTL)) + (list(range(NTL, NT)) if upd else [])
            DH = min(2048, D) if D > 1024 else D // 2
            NDH = D // DH
            M2 = M_d.rearrange("t (h d) -> (t h) d", d=DH)
            iot = S.sb("iot", [P, NS], F32)
            S.op("pool", lambda e: e.iota(iot[:], pattern=[[1, NS]], base=0, channel_multiplier=0, allow_small_or_imprecise_dtypes=True), w=[iot])
            pg = S.ps("pg", [P, 512], F32); pu = S.ps("pu", [P, 512], F32)
            py = [S.ps("py", [P, 512], F32) for _ in range(2)]
            ptr = [S.ps("ptr", [P, P], BF16) for _ in range(2)]
            pi = S.ps("pi", [P, 8], F32)
            pis = S.sb("pis", [P, 8], F32)
            pcol = S.sb("pcol", [P, 1], F32); pbase = S.sb("pbase", [P, 1], F32)
            S.op("pe", lambda e: e.matmul(pi[:, 0:1], ustr[:], ones_f[:, 0:1], start=True, stop=True), r=[ustr, ones_f], w=[pi])
            S.op("dve", lambda e: e.tensor_copy(pcol[:], pi[:, 0:1]), r=[pi], w=[pcol])
            S.op("dve", lambda e: e.tensor_scalar(pbase[:], pcol[:], float(NTOK), None, op0=ALU.add), r=[pcol], w=[pbase])
            rv5 = S.sb("rv5", [P, E, NT, 5], BF16)
            S.push()
            r3 = S.sb("r3", [P, NT, 3], F32)
            gtmp = S.sb("gtmp", [P, E, NT], F32)
            for ti in range(NT):
                S.op("pool", lambda e: e.memset(r3[:, ti, 0:1], float(ti)), w=[(r3, (ti, 0))])
                S.op("dve", lambda e: e.tensor_copy(r3[:, ti, 1:2], pcol[:]), r=[pcol], w=[(r3, (ti, 1))])
                S.op("pool", lambda e: e.memset(r3[:, ti, 2:3], 1.0), w=[(r3, (ti, 2))])
            for ex in range(E):
                S.op("dve", lambda e: e.tensor_copy(rv5[:, ex, :, 0:3], r3[:]), r=[r3], w=[(rv5, ("c", ex))])
            S.op("dve", lambda e: e.tensor_copy(rv5[:, :, :, 3], gate[:]), r=[gate], w=[(rv5, "hi")])
            S.op("dve", lambda e: e.tensor_copy(gtmp[:], rv5[:, :, :, 3]), r=[rv5], w=[gtmp])
            S.op("dve", lambda e: e.tensor_tensor(out=gtmp[:], in0=gate[:], in1=gtmp[:], op=ALU.subtract), r=[gate, gtmp], w=[gtmp])
            S.op("dve", lambda e: e.tensor_copy(rv5[:, :, :, 4], gtmp[:]), r=[gtmp], w=[(rv5, "lo")])
            S.pop()
            oh = [S.sb("oh", [P, P], BF16) for _ in range(3)]
            sif = S.sb("sif", [P, 1], F32); dif = S.sb("dif", [P, 1], F32)
            sidx = [S.sb("sidx", [P, 1], I32) for _ in range(2)]
            si2 = [[S.sb("si2", [P, 1], I32) for _ in range(NDH)] for _ in range(NSB)]
            gs = [S.sb("gs", [P, 1], F32) for _ in range(NSB)]
            hg = [S.sb("hg", [P, D], BF16) for _ in range(1)]
            hsT = S.sb("hsT", [P, KD, NS], BF16)
            FH = 128
            SZ = KD * 2 * FH
            wbuf = S.sb("wbuf", [P, max(2 * SZ, FC * DH)], BF16)
            wgus = [wbuf[:, sl * SZ:(sl + 1) * SZ].rearrange("p (k w f) -> p k w f", k=KD, w=2) for sl in range(2)]
            wd = wbuf[:, 0:FC * DH].rearrange("p (f d) -> p f d", f=FC)
            wd_slots = lambda f: list(range((f * DH) // SZ, min(1, ((f + 1) * DH - 1) // SZ) + 1))
            KS = min(4, KD)
            stg = [S.sb("stgm", [P, KS, FH], F32) for _ in range(2)]
            gc = 0
            aTt = S.sb("aTt", [P, FC, NS], BF16)
            sgt = S.sb("sgt", [P, 512], F32)
            DS = min(1024, DH)
            stgd = [S.sb("stgd", [P, DS], F32) for _ in range(2)]
            xg = [S.sb("xg", [P, DH], F32) for _ in range(2)]
            nst = [0]; gcn = [0]
            hg2 = [hg[0], S.sb("hgB", [P, D], BF16)]
            si2p = [si2, [[S.sb("si2b", [P, 1], I32) for _ in range(NDH)] for _ in range(NSB)]]
            gsp = [gs, [S.sb("gsb", [P, 1], F32) for _ in range(NSB)]]
            sidxp = [[S.sb("sidxa", [P, 1], I32) for _ in range(NSB)], [S.sb("sidxb", [P, 1], I32) for _ in range(NSB)]]

            def a_idx(ex, sb):
                par = ex % 2
                for i, ti in enumerate(tiles):
                    o = oh[i % 3]
                    S.op("dve", lambda e: e.tensor_scalar(o[:], iot[:, sb * P:(sb + 1) * P], posm[:, ex, ti:ti + 1], None, op0=ALU.is_equal), r=[iot, posm], w=[o])
                    S.op("pe", lambda e: e.matmul(pi[:, 0:5], o[:], rv5[:, ex, ti, :], start=(i == 0), stop=(i == len(tiles) - 1)), r=[o, rv5], w=[pi])
                si = sidxp[par][sb]; hgt = hg2[sb % 2]
                S.op("dve", lambda e: e.tensor_copy(pis[:, 0:5], pi[:, 0:5]), r=[pi], w=[pis])
                S.op("dve", lambda e: e.scalar_tensor_tensor(out=sif[:], in0=pis[:, 0:1], scalar=128.0, in1=pis[:, 1:2], op0=ALU.mult, op1=ALU.add), r=[pis], w=[sif])
                S.op("dve", lambda e: e.tensor_tensor(out=dif[:], in0=sif[:], in1=pbase[:], op=ALU.subtract), r=[sif, pbase], w=[dif])
                S.op("dve", lambda e: e.scalar_tensor_tensor(out=sif[:], in0=dif[:], scalar=pis[:, 2:3], in1=pbase[:], op0=ALU.mult, op1=ALU.add), r=[dif, pis, pbase], w=[sif])
                S.op("dve", lambda e: e.tensor_copy(si[:], sif[:]), r=[sif], w=[si])
                for dh in range(NDH):
                    S.op("dve", lambda e: e.tensor_scalar(si2p[par][sb][dh][:], sif[:], float(NDH), float(dh), op0=ALU.mult, op1=ALU.add), r=[sif], w=[si2p[par][sb][dh]])
                S.op("dve", lambda e: e.tensor_tensor(out=gsp[par][sb][:], in0=pis[:, 3:4], in1=pis[:, 4:5], op=ALU.add), r=[pis], w=[gsp[par][sb]])
                S.dma_custom("pool", lambda e: e.indirect_dma_start(out=hgt[:], out_offset=None, in_=htm_d, in_offset=bass.IndirectOffsetOnAxis(ap=si[:, 0:1], axis=0)),
                             r=[si, "htm_d"], w_=[hgt])

            def a_tr(ex, sb):
                hgt = hg2[sb % 2]
                for k in range(KD):
                    pt = ptr[k % 2]
                    S.op("pe", lambda e: e.transpose(pt[:], hgt[:, k * P:(k + 1) * P], ident_b[:]), r=[hgt, ident_b], w=[pt])
                    if k % 2 == 0:
                        S.op("act", lambda e: e.activation(hsT[:, k, sb * P:(sb + 1) * P], pt[:], AF.Copy), r=[pt], w=[(hsT, (k, sb))])
                    else:
                        S.op("dve", lambda e: e.tensor_copy(hsT[:, k, sb * P:(sb + 1) * P], pt[:]), r=[pt], w=[(hsT, (k, sb))])

            def gate_up(ex):
                for fc in range(FF // FH):
                    sl = gcn[0] % 2; gcn[0] += 1
                    wgu = wgus[sl]
                    for which, wsrc in ((0, w_gate), (1, w_up)):
                        for kk in range(0, KD, KS):
                            st = stg[nst[0] % 2]; nst[0] += 1
                            S.dma("sp", st[:], wsrc[l, ex][kk * P:(kk + KS) * P, fc * FH:(fc + 1) * FH].rearrange("(k p) c -> p k c", p=P), w_=[st])
                            S.op("pool", lambda e: e.tensor_copy(wgu[:, kk:kk + KS, which, :], st[:]), r=[st], w=[(wbuf, sl)])
                    for s0 in range(0, NS, 512):
                        ns = min(512, NS - s0)
                        for k in range(KD):
                            S.op("pe", lambda e: e.matmul(pg[:, :ns], wgu[:, k, 0, :], hsT[:, k, s0:s0 + ns], start=(k == 0), stop=(k == KD - 1)), r=[(wbuf, sl), hsT], w=[pg], sig=(k == KD - 1))
                        for k in range(KD):
                            S.op("pe", lambda e: e.matmul(pu[:, :ns], wgu[:, k, 1, :], hsT[:, k, s0:s0 + ns], start=(k == 0), stop=(k == KD - 1)), r=[(wbuf, sl), hsT], w=[pu], sig=(k == KD - 1))
                        S.op("act", lambda e: e.activation(sgt[:, :ns], pg[:, :ns], AF.Silu), r=[pg], w=[sgt])
                        S.op("dve", lambda e: e.tensor_tensor(out=aTt[:, fc, s0:s0 + ns], in0=sgt[:, :ns], in1=pu[:, :ns], op=ALU.mult), r=[sgt, pu], w=[(aTt, (fc, s0))])

            def load_wd(ex, dh):
                for f in range(FC):
                    for d0 in range(0, DH, DS):
                        st = stgd[nst[0] % 2]; nst[0] += 1
                        S.dma("sp", st[:], w_down[l, ex][f * P:(f + 1) * P, dh * DH + d0: dh * DH + d0 + DS], w_=[st])
                        S.op("pool", lambda e: e.tensor_copy(wd[:, f, d0:d0 + DS], st[:]), r=[st], w=[(wbuf, sl_) for sl_ in wd_slots(f)])

            def gather_x(ex, dh, sb):
                x_ = xg[sb % 2]; ixt = si2p[ex % 2][sb][dh]
                prev_keys = [("M_d", (dh, (ex - 1) % 2, sbp)) for sbp in range(NSB)]
                S.dma_custom("pool", lambda e: e.indirect_dma_start(out=x_[:], out_offset=None, in_=M2, in_offset=bass.IndirectOffsetOnAxis(ap=ixt[:, 0:1], axis=0)),
                             r=[ixt] + prev_keys, w_=[x_])

            def down_block(ex, dh, sb):
                par = ex % 2
                if sb == 0:
                    load_wd(ex, dh)
                    gather_x(ex, dh, 0)
                if sb + 1 < NSB:
                    gather_x(ex, dh, sb + 1)
                x_ = xg[sb % 2]; ixt = si2p[par][sb][dh]
                for cb in range(DH // 512):
                    pp = py[cb % 2]
                    for f in range(FC):
                        S.op("pe", lambda e: e.matmul(pp[:], aTt[:, f, sb * P:(sb + 1) * P], wd[:, f, cb * 512:(cb + 1) * 512], start=(f == 0), stop=(f == FC - 1)), r=[aTt, (wbuf, 0), (wbuf, 1)], w=[pp], sig=(f == FC - 1))
                    S.op("dve", lambda e: e.scalar_tensor_tensor(out=x_[:, cb * 512:(cb + 1) * 512], in0=pp[:], scalar=gsp[par][sb][:, 0:1], in1=x_[:, cb * 512:(cb + 1) * 512], op0=ALU.mult, op1=ALU.add),
                         r=[pp, gsp[par][sb], x_], w=[x_])
                S.dma_custom("pool", lambda e: e.indirect_dma_start(out=M2, out_offset=bass.IndirectOffsetOnAxis(ap=ixt[:, 0:1], axis=0), in_=x_[:], in_offset=None),
                             r=[ixt, x_], w_=[("M_d", (dh, par, sb))])

            for sb in range(NSB):
                a_idx(0, sb)
                if sb >= 1:
                    a_tr(0, sb - 1)
            a_tr(0, NSB - 1)
            for ex in range(E):
                gate_up(ex)
                blocks = [(dh, sb) for dh in range(NDH) for sb in range(NSB)]
                nxt = ex + 1 < E
                ia = 0; it_ = 0
                for bi, (dh, sb) in enumerate(blocks):
                    if nxt and ia < NSB and (bi * NSB) // len(blocks) >= ia:
                        a_idx(ex + 1, ia); ia += 1
                    down_block(ex, dh, sb)
                    if nxt and it_ < ia - 1:
                        a_tr(ex + 1, it_); it_ += 1
                if nxt:
                    while ia < NSB:
                        a_idx(ex + 1, ia); ia += 1
                    while it_ < NSB:
                        a_tr(ex + 1, it_); it_ += 1
            S.pop()

        def phase_combine(l, upd, last):
            S.push()
            tiles = list(range(NTL)) + (list(range(NTL, NT)) if upd else [])
            g2 = {v: S.sb("g2", [P, D], F32) for v in ((0, 1) if upd else (0,))}
            for v in g2:
                S.dma("sp", g2[v][:], bc_row(mods_v[v, 5], D), r=["mods_d"], w_=[g2[v]])
            xts = [S.sb("xtc", [P, D], F32) for _ in range(2)]
            mts = [S.sb("mtc", [P, D], F32) for _ in range(2)]
            for i, ti in enumerate(tiles):
                v = 0 if ti < NTL else 1
                xt = xts[i % 2]; mt = mts[i % 2]
                S.dma("sp", xt[:], x_d[ti * P:(ti + 1) * P, :], r=[("x_d", ti)], w_=[xt])
                S.dma("act", mt[:], M_d[ti * P:(ti + 1) * P, :], r=["M_d"], w_=[mt])
                S.op("dve", lambda e: e.tensor_tensor(out=mt[:], in0=mt[:], in1=g2[v][:], op=ALU.mult), r=[mt, g2[v]], w=[mt])
                S.op("pool", lambda e: e.tensor_tensor(out=mt[:], in0=mt[:], in1=xt[:], op=ALU.add), r=[mt, xt], w=[mt])
                if last:
                    S.dma("act", out[ti * P:(ti + 1) * P, :], mt[:], r=[mt], w_=[("out", ti)], out_dram=True)
                else:
                    S.dma("act", x_d[ti * P:(ti + 1) * P, :], mt[:], r=[mt], w_=[("x_d", ti)])
            S.pop()

        all_tiles = list(range(NT))
        lat_tiles = list(range(NTL))
        stop_after = cfg.get("stop_after")
        plist = []
        for l in range(DEPTH):
            upd = l < DEPTH - 1
            plist += [lambda l=l: phase_ada(l),
                      lambda l=l: phase_norm(l, all_tiles, norm1, 0, 1, router=False),
                      lambda l=l: phase_inproj(l),
                      lambda l=l, upd=upd: phase_attn(l, upd),
                      lambda l=l, upd=upd: phase_conv_sg(l, upd),
                      lambda l=l, upd=upd: phase_outproj(l, upd),
                      lambda l=l, upd=upd: phase_norm(l, all_tiles if upd else lat_tiles, norm2, 3, 4, router=True),
                      lambda l=l, upd=upd: phase_topk(upd),
                      lambda l=l, upd=upd: phase_moe(l, upd),
                      lambda l=l, upd=upd: phase_combine(l, upd, last=(l == DEPTH - 1))]
        import os as _os
        nstop = int(_os.environ.get("MK_STOP", "1000"))
        for i, ph in enumerate(plist):
            if i >= nstop:
                break
            ph()
        S.finish()
    cfg["ninst"] = S.ninst; cfg["nwait"] = S.nwait; cfg["nsem"] = getattr(S, "nsem", 0)
    return nc


def host_prep(cfg, inputs, b):
    D, SEQ, CTX, E, FF, DEPTH, H, NG, KD = (cfg[k] for k in ("D", "SEQ", "CTX", "E", "FF", "DEPTH", "H", "NG", "KD"))
    f = lambda a: np.ascontiguousarray(np.asarray(a, dtype=np.float32))
    NAW = H * 128; CW = NG * 128
    m = {}
    m["x"] = f(inputs["x"][b]); m["ctx"] = f(inputs["ctx"][b])
    m["cvec"] = f(np.stack([np.asarray(inputs["c"])[b], np.asarray(inputs["c_ctx"])], 0).reshape(2 * KD, 128))
    m["w_ada"] = f(inputs["w_ada"]); m["b_ada"] = f(np.asarray(inputs["b_ada"]).reshape(DEPTH, 1, 6 * D))
    m["norm1"] = f(inputs["norm1"]); m["norm2"] = f(inputs["norm2"])
    w_in = np.asarray(inputs["w_in"])
    cols = []
    for g in range(NG):
        for base in (0, NAW):
            for hh in (2 * g, 2 * g + 1):
                cols.append(np.arange(base + hh * 128, base + (hh + 1) * 128))
        o = 3 * NAW
        for j in range(3):
            cols.append(np.arange(o + j * CW + g * 128, o + j * CW + (g + 1) * 128))
        o2 = 3 * NAW + 3 * CW
        cols.append(np.arange(o2 + g * 128, o2 + (g + 1) * 128))
        for hh in (2 * g, 2 * g + 1):
            cols.append(np.arange(2 * NAW + hh * 128, 2 * NAW + (hh + 1) * 128))
        cols.append(np.arange(o2 + CW + g * 128, o2 + CW + (g + 1) * 128))
    cols = np.concatenate(cols)
    m["w_in"] = f(w_in[:, :, cols])
    m["q_norm"] = f(inputs["q_norm"]); m["k_norm"] = f(inputs["k_norm"])
    rpb = np.asarray(inputs["rpb"])
    ck = np.arange(64)[:, None]; cq = np.arange(64)[None, :]
    dc = np.clip(ck - cq + 15, 0, 30)
    t = rpb[:, :, :, dc]
    top = t.transpose(0, 1, 3, 2, 4)
    bot = np.concatenate([top[:, :, :, 1:, :], top[:, :, :, 14:15, :]], axis=3)
    m["rpbT"] = f(np.concatenate([top, bot], axis=2).reshape(DEPTH, H, 128, 15 * 64))
    cs = np.clip(np.arange(64) - 8, 0, 48)
    valid = (ck >= cs[None, :]) & (ck < cs[None, :] + 16)
    mk = np.where(valid, 0.0, -30000.0).astype(np.float32)
    mk = np.broadcast_to(np.concatenate([mk, mk], 0)[:, None, :], (128, 15, 64))
    m["maskT"] = f(mk.reshape(128, 15 * 64))
    cwv = np.asarray(inputs["conv_w"])
    m["conv_w"] = f(cwv.transpose(0, 2, 1).reshape(DEPTH, NG, 128, 3))
    m["sg_norm"] = f(inputs["sg_norm"])
    sgw = np.asarray(inputs["sg_w"])
    m["sg_wT"] = f(sgw.transpose(0, 3, 1, 2).reshape(DEPTH, 128, NG * 128))
    m["sg_b"] = f(np.asarray(inputs["sg_b"]).reshape(DEPTH, NG * 128))
    m["w_out"] = f(inputs["w_out"]); m["w_router"] = f(inputs["w_router"])
    m["w_gate"] = f(inputs["w_gate"]); m["w_up"] = f(inputs["w_up"]); m["w_down"] = f(inputs["w_down"])
    return m


_CACHE = {}


def kernel(**inputs):
    x = np.asarray(inputs["x"])
    B, SEQ, D = x.shape
    cfg = make_cfg(D=D, SEQ=SEQ, CTX=np.asarray(inputs["ctx"]).shape[1], E=np.asarray(inputs["w_router"]).shape[-1],
                   FF=np.asarray(inputs["w_gate"]).shape[-1], DEPTH=np.asarray(inputs["w_ada"]).shape[0])
    key = tuple(sorted((k, v) for k, v in cfg.items() if isinstance(v, int)))
    if key not in _CACHE:
        _CACHE[key] = build(cfg)
    nc = _CACHE[key]
    in_maps = [host_prep(cfg, inputs, b) for b in range(B)]
    res = run_bass_kernel_spmd(nc, in_maps, core_ids=list(range(B)))
    return np.stack([res.results[b]["out"] for b in range(B)], 0).astype(np.float32)
```

```python
import numpy as np
from contextlib import ExitStack, contextmanager
import concourse.bass as bass
import concourse.mybir as mybir
from concourse.bass_utils import run_bass_kernel_spmd

F32 = mybir.dt.float32
BF16 = mybir.dt.bfloat16
I32 = mybir.dt.int32
AF = mybir.ActivationFunctionType
ALU = mybir.AluOpType
AX = mybir.AxisListType

SEM_WRAP = 1 << 30


class Sync:
    def __init__(self, nc):
        self.nc = nc
        self.stack = ExitStack()
        self.eng = {"pe": nc.tensor, "act": nc.scalar, "dve": nc.vector, "pool": nc.gpsimd, "sp": nc.sync}
        self.cnt = {}
        self.esem = {}
        self.seen = {e: {} for e in self.eng}
        self.rec = {}
        self.dsem = {}
        self.semname = {}
        self.out_tokens = []
        self.nwait = 0
        self.ninst = 0

    @contextmanager
    def ctx(self):
        with self.stack:
            for e in ("pe", "act", "dve", "pool"):
                self.esem[e] = self.stack.enter_context(self.nc.semaphore(f"c_{e}"))
                self.cnt[e] = 0
            yield self

    def sb(self, name, shape, dt):
        return self.stack.enter_context(self.nc.sbuf_tensor(name, list(shape), dt))

    def ps(self, name, shape, dt):
        return self.stack.enter_context(self.nc.psum_tensor(name, list(shape), dt))

    @staticmethod
    def _split(k):
        if isinstance(k, tuple):
            return (k[0] if isinstance(k[0], str) else id(k[0])), k[1]
        return (k if isinstance(k, str) else id(k)), None

    def _recs(self, k, create):
        oid, sub = self._split(k)
        d = self.rec.setdefault(oid, {})
        if sub is None:
            if create and None not in d:
                d[None] = [None, []]
            return list(d.values()) if not create else list(d.values())
        out = []
        if None in d:
            out.append(d[None])
        if sub not in d and create:
            d[sub] = [None, []]
        if sub in d:
            out.append(d[sub])
        return out

    def _tok(self, t):
        sem, val, isdma, key = t
        if isdma and key in self.dsem and self.dsem[key][0] is sem:
            val = max(val, self.dsem[key][1])
        return sem, val

    def _deps(self, r, w):
        deps = {}
        def add(t):
            if t is None:
                return
            sem, val = self._tok(t)
            sid = id(sem)
            if sid not in deps or deps[sid][1] < val:
                deps[sid] = (sem, val)
        for k in r:
            for rc in self._recs(k, False):
                add(rc[0])
        for k in w:
            for rc in self._recs(k, False):
                add(rc[0])
                for t in rc[1]:
                    add(t)
        return deps

    def _commit(self, r, w, tok):
        for k in w:
            oid, sub = self._split(k)
            d = self.rec.setdefault(oid, {})
            if sub is None:
                d.clear()
                d[None] = [tok, []]
            else:
                d[sub] = [tok, []]
        for k in r:
            oid, sub = self._split(k)
            d = self.rec.setdefault(oid, {})
            if sub not in d:
                d[sub] = [None, []]
            rd = d[sub][1]
            rd[:] = [t for t in rd if t[0] is not tok[0]]
            rd.append(tok)

    def _emit_waits(self, e, deps, skip_sem=None):
        eng = self.eng[e]
        for sid, (sem, val) in deps.items():
            if skip_sem is not None and sem is skip_sem:
                continue
            if self.seen[e].get(sid, 0) >= val:
                continue
            eng.wait_ge(sem, val)
            self.nwait += 1
            self.seen[e][sid] = val

    def op(self, e, fn, r=(), w=(), acc=False, sig=True):
        deps = self._deps(r, w)
        self._emit_waits(e, deps, skip_sem=self.esem["pe"] if e == "pe" else None)
        ins = fn(self.eng[e])
        self.ninst += 1
        if sig:
            self.cnt[e] += 1
            ins.then_inc(self.esem[e], 1)
            tok = (self.esem[e], self.cnt[e], False, None)
        else:
            tok = (self.esem[e], self.cnt[e] + 1, False, None)
        self._commit(r, w, tok)
        return ins

    def dma(self, q, out, in_, r=(), w_=(), sem_key=None, out_dram=False, **kw):
        if sem_key is None:
            ks = list(w_) + list(r)
            nk = [k for k in ks if not isinstance(k[0] if isinstance(k, tuple) else k, str)]
            k0 = (nk or ks)[0]
            sem_key = self._split(k0)[0]
        self._get_dsem(sem_key)
        deps = self._deps(r, w_)
        self._emit_waits(q, deps)
        ent = self.dsem[sem_key]
        ent[1] += 16
        ins = self.eng[q].dma_start(out=out, in_=in_, **kw)
        ins.then_inc(ent[0], 16)
        self.ninst += 1
        tok = (ent[0], ent[1], True, sem_key)
        self._commit(r, w_, tok)
        if out_dram:
            self.out_tokens.append(tok)
        return ins

    def _get_dsem(self, sem_key):
        if sem_key not in self.dsem:
            fl = self.__dict__.setdefault("free_sems", [])
            if fl:
                sem, cntv = fl.pop()
                self.dsem[sem_key] = [sem, cntv]
            else:
                self.nsem = getattr(self, "nsem", 0) + 1
                sem = self.stack.enter_context(self.nc.semaphore(f"d{self.nsem}"))
                self.dsem[sem_key] = [sem, 0]

    def finish(self):
        deps = {}
        for t in self.out_tokens:
            sem, val = self._tok(t)
            sid = id(sem)
            if sid not in deps or deps[sid][1] < val:
                deps[sid] = (sem, val)
        for e in ("pe", "act", "dve", "pool"):
            if self.cnt[e] > 0:
                deps[id(self.esem[e])] = (self.esem[e], self.cnt[e])
        for k, (sem, issued) in self.dsem.items():
            if issued > 0:
                deps[id(sem)] = (sem, issued)
        for (sem, issued) in self.__dict__.get("free_sems", []):
            if issued > 0 and id(sem) not in deps:
                deps[id(sem)] = (sem, issued)
        self._emit_waits("sp", deps)


def make_identity(S, ident):
    nc = S.nc
    n = ident.shape[-1]
    def f(e):
        return e.memset(ident[:], 1.0)
    S.op("pool", f, w=[ident])
    def g(e):
        return e.affine_select(out=ident[:], in_=ident[:], pattern=[[1, n]], compare_op=ALU.is_equal,
                               fill=0.0, base=0, channel_multiplier=-1)
    S.op("pool", g, r=[ident], w=[ident])


def _coll(self, kind, op, ins_ap, outs_ap, r=(), w_=(), groups=None, sem_key=None):
    if sem_key is None:
        sem_key = ("coll", len(self.dsem))
    if sem_key not in self.dsem:
        sem = self.stack.enter_context(self.nc.semaphore(f"d{len(self.dsem)}"))
        self.dsem[sem_key] = [sem, 0]
    deps = self._deps(r, w_)
    self._emit_waits("pool", deps)
    ent = self.dsem[sem_key]
    ent[1] += 16
    ins = self.nc.gpsimd.collective_compute(kind, op, replica_groups=groups, ins=[ins_ap], outs=[outs_ap])
    ins.then_inc(ent[0], 16)
    tok = (ent[0], ent[1], True, sem_key)
    self._commit(r, w_, tok)
    return ins

Sync.coll = _coll


def _dma_custom(self, q, fn, r=(), w_=(), sem_key=None, out_dram=False):
    if sem_key is None:
        ks = list(w_) + list(r)
        nk = [k for k in ks if not isinstance(k[0] if isinstance(k, tuple) else k, str)]
        k0 = (nk or ks)[0]
        sem_key = self._split(k0)[0]
    self._get_dsem(sem_key)
    deps = self._deps(r, w_)
    self._emit_waits(q, deps)
    ent = self.dsem[sem_key]
    ent[1] += 16
    ins = fn(self.eng[q])
    ins.then_inc(ent[0], 16)
    self.ninst += 1
    tok = (ent[0], ent[1], True, sem_key)
    self._commit(r, w_, tok)
    if out_dram:
        self.out_tokens.append(tok)
    return ins

Sync.dma_custom = _dma_custom


U32 = mybir.dt.uint32
EPS = 1e-6


def _scope_push(self):
    st = ExitStack()
    self.tstacks.append(st)


def _scope_pop(self):
    self.barrier()
    self.tstacks.pop().close()
    fl = self.__dict__.setdefault("free_sems", [])
    for k, (sem, issued) in self.dsem.items():
        fl.append((sem, issued))
    self.dsem.clear()
    self.rec.clear()


def _barrier(self):
    deps = {}
    for e in ("pe", "act", "dve", "pool"):
        if self.cnt[e] > 0:
            deps[id(self.esem[e])] = (self.esem[e], self.cnt[e])
    for k, (sem, issued) in self.dsem.items():
        if issued > 0:
            deps[id(sem)] = (sem, issued)
    for e in ("pe", "act", "dve", "pool", "sp"):
        self._emit_waits(e, dict(deps))


def _sb2(self, name, shape, dt):
    self.uid = getattr(self, "uid", 0) + 1
    st = self.tstacks[-1] if getattr(self, "tstacks", None) else self.stack
    return st.enter_context(self.nc.sbuf_tensor(f"{name}_{self.uid}", list(shape), dt))


def _ps2(self, name, shape, dt):
    self.uid = getattr(self, "uid", 0) + 1
    st = self.tstacks[-1] if getattr(self, "tstacks", None) else self.stack
    return st.enter_context(self.nc.psum_tensor(f"{name}_{self.uid}", list(shape), dt))


Sync.push = _scope_push
Sync.pop = _scope_pop
Sync.barrier = _barrier
Sync.sb = _sb2
Sync.ps = _ps2


def make_cfg(D=4096, SEQ=8192, CTX=256, E=16, FF=1024, DEPTH=2):
    c = dict(D=D, SEQ=SEQ, CTX=CTX, E=E, FF=FF, DEPTH=DEPTH)
    c["H"] = (D // 2) // 128
    c["NG"] = c["H"] // 2
    c["CW"] = D // 4
    c["SGW"] = D // 4
    assert c["CW"] == c["NG"] * 128
    c["KD"] = D // 128
    c["NTL"] = SEQ // 128
    c["NTC"] = CTX // 128
    c["NT"] = c["NTL"] + c["NTC"]
    c["NTOK"] = SEQ + CTX
    c["ROWS"] = SEQ // 64
    c["capL"] = 2 * SEQ // E
    c["capC"] = 2 * CTX // E
    c["SLABC"] = 11 * 128
    return c


def build(cfg):
    D, SEQ, CTX, E, FF, DEPTH = (cfg[k] for k in ("D", "SEQ", "CTX", "E", "FF", "DEPTH"))
    H, NG, CW, SGW, KD, NTL, NTC, NT, NTOK, ROWS = (cfg[k] for k in ("H", "NG", "CW", "SGW", "KD", "NTL", "NTC", "NT", "NTOK", "ROWS"))
    capL, capC, SLABC = cfg["capL"], cfg["capC"], cfg["SLABC"]
    KM = KD
    FC = FF // 128
    P = 128
    nc = bass.Bass("TRN2", target_bir_lowering=False)

    def din(name, shape, dt=F32):
        return nc.dram_tensor(name, list(shape), dt, kind="ExternalInput").ap()

    x_in = din("x", [SEQ, D]); ctx_in = din("ctx", [CTX, D]); cvec = din("cvec", [2 * KD, 128])
    w_ada = din("w_ada", [DEPTH, D, 6 * D]); b_ada = din("b_ada", [DEPTH, 1, 6 * D])
    norm1 = din("norm1", [DEPTH, D]); norm2 = din("norm2", [DEPTH, D])
    w_in = din("w_in", [DEPTH, D, NG * SLABC])
    q_norm = din("q_norm", [DEPTH, 128]); k_norm = din("k_norm", [DEPTH, 128])
    rpbT = din("rpbT", [DEPTH, H, 128, 15 * 64]); maskT = din("maskT", [128, 15 * 64])
    conv_w = din("conv_w", [DEPTH, NG, 128, 3])
    sg_norm = din("sg_norm", [DEPTH, SGW]); sg_wT = din("sg_wT", [DEPTH, 128, NG * 128]); sg_b = din("sg_b", [DEPTH, NG * 128])
    w_out = din("w_out", [DEPTH, D, D]); w_router = din("w_router", [DEPTH, D, E])
    w_gate = din("w_gate", [DEPTH, E, D, FF]); w_up = din("w_up", [DEPTH, E, D, FF]); w_down = din("w_down", [DEPTH, E, FF, D])
    out = nc.dram_tensor("out", [SEQ, D], F32, kind="ExternalOutput").ap()

    def dsc(name, shape, dt):
        return nc.dram_tensor(name, list(shape), dt).ap()

    SLOTS_MAX = ((capL + capC + 127) // 128) * 128
    mods_d = dsc("mods_d", [2 * 6 * KD, 128], F32)
    x_d = dsc("x_d", [NTOK, D], F32)
    hT_d = dsc("hT_d", [128, KD, NTOK], BF16)
    htm_d = dsc("htm_d", [NTOK + 128, D], BF16)
    M_d = dsc("M_d", [NTOK + 128, D], F32)
    qT_d = dsc("qT_d", [H, 128, NTOK], BF16); kT_d = dsc("kT_d", [H, 128, NTOK], BF16)
    v_d = dsc("v_d", [NTOK, H * 128], BF16)
    zT_d = dsc("zT_d", [NG, 128, NTOK], BF16); gbT_d = dsc("gbT_d", [NG, 128, NTOK], BF16)
    suT_d = dsc("suT_d", [NG, 128, NTOK], BF16); sv_d = dsc("sv_d", [NTOK, SGW], BF16)
    mT_d = dsc("mT_d", [128, KM, NTOK], BF16)
    Y_d = dsc("Y_d", [E * SLOTS_MAX, D], BF16)

    S = Sync(nc)
    S.tstacks = []
    mods_v = mods_d.rearrange("(v m k) p -> v m (k p)", v=2, m=6)

    def bc_row(ap1d, n):
        return ap1d.rearrange("(o d) -> o d", o=1).to_broadcast([P, n])

    with S.ctx():
        ident_b = S.sb("identb", [P, P], BF16); make_identity(S, ident_b)
        ident_f = S.sb("identf", [P, P], F32); make_identity(S, ident_f)
        ones_b = S.sb("onesb", [P, P], BF16); S.op("pool", lambda e: e.memset(ones_b[:], 1.0), w=[ones_b])
        ones_f = S.sb("onesf", [P, P], F32); S.op("pool", lambda e: e.memset(ones_f[:], 1.0), w=[ones_f])
        epsc = S.sb("epsc", [P, 1], F32); S.op("pool", lambda e: e.memset(epsc[:], EPS), w=[epsc])
        ustr = S.sb("ustr", [P, P], F32); S.op("pool", lambda e: e.memset(ustr[:], 1.0), w=[ustr])
        S.op("pool", lambda e: e.affine_select(out=ustr[:], in_=ustr[:], pattern=[[1, P]], compare_op=ALU.is_ge, fill=0.0,
                                               base=-1, channel_multiplier=-1), r=[ustr], w=[ustr])
        aff_e = S.sb("aff_e", [P, E, NT], F32)
        msk = S.sb("msk", [P, E, NT], F32)
        gate = S.sb("gate", [P, E, NT], F32)
        posm = S.sb("posm", [P, E, NT], F32)

        S.push()
        cp = [S.sb("cp", [P, D], F32) for _ in range(2)]
        for ti in range(NT):
            src = x_in[ti * P:(ti + 1) * P, :] if ti < NTL else ctx_in[(ti - NTL) * P:(ti - NTL + 1) * P, :]
            t = cp[ti % 2]
            S.dma("sp", t[:], src, w_=[t])
            S.dma("act", x_d[ti * P:(ti + 1) * P, :], t[:], r=[t], w_=[("x_d", ti)])
        S.pop()

        def rstd_of(ss, n, tag):
            sd = S.sb("sd" + tag, [P, 1], F32); rs = S.sb("rs" + tag, [P, 1], F32)
            return sd, rs

        def phase_ada(l):
            S.push()
            cv = S.sb("cv", [2 * KD, P], F32)
            S.dma("sp", cv[:], cvec, w_=[cv])
            S.op("act", lambda e: e.activation(cv[:], cv[:], AF.Silu), r=[cv], w=[cv])
            pc = S.ps("pc", [P, 2 * KD], F32)
            S.op("pe", lambda e: e.transpose(pc[:], cv[:], ident_f[:2 * KD, :2 * KD]), r=[cv, ident_f], w=[pc])
            cT = S.sb("cT", [P, KD, 2], F32)
            for v in range(2):
                S.op("dve", lambda e: e.tensor_copy(cT[:, :, v], pc[:, v * KD:(v + 1) * KD]), r=[pc], w=[(cT, v)])
            brow = S.sb("brow", [1, 6 * D], F32)
            S.dma("sp", brow[:], b_ada[l], w_=[brow])
            modsT = S.sb("modsT", [P, 2, 6 * KD], F32)
            CG = 256
            wa = [S.sb("wa", [P, KD, CG], F32) for _ in range(2)]
            pm = [S.ps("pm", [P, 2], F32) for _ in range(2)]
            for cg in range(6 * D // CG):
                w = wa[cg % 2]
                S.dma("sp" if cg % 2 == 0 else "act", w[:], w_ada[l][:, cg * CG:(cg + 1) * CG].rearrange("(k p) c -> p k c", p=P), w_=[w])
                for sub in range(CG // P):
                    j = cg * (CG // P) + sub
                    pp = pm[j % 2]
                    for k in range(KD):
                        S.op("pe", lambda e: e.matmul(pp[:], w[:, k, sub * P:(sub + 1) * P], cT[:, k, :], start=(k == 0), stop=False),
                             r=[w, cT], w=[pp], sig=False)
                    S.op("pe", lambda e: e.matmul(pp[:], brow[0:1, j * P:(j + 1) * P], ones_f[0:1, 0:2], start=False, stop=True),
                         r=[brow, ones_f], w=[pp])
                    S.op("dve", lambda e: e.tensor_copy(modsT[:, :, j], pp[:]), r=[pp], w=[(modsT, j)])
            mflat = modsT[:].rearrange("p v j -> p (v j)")
            nrow = 2 * 6 * KD
            for r0 in range(0, nrow, P):
                n = min(P, nrow - r0)
                pt = S.ps("ptm", [P, P], F32)
                S.op("pe", lambda e: e.transpose(pt[:n, :], mflat[:, r0:r0 + n], ident_f[:]), r=[modsT, ident_f], w=[pt])
                mt = S.sb("mtm", [P, P], F32)
                S.op("dve", lambda e: e.tensor_copy(mt[:n, :], pt[:n, :]), r=[pt], w=[mt])
                S.dma("sp", mods_d[r0:r0 + n, :], mt[:n, :], r=[mt], w_=["mods_d"])
            S.pop()

        def phase_norm(l, tiles, normw, m_sh, m_sc, router):
            S.push()
            gam = {}; bet = {}
            nw = S.sb("nw", [P, D], F32)
            S.dma("sp", nw[:], bc_row(normw[l], D), w_=[nw])
            for v in sorted(set(0 if ti < NTL else 1 for ti in tiles)):
                g = S.sb("gam", [P, D], F32); b = S.sb("bet", [P, D], F32)
                S.dma("sp", g[:], bc_row(mods_v[v, m_sc], D), r=["mods_d"], w_=[g])
                S.dma("act", b[:], bc_row(mods_v[v, m_sh], D), r=["mods_d"], w_=[b])
                S.op("dve", lambda e: e.scalar_tensor_tensor(out=g[:], in0=g[:], scalar=1.0, in1=nw[:], op0=ALU.add, op1=ALU.mult), r=[g, nw], w=[g])
                gam[v] = g; bet[v] = b
            if router:
                wrf = S.sb("wrf", [P, KD, E], F32); wr = S.sb("wr", [P, KD, E], BF16)
                S.dma("sp", wrf[:], w_router[l].rearrange("(k p) e -> p k e", p=P), w_=[wrf])
                S.op("dve", lambda e: e.tensor_copy(wr[:], wrf[:]), r=[wrf], w=[wr])
            xts = [S.sb("xt", [P, D], F32) for _ in range(2)]
            junk = S.sb("junk", [P, D], BF16)
            hbs = [S.sb("hb", [P, D], BF16) for _ in range(2)]
            GN = 2
            hTs = [S.sb("hTt", [P, KD, GN * P], BF16) for _ in range(2)]
            groups = []
            for ti in tiles:
                if groups and len(groups[-1]) < GN and groups[-1][-1] == ti - 1 and (ti < NTL) == (groups[-1][0] < NTL):
                    groups[-1].append(ti)
                else:
                    groups.append([ti])
            gidx = {}
            for gi, grp in enumerate(groups):
                for j, ti in enumerate(grp):
                    gidx[ti] = (gi, j, len(grp))
            pts = [S.ps("ptn", [P, P], BF16) for _ in range(4)]
            plg = S.ps("plg", [P, E], F32)
            ss = S.sb("ss", [P, 1], F32); sd = S.sb("sd", [P, 1], F32); rs = S.sb("rs", [P, 1], F32)
            mx = S.sb("mx", [P, 1], F32); sm = S.sb("sm", [P, 1], F32); ex = S.sb("ex", [P, E], F32)
            def stage1(i, ti):
                v = 0 if ti < NTL else 1
                xt = xts[i % 2]; hb = hbs[i % 2]
                S.dma("sp", xt[:], x_d[ti * P:(ti + 1) * P, :], r=[("x_d", ti)], w_=[xt])
                S.op("act", lambda e: e.activation(junk[:], xt[:], AF.Square, accum_out=ss[:]), r=[xt], w=[junk, ss])
                S.op("act", lambda e: e.activation(sd[:], ss[:], AF.Sqrt, bias=epsc[:], scale=1.0 / D), r=[ss, epsc], w=[sd])
                S.op("dve", lambda e: e.reciprocal(rs[:], sd[:]), r=[sd], w=[rs])
                S.op("dve", lambda e: e.scalar_tensor_tensor(out=xt[:], in0=xt[:], scalar=rs[:, 0:1], in1=gam[v][:], op0=ALU.mult, op1=ALU.mult),
                     r=[xt, rs, gam[v]], w=[xt])
                S.op("dve", lambda e: e.tensor_tensor(out=hb[:], in0=xt[:], in1=bet[v][:], op=ALU.add), r=[xt, bet[v]], w=[hb])
                if router:
                    S.dma("act", htm_d[ti * P:(ti + 1) * P, :], hb[:], r=[hb], w_=[("htm_d", ti)])

            def stage2(i, ti):
                gi, gj, gn = gidx[ti]
                hb = hbs[i % 2]; hTg = hTs[gi % 2]
                hTt = hTg[:, :, gj * P:(gj + 1) * P]
                for k in range(KD):
                    pt = pts[k % 4]
                    S.op("pe", lambda e: e.transpose(pt[:], hb[:, k * P:(k + 1) * P], ident_b[:]), r=[hb, ident_b], w=[pt])
                    if k % 2 == 0:
                        S.op("act", lambda e: e.activation(hTt[:, k, :], pt[:], AF.Copy), r=[pt], w=[(hTg, (gj, k))])
                    else:
                        S.op("dve", lambda e: e.tensor_copy(hTt[:, k, :], pt[:]), r=[pt], w=[(hTg, (gj, k))])
                if gj == gn - 1:
                    t0g = (ti - gn + 1) * P
                    S.dma("sp", hT_d[:, :, t0g:t0g + gn * P], hTg[:, :, 0:gn * P], r=[hTg], w_=[("hT_d", ti)])
                if router:
                    for k in range(KD):
                        S.op("pe", lambda e: e.matmul(plg[:], hTt[:, k, :], wr[:, k, :], start=(k == 0), stop=(k == KD - 1)),
                             r=[hTg, wr], w=[plg], sig=(k == KD - 1))
                    S.op("dve", lambda e: e.tensor_reduce(out=mx[:], in_=plg[:], axis=AX.X, op=ALU.max), r=[plg], w=[mx])
                    S.op("dve", lambda e: e.tensor_scalar(mx[:], mx[:], -1.0, None, op0=ALU.mult), r=[mx], w=[mx])
                    S.op("act", lambda e: e.activation(ex[:], plg[:], AF.Exp, bias=mx[:], scale=1.0, accum_out=sm[:]), r=[plg, mx], w=[ex, sm])
                    S.op("dve", lambda e: e.reciprocal(sm[:], sm[:]), r=[sm], w=[sm])
                    S.op("dve", lambda e: e.tensor_scalar(aff_e[:, :, ti], ex[:], sm[:, 0:1], None, op0=ALU.mult), r=[ex, sm], w=[(aff_e, ti)])

            nt_ = len(tiles)
            for step in range(nt_ + 1):
                if step < nt_:
                    stage1(step, tiles[step])
                if step >= 1:
                    stage2(step - 1, tiles[step - 1])
            S.pop()

        def phase_inproj(l):
            S.push()
            qg = S.sb("qg", [P, 1], F32); kg = S.sb("kg", [P, 1], F32)
            S.dma("sp", qg[:], q_norm[l].rearrange("(p o) -> p o", o=1), w_=[qg])
            S.dma("sp", kg[:], k_norm[l].rearrange("(p o) -> p o", o=1), w_=[kg])
            S.op("dve", lambda e: e.tensor_scalar(qg[:], qg[:], float(128 ** -0.5), None, op0=ALU.mult), r=[qg], w=[qg])
            wsl = S.sb("wsl", [P, KD, SLABC], BF16)
            stg = [S.sb("stg", [P, 1, SLABC], F32) for _ in range(2)]
            hTb = [S.sb("hTb", [P, KD, 512], BF16) for _ in range(2)]
            pf = [S.ps("pf", [P, 512], F32) for _ in range(3)]
            pss = S.ps("pss", [P, 512], F32)
            ptm = [S.ps("ptmm", [P, 512], F32) for _ in range(2)]
            qf = S.sb("qf", [P, 512], F32); sq = S.sb("sq", [P, 512], BF16); rq = S.sb("rq", [P, 512], F32)
            ob = [S.sb("ob", [P, 512], BF16) for _ in range(3)]
            xin_sb = S.sb("xin_sb", [P, 512], F32)
            ot = [S.sb("ot", [P, 384], BF16) for _ in range(2)]
            nblk = (NTOK + 511) // 512
            cnt = 0
            for g in range(NG):
                for kk in range(0, KD):
                    st = stg[kk % 2]
                    S.dma("sp" if kk % 2 == 0 else "act", st[:],
                          w_in[l][kk * P:(kk + 1) * P, g * SLABC:(g + 1) * SLABC].rearrange("(k p) c -> p k c", p=P), w_=[st])
                    S.op("pool", lambda e: e.tensor_copy(wsl[:, kk:kk + 1, :], st[:]), r=[st], w=[(wsl, kk)])
                for tb in range(nblk):
                    t0 = tb * 512; nb = min(512, NTOK - t0)
                    hb = hTb[tb % 2]
                    S.dma("sp", hb[:, :, :nb], hT_d[:, :, t0:t0 + nb], r=["hT_d"], w_=[hb])
                    for j in range(8):
                        pp = pf[j % 3]
                        for k in range(KD):
                            S.op("pe", lambda e: e.matmul(pp[:, :nb], wsl[:, k, j * P:(j + 1) * P], hb[:, k, :nb], start=(k == 0), stop=(k == KD - 1)),
                                 r=[wsl, hb], w=[pp], sig=(k == KD - 1))
                        o = ob[cnt % 3]; cnt += 1
                        if j < 4:
                            gn = qg if j < 2 else kg
                            head = 2 * g + (j % 2)
                            dst = (qT_d if j < 2 else kT_d)[head][:, t0:t0 + nb]
                            S.op("act", lambda e: e.activation(qf[:, :nb], pp[:, :nb], AF.Copy), r=[pp], w=[qf])
                            S.op("act", lambda e: e.activation(sq[:, :nb], pp[:, :nb], AF.Square), r=[pp], w=[sq])
                            S.op("pe", lambda e: e.matmul(pss[:, :nb], ones_b[:], sq[:, :nb], start=True, stop=True), r=[ones_b, sq], w=[pss])
                            S.op("act", lambda e: e.activation(rq[:, :nb], pss[:, :nb], AF.Sqrt, bias=epsc[:], scale=1.0 / 128), r=[pss, epsc], w=[rq])
                            S.op("dve", lambda e: e.reciprocal(rq[:, :nb], rq[:, :nb]), r=[rq], w=[rq])
                            S.op("dve", lambda e: e.scalar_tensor_tensor(out=o[:, :nb], in0=qf[:, :nb], scalar=gn[:, 0:1], in1=rq[:, :nb], op0=ALU.mult, op1=ALU.mult),
                                 r=[qf, gn, rq], w=[o])
                            S.dma("act", dst, o[:, :nb], r=[o], w_=[("qT_d" if j < 2 else "kT_d", head)])
                        elif j == 4:
                            S.op("act", lambda e: e.activation(xin_sb[:, :nb], pp[:, :nb], AF.Copy), r=[pp], w=[xin_sb])
                        elif j == 6:
                            S.op("dve", lambda e: e.tensor_tensor(out=o[:, :nb], in0=pp[:, :nb], in1=xin_sb[:, :nb], op=ALU.mult), r=[pp, xin_sb], w=[o])
                            S.dma("act", zT_d[g][:, t0:t0 + nb], o[:, :nb], r=[o], w_=[("zT_d", g)])
                        else:
                            S.op("act", lambda e: e.activation(o[:, :nb], pp[:, :nb], AF.Copy), r=[pp], w=[o])
                            dd = gbT_d if j == 5 else suT_d
                            S.dma("act", dd[g][:, t0:t0 + nb], o[:, :nb], r=[o], w_=[("dd", g)])
                    for sub in range(nb // P):
                        pt = ptm[sub % 2]; o2 = ot[sub % 2]
                        for k in range(KD):
                            S.op("pe", lambda e: e.matmul(pt[:, 0:384], hb[:, k, sub * P:(sub + 1) * P], wsl[:, k, 1024:1408], start=(k == 0), stop=(k == KD - 1)),
                                 r=[hb, wsl], w=[pt], sig=(k == KD - 1))
                        S.op("dve", lambda e: e.tensor_copy(o2[:], pt[:, 0:384]), r=[pt], w=[o2])
                        r0 = t0 + sub * P
                        S.dma("act", v_d[r0:r0 + P, 2 * g * P:(2 * g + 2) * P], o2[:, 0:256], r=[o2], w_=[("v_d", g)])
                        S.dma("act", sv_d[r0:r0 + P, g * P:(g + 1) * P], o2[:, 256:384], r=[o2], w_=[("sv_d", g)])
            S.pop()

        def phase_attn(l, upd):
            S.push()
            NCH = 4 + NTC
            mk_ = S.sb("mk_", [P, 15 * 64], F32)
            S.dma("sp", mk_[:], maskT, w_=[mk_])
            kT = S.sb("kT", [P, NTOK], BF16); qT = S.sb("qT", [P, NTOK], BF16)
            V = S.sb("V", [P, NT, P], BF16); Vs = S.sb("Vs", [P, NTL - 1, P], BF16)
            TB = S.sb("TB", [P, 15, 64], F32)
            aT = S.sb("aT", [P, NTOK], BF16)
            sp_ = [S.ps("sp_", [P, 8, 64], F32) for _ in range(4)]
            popr = [S.ps("popr", [P, 2, P], F32) for _ in range(4)]
            po = [t[:, 0, :] for t in popr]
            pr = [t[:, 1, :] for t in popr]
            pokey = popr
            sbb = [S.sb("sbb", [P, 4, 64], F32) for _ in range(4)]
            pT = [S.sb("pT", [P, NCH, 64], BF16) for _ in range(4)]
            pT2 = S.sb("pT2", [P, NTC, P], BF16)
            rinv = [S.sb("rinv", [P, P], F32) for _ in range(4)]
            TBf = TB[:].rearrange("p a b -> p (a b)")
            for h in range(H):
                S.dma("sp", kT[:], kT_d[h], r=["kT_d"], w_=[kT])
                S.dma("act", qT[:], qT_d[h], r=["qT_d"], w_=[qT])
                S.dma("sp", V[:], v_d[:, h * P:(h + 1) * P].rearrange("(t p) c -> p t c", p=P), r=["v_d"], w_=[V])
                S.dma("act", Vs[:], v_d[64:64 + (NTL - 1) * P, h * P:(h + 1) * P].rearrange("(t p) c -> p t c", p=P), r=["v_d"], w_=[Vs])
                S.dma("sp", TBf, rpbT[l, h], w_=[TB])
                S.op("dve", lambda e: e.tensor_tensor(out=TBf, in0=TBf, in1=mk_[:], op=ALU.add), r=[TB, mk_], w=[TB])
                def stage_a(r):
                    s = min(max(r - 4, 0), ROWS - 8); off = s - r + 7; tok0 = 64 * s
                    ps_ = sp_[r % 4]; pp = pT[r % 4]; sb_ = sbb[r % 4]
                    qs = qT[:, 64 * r:64 * r + 64]
                    for c in range(NCH):
                        ks = kT[:, tok0 + P * c: tok0 + P * (c + 1)] if c < 4 else kT[:, SEQ + P * (c - 4): SEQ + P * (c - 3)]
                        S.op("pe", lambda e: e.matmul(ps_[:, c, :], ks, qs, start=True, stop=True), r=[kT, qT], w=[ps_], sig=(c == NCH - 1))
                    S.op("dve", lambda e: e.tensor_tensor(out=sb_[:], in0=ps_[:, 0:4, :], in1=TB[:, off:off + 8:2, :], op=ALU.add), r=[ps_, TB], w=[sb_])
                    S.op("act", lambda e: e.activation(pp[:, 0:4, :], sb_[:], AF.Exp), r=[sb_], w=[(pp, 0)])
                    S.op("act", lambda e: e.activation(pp[:, 4:, :], ps_[:, 4:NCH, :], AF.Exp), r=[ps_], w=[(pp, 1)])

                def stage_b(r):
                    s = min(max(r - 4, 0), ROWS - 8)
                    pp = pT[r % 4]; pov = po[r % 4]; prv = pr[r % 4]; ri = rinv[r % 4]
                    for c in range(NCH):
                        if c < 4:
                            vt = V[:, s // 2 + c, :] if s % 2 == 0 else Vs[:, (s - 1) // 2 + c, :]
                        else:
                            vt = V[:, NTL + (c - 4), :]
                        S.op("pe", lambda e: e.matmul(pov[:, 0:64], vt, pp[:, c, :], start=(c == 0), stop=(c == NCH - 1)), r=[V, Vs, pp], w=[pov], sig=(c == NCH - 1))
                    for c in range(NCH):
                        S.op("pe", lambda e: e.matmul(prv[:, 0:64], ones_b[:], pp[:, c, :], start=(c == 0), stop=(c == NCH - 1)), r=[ones_b, pp], w=[prv], sig=(c == NCH - 1))
                    S.op("dve", lambda e: e.reciprocal(ri[:, 0:64], prv[:, 0:64]), r=[prv], w=[ri])
                    S.op("dve", lambda e: e.tensor_tensor(out=aT[:, 64 * r:64 * r + 64], in0=pov[:, 0:64], in1=ri[:, 0:64], op=ALU.mult), r=[pov, ri], w=[(aT, r)])

                LAG = 2
                for step in range(ROWS + LAG):
                    if step < ROWS:
                        stage_a(step)
                    if step >= LAG:
                        stage_b(step - LAG)
                nq = SEQ
                if upd:
                    nq = NTOK
                    for qb in range(NTC):
                        ps_ = sp_[qb % 2]; pov = po[qb % 2]; prv = pr[qb % 2]; ri = rinv[qb % 2]
                        psv = ps_[:].rearrange("p a b -> p (a b)")
                        qs = qT[:, SEQ + P * qb: SEQ + P * (qb + 1)]
                        for c in range(NTC):
                            S.op("pe", lambda e: e.matmul(psv[:, c * P:(c + 1) * P], kT[:, SEQ + P * c: SEQ + P * (c + 1)], qs, start=True, stop=True),
                                 r=[kT, qT], w=[ps_], sig=(c == NTC - 1))
                        S.op("act", lambda e: e.activation(pT2[:].rearrange("p a b -> p (a b)"), psv[:, 0:NTC * P], AF.Exp), r=[ps_], w=[pT2])
                        for c in range(NTC):
                            S.op("pe", lambda e: e.matmul(pov[:], V[:, NTL + c, :], pT2[:, c, :], start=(c == 0), stop=(c == NTC - 1)), r=[V, pT2], w=[pov], sig=(c == NTC - 1))
                        for c in range(NTC):
                            S.op("pe", lambda e: e.matmul(prv[:], ones_b[:], pT2[:, c, :], start=(c == 0), stop=(c == NTC - 1)), r=[ones_b, pT2], w=[prv], sig=(c == NTC - 1))
                        S.op("dve", lambda e: e.reciprocal(ri[:], prv[:]), r=[prv], w=[ri])
                        S.op("dve", lambda e: e.tensor_tensor(out=aT[:, SEQ + P * qb: SEQ + P * (qb + 1)], in0=pov[:], in1=ri[:], op=ALU.mult), r=[pov, ri], w=[(aT, 1000 + qb)])
                S.dma("sp", mT_d[:, h, 0:nq], aT[:, 0:nq], r=[aT], w_=[("mT_d", h)])
            S.pop()

        def phase_conv_sg(l, upd):
            S.push()
            z = S.sb("z", [P, NTOK], BF16); gb = S.sb("gb", [P, NTOK], BF16)
            acc = S.sb("acc", [P, NTOK], F32); y = S.sb("y", [P, NTOK], BF16)
            cw = S.sb("cw", [P, 3], F32)
            seqs = [(0, SEQ)] + ([(SEQ, CTX)] if upd else [])
            nq = NTOK if upd else SEQ
            for g in range(NG):
                S.dma("sp", z[:], zT_d[g], r=["zT_d"], w_=[z])
                S.dma("act", gb[:], gbT_d[g], r=["gbT_d"], w_=[gb])
                S.dma("sp", cw[:], conv_w[l, g], w_=[cw])
                for (a, n) in seqs:
                    S.op("dve", lambda e: e.tensor_scalar(acc[:, a:a + n], z[:, a:a + n], cw[:, 1:2], None, op0=ALU.mult), r=[z, cw], w=[acc])
                    S.op("dve", lambda e: e.scalar_tensor_tensor(out=acc[:, a + 1:a + n], in0=z[:, a:a + n - 1], scalar=cw[:, 0:1], in1=acc[:, a + 1:a + n], op0=ALU.mult, op1=ALU.add),
                         r=[z, cw, acc], w=[acc])
                    S.op("dve", lambda e: e.scalar_tensor_tensor(out=acc[:, a:a + n - 1], in0=z[:, a + 1:a + n], scalar=cw[:, 2:3], in1=acc[:, a:a + n - 1], op0=ALU.mult, op1=ALU.add),
                         r=[z, cw, acc], w=[acc])
                    S.op("dve", lambda e: e.tensor_tensor(out=y[:, a:a + n], in0=acc[:, a:a + n], in1=gb[:, a:a + n], op=ALU.mult), r=[acc, gb], w=[y])
                S.dma("sp", mT_d[:, H + g, 0:nq], y[:, 0:nq], r=[y], w_=[("mT_d", H + g)])
            S.pop()
            S.push()
            sgn = S.sb("sgn", [P, SGW], F32)
            S.dma("sp", sgn[:], bc_row(sg_norm[l], SGW), w_=[sgn])
            swf = S.sb("swf", [P, NG * P], F32); sw = S.sb("sw", [P, NG * P], BF16)
            S.dma("sp", swf[:], sg_wT[l], w_=[swf])
            S.op("dve", lambda e: e.tensor_copy(sw[:], swf[:]), r=[swf], w=[sw])
            sgb = S.sb("sgb", [P, NG * P], F32)
            S.dma("sp", sgb[:], bc_row(sg_b[l], NG * P), w_=[sgb])
            svt = [S.sb("svt", [P, SGW], BF16) for _ in range(2)]
            sut = [S.sb("sut", [P, NG, P], BF16) for _ in range(2)]
            vn = [S.sb("vn", [P, SGW], BF16) for _ in range(2)]
            junk = S.sb("junk2", [P, SGW], BF16)
            ss = S.sb("ss2", [P, 1], F32); sd = S.sb("sd2", [P, 1], F32); rs = S.sb("rs2", [P, 1], F32)
            pmx = [S.ps("pmx", [P, P], F32) for _ in range(2)]
            tmp = [S.sb("tmp", [P, P], F32) for _ in range(2)]
            og = [S.sb("og", [P, NG, P], BF16) for _ in range(2)]
            tiles = list(range(NTL)) + (list(range(NTL, NT)) if upd else [])
            for i, ti in enumerate(tiles):
                sv = svt[i % 2]; su = sut[i % 2]; vv = vn[i % 2]; o = og[i % 2]
                S.dma("sp", sv[:], sv_d[ti * P:(ti + 1) * P, :], r=["sv_d"], w_=[sv])
                S.dma("act", su[:], suT_d[:, :, ti * P:(ti + 1) * P].rearrange("g p t -> p g t"), r=["suT_d"], w_=[su])
                S.op("act", lambda e: e.activation(junk[:], sv[:], AF.Square, accum_out=ss[:]), r=[sv], w=[junk, ss])
                S.op("act", lambda e: e.activation(sd[:], ss[:], AF.Sqrt, bias=epsc[:], scale=1.0 / SGW), r=[ss, epsc], w=[sd])
                S.op("dve", lambda e: e.reciprocal(rs[:], sd[:]), r=[sd], w=[rs])
                S.op("dve", lambda e: e.scalar_tensor_tensor(out=vv[:], in0=sv[:], scalar=rs[:, 0:1], in1=sgn[:], op0=ALU.mult, op1=ALU.mult), r=[sv, rs, sgn], w=[vv])
                for g in range(NG):
                    pm = pmx[g % 2]; tp = tmp[g % 2]
                    S.op("pe", lambda e: e.matmul(pm[:], vv[:, g * P:(g + 1) * P], sw[:, g * P:(g + 1) * P], start=True, stop=True), r=[vv, sw], w=[pm])
                    S.op("dve", lambda e: e.tensor_tensor(out=tp[:], in0=pm[:], in1=sgb[:, g * P:(g + 1) * P], op=ALU.add), r=[pm, sgb], w=[tp])
                    S.op("dve", lambda e: e.tensor_tensor(out=o[:, g, :], in0=tp[:], in1=su[:, g, :], op=ALU.mult), r=[tp, su], w=[(o, g)])
                S.dma("sp", mT_d[:, H + NG:H + 2 * NG, ti * P:(ti + 1) * P], o[:], r=[o], w_=[("mT_d", 5000 + ti)])
            S.pop()

        def phase_outproj(l, upd):
            S.push()
            OC = min(1024, D)
            wo = S.sb("wo", [P, KM, OC], BF16)
            stg = [S.sb("stgo", [P, 2, OC], F32) for _ in range(2)]
            g1 = {v: S.sb("g1", [P, OC], F32) for v in ((0, 1) if upd else (0,))}
            mts = [S.sb("mt", [P, KM, 4 * P], BF16) for _ in range(2)]
            xts = [S.sb("xto", [P, OC], F32) for _ in range(2)]
            xos = [S.sb("xoo", [P, OC], F32) for _ in range(2)]
            pps = [S.ps("ppo", [P, 512], F32) for _ in range(2)]
            tiles = list(range(NTL)) + (list(range(NTL, NT)) if upd else [])
            for cs in range(D // OC):
                for kk in range(0, KM, 2):
                    st = stg[(kk // 2) % 2]
                    S.dma("sp" if (kk // 2) % 2 == 0 else "act", st[:], w_out[l][kk * P:(kk + 2) * P, cs * OC:(cs + 1) * OC].rearrange("(k p) c -> p k c", p=P), w_=[st])
                    S.op("pool", lambda e: e.tensor_copy(wo[:, kk:kk + 2, :], st[:]), r=[st], w=[(wo, kk)])
                for v in g1:
                    S.dma("sp", g1[v][:], bc_row(mods_v[v, 2, cs * OC:(cs + 1) * OC], OC), r=["mods_d"], w_=[g1[v]])
                for i, ti in enumerate(tiles):
                    v = 0 if ti < NTL else 1
                    xt = xts[i % 2]; xo = xos[i % 2]
                    if ti < NTL:
                        g0 = (ti // 4) * 4; gn = min(4, NTL - g0)
                    else:
                        g0 = NTL + ((ti - NTL) // 4) * 4; gn = min(4, NT - g0)
                    mtg = mts[(g0 // 4) % 2] if ti < NTL else mts[((NTL + 3) // 4 + (ti - NTL) // 4) % 2]
                    if ti == g0:
                        S.dma("sp", mtg[:, :, 0:gn * P], mT_d[:, :, g0 * P:(g0 + gn) * P], r=["mT_d"], w_=[mtg])
                    mt = mtg[:, :, (ti - g0) * P:(ti - g0 + 1) * P]
                    S.dma("act", xt[:], x_d[ti * P:(ti + 1) * P, cs * OC:(cs + 1) * OC], r=[("x_d", ti)], w_=[xt])
                    for hf in range(OC // 512):
                        pp = pps[hf % 2]
                        for k in range(KM):
                            S.op("pe", lambda e: e.matmul(pp[:], mt[:, k, :], wo[:, k, hf * 512:(hf + 1) * 512], start=(k == 0), stop=(k == KM - 1)), r=[mtg, wo], w=[pp], sig=(k == KM - 1))
                        S.op("dve", lambda e: e.tensor_tensor(out=xo[:, hf * 512:(hf + 1) * 512], in0=pp[:], in1=g1[v][:, hf * 512:(hf + 1) * 512], op=ALU.mult), r=[pp, g1[v]], w=[(xo, hf)])
                    S.op("dve", lambda e: e.tensor_tensor(out=xo[:], in0=xo[:], in1=xt[:], op=ALU.add), r=[xo, xt], w=[xo])
                    S.dma("sp", x_d[ti * P:(ti + 1) * P, cs * OC:(cs + 1) * OC], xo[:], r=[xo], w_=[("x_d", ti)])
            S.pop()

        def phase_topk(upd):
            S.push()
            sets = [(0, NTL, capL, 0)] + ([(NTL, NT, capC, capL)] if upd else [])
            zt = S.sb("zt", [P, D], F32)
            S.op("pool", lambda e: e.memset(zt[:], 0.0), w=[zt])
            for ti in range(NT + 1):
                S.dma("sp" if ti % 2 == 0 else "act", M_d[ti * P:(ti + 1) * P, :], zt[:], r=[zt], w_=["M_d"])
            ztb = S.sb("ztb", [P, D], BF16)
            S.op("pool", lambda e: e.memset(ztb[:], 0.0), w=[ztb])
            S.dma("sp", htm_d[NTOK:NTOK + P, :], ztb[:], r=[ztb], w_=["htm_d"])
            lo = S.sb("lo", [P, E], F32); hi = S.sb("hi", [P, E], F32); mid = S.sb("mid", [P, E], F32)
            cnp = S.sb("cnp", [P, E], F32); ge = S.sb("ge", [P, E], F32); d1 = S.sb("d1", [P, E], F32); d2 = S.sb("d2", [P, E], F32)
            cmpj = S.sb("cmpj", [P, NT], F32)
            pq = [S.ps("pq", [P, 512], F32) for _ in range(2)]
            pct = S.ps("pct", [P, E], F32)
            for (a, b, cap, base) in sets:
                S.op("dve", lambda e: e.memset(lo[:], 0.0), w=[lo])
                S.op("dve", lambda e: e.memset(hi[:], 1.0), w=[hi])
                for it in range(30):
                    S.op("dve", lambda e: e.tensor_tensor(out=mid[:], in0=lo[:], in1=hi[:], op=ALU.add), r=[lo, hi], w=[mid])
                    S.op("dve", lambda e: e.tensor_scalar(mid[:], mid[:], 0.5, None, op0=ALU.mult), r=[mid], w=[mid])
                    for ex in range(E):
                        S.op("dve", lambda e: e.tensor_scalar(cmpj[:, a:b], aff_e[:, ex, a:b], mid[:, ex:ex + 1], 0.0, op0=ALU.is_ge, op1=ALU.add, accum_out=cnp[:, ex:ex + 1]),
                             r=[aff_e, mid], w=[cmpj, (cnp, ex)])
                    S.op("pe", lambda e: e.matmul(pct[:], ones_f[:], cnp[:], start=True, stop=True), r=[ones_f, cnp], w=[pct])
                    S.op("dve", lambda e: e.tensor_scalar(ge[:], pct[:], float(cap) - 0.5, None, op0=ALU.is_ge), r=[pct], w=[ge])
                    S.op("dve", lambda e: e.tensor_tensor(out=d1[:], in0=mid[:], in1=lo[:], op=ALU.subtract), r=[mid, lo], w=[d1])
                    S.op("dve", lambda e: e.tensor_tensor(out=d2[:], in0=hi[:], in1=mid[:], op=ALU.subtract), r=[mid, hi], w=[d2])
                    S.op("dve", lambda e: e.tensor_tensor(out=d1[:], in0=d1[:], in1=ge[:], op=ALU.mult), r=[d1, ge], w=[d1])
                    S.op("dve", lambda e: e.tensor_tensor(out=d2[:], in0=d2[:], in1=ge[:], op=ALU.mult), r=[d2, ge], w=[d2])
                    S.op("dve", lambda e: e.tensor_tensor(out=lo[:], in0=lo[:], in1=d1[:], op=ALU.add), r=[lo, d1], w=[lo])
                    S.op("dve", lambda e: e.tensor_tensor(out=hi[:], in0=mid[:], in1=d2[:], op=ALU.add), r=[mid, d2], w=[hi])
                for ex in range(E):
                    S.op("dve", lambda e: e.tensor_scalar(msk[:, ex, a:b], aff_e[:, ex, a:b], lo[:, ex:ex + 1], None, op0=ALU.is_ge), r=[aff_e, lo], w=[(msk, (ex, a))])
            S.op("dve", lambda e: e.tensor_tensor(out=gate[:], in0=msk[:], in1=aff_e[:], op=ALU.mult), r=[msk, aff_e], w=[gate])
            mflat = msk[:].rearrange("p e t -> p (e t)")
            pw = S.sb("pw", [P, E, NT], F32); tot = S.sb("tot", [P, E, NT], F32); tb2 = S.sb("tb2", [P, E, NT], F32)
            pwf = pw[:].rearrange("p e t -> p (e t)"); totf = tot[:].rearrange("p e t -> p (e t)")
            n = E * NT
            for c0 in range(0, n, 512):
                m = min(512, n - c0)
                S.op("pe", lambda e: e.matmul(pq[0][:, :m], ustr[:], mflat[:, c0:c0 + m], start=True, stop=True), r=[ustr, msk], w=[pq[0]])
                S.op("dve", lambda e: e.tensor_copy(pwf[:, c0:c0 + m], pq[0][:, :m]), r=[pq[0]], w=[pw])
                S.op("pe", lambda e: e.matmul(pq[1][:, :m], ones_f[:], mflat[:, c0:c0 + m], start=True, stop=True), r=[ones_f, msk], w=[pq[1]])
                S.op("dve", lambda e: e.tensor_copy(totf[:, c0:c0 + m], pq[1][:, :m]), r=[pq[1]], w=[tot])
            for (a, b, cap, base) in sets:
                nxt = tb2
                src0 = S.sb("src0", [P, E, NT], F32)
                S.op("dve", lambda e: e.tensor_copy(src0[:, :, a:b], tot[:, :, a:b]), r=[tot], w=[src0])
                cur = src0
                s = 1
                while s < (b - a):
                    S.op("dve", lambda e: e.tensor_tensor(out=nxt[:, :, a + s:b], in0=cur[:, :, a + s:b], in1=cur[:, :, a:b - s], op=ALU.add), r=[cur], w=[nxt])
                    S.op("dve", lambda e: e.tensor_copy(nxt[:, :, a:a + s], cur[:, :, a:a + s]), r=[cur], w=[nxt])
                    cur, nxt = nxt, cur
                    s *= 2
                S.op("dve", lambda e: e.tensor_tensor(out=cur[:, :, a:b], in0=cur[:, :, a:b], in1=tot[:, :, a:b], op=ALU.subtract), r=[cur, tot], w=[cur])
                S.op("dve", lambda e: e.tensor_tensor(out=pw[:, :, a:b], in0=pw[:, :, a:b], in1=cur[:, :, a:b], op=ALU.add), r=[pw, cur], w=[pw])
                if base:
                    S.op("dve", lambda e: e.tensor_scalar(pw[:, :, a:b], pw[:, :, a:b], float(base), None, op0=ALU.add), r=[pw], w=[pw])
            hiT = NT if upd else NTL
            S.op("dve", lambda e: e.scalar_tensor_tensor(out=posm[:, :, 0:hiT], in0=pw[:, :, 0:hiT], scalar=1.0, in1=msk[:, :, 0:hiT], op0=ALU.add, op1=ALU.mult), r=[pw, msk], w=[posm])
            S.op("dve", lambda e: e.tensor_scalar(posm[:, :, 0:hiT], posm[:, :, 0:hiT], -1.0, None, op0=ALU.add), r=[posm], w=[posm])
            S.pop()

        def phase_moe(l, upd):
            S.push()
            nslots = capL + (capC if upd else 0)
            NSB = (nslots + P - 1) // P
            NS = NSB * P
            tiles = list(range(NTL)) + (list(range(NTL, NT)) if upd else [])
            DH = min(2048, D) if D > 1024 else D // 2
            NDH = D // DH
            M2 = M_d.rearrange("t (h d) -> (t h) d", d=DH)
            iot = S.sb("iot", [P, NS], F32)
            S.op("pool", lambda e: e.iota(iot[:], pattern=[[1, NS]], base=0, channel_multiplier=0, allow_small_or_imprecise_dtypes=True), w=[iot])
            pg = S.ps("pg", [P, 512], F32); pu = S.ps("pu", [P, 512], F32)
            py = [S.ps("py", [P, 512], F32) for _ in range(2)]
            ptr = [S.ps("ptr", [P, P], BF16) for _ in range(2)]
            pi = S.ps("pi", [P, 8], F32)
            pis = S.sb("pis", [P, 8], F32)
            pcol = S.sb("pcol", [P, 1], F32); pbase = S.sb("pbase", [P, 1], F32)
            S.op("pe", lambda e: e.matmul(pi[:, 0:1], ustr[:], ones_f[:, 0:1], start=True, stop=True), r=[ustr, ones_f], w=[pi])
            S.op("dve", lambda e: e.tensor_copy(pcol[:], pi[:, 0:1]), r=[pi], w=[pcol])
            S.op("dve", lambda e: e.tensor_scalar(pbase[:], pcol[:], float(NTOK), None, op0=ALU.add), r=[pcol], w=[pbase])
            rv5 = S.sb("rv5", [P, E, NT, 5], BF16)
            S.push()
            r3 = S.sb("r3", [P, NT, 3], F32)
            gtmp = S.sb("gtmp", [P, E, NT], F32)
            for ti in range(NT):
                S.op("pool", lambda e: e.memset(r3[:, ti, 0:1], float(ti)), w=[(r3, (ti, 0))])
                S.op("dve", lambda e: e.tensor_copy(r3[:, ti, 1:2], pcol[:]), r=[pcol], w=[(r3, (ti, 1))])
                S.op("pool", lambda e: e.memset(r3[:, ti, 2:3], 1.0), w=[(r3, (ti, 2))])
            for ex in range(E):
                S.op("dve", lambda e: e.tensor_copy(rv5[:, ex, :, 0:3], r3[:]), r=[r3], w=[(rv5, ("c", ex))])
            S.op("dve", lambda e: e.tensor_copy(rv5[:, :, :, 3], gate[:]), r=[gate], w=[(rv5, "hi")])
            S.op("dve", lambda e: e.tensor_copy(gtmp[:], rv5[:, :, :, 3]), r=[rv5], w=[gtmp])
            S.op("dve", lambda e: e.tensor_tensor(out=gtmp[:], in0=gate[:], in1=gtmp[:], op=ALU.subtract), r=[gate, gtmp], w=[gtmp])
            S.op("dve", lambda e: e.tensor_copy(rv5[:, :, :, 4], gtmp[:]), r=[gtmp], w=[(rv5, "lo")])
            S.pop()
            oh = [S.sb("oh", [P, P], BF16) for _ in range(3)]
            sif = S.sb("sif", [P, 1], F32); dif = S.sb("dif", [P, 1], F32)
            sidx = [S.sb("sidx", [P, 1], I32) for _ in range(2)]
            si2 = [[S.sb("si2", [P, 1], I32) for _ in range(NDH)] for _ in range(NSB)]
            gs = [S.sb("gs", [P, 1], F32) for _ in range(NSB)]
            hg = [S.sb("hg", [P, D], BF16) for _ in range(1)]
            hsT = S.sb("hsT", [P, KD, NS], BF16)
            FH = 128
            SZ = KD * 2 * FH
            wbuf = S.sb("wbuf", [P, max(2 * SZ, FC * DH)], BF16)
            wgus = [wbuf[:, sl * SZ:(sl + 1) * SZ].rearrange("p (k w f) -> p k w f", k=KD, w=2) for sl in range(2)]
            wd = wbuf[:, 0:FC * DH].rearrange("p (f d) -> p f d", f=FC)
            wd_slots = lambda f: list(range((f * DH) // SZ, min(1, ((f + 1) * DH - 1) // SZ) + 1))
            KS = min(4, KD)
            stg = [S.sb("stgm", [P, KS, FH], F32) for _ in range(2)]
            gc = 0
            aTt = S.sb("aTt", [P, FC, NS], BF16)
            sgt = S.sb("sgt", [P, 512], F32)
            DS = min(1024, DH)
            stgd = [S.sb("stgd", [P, DS], F32) for _ in range(2)]
            xg = [S.sb("xg", [P, DH], F32) for _ in range(2)]
            nst = [0]; gcn = [0]
            hg2 = [hg[0], S.sb("hgB", [P, D], BF16)]
            si2p = [si2, [[S.sb("si2b", [P, 1], I32) for _ in range(NDH)] for _ in range(NSB)]]
            gsp = [gs, [S.sb("gsb", [P, 1], F32) for _ in range(NSB)]]
            sidxp = [[S.sb("sidxa", [P, 1], I32) for _ in range(NSB)], [S.sb("sidxb", [P, 1], I32) for _ in range(NSB)]]

            def a_idx(ex, sb):
                par = ex % 2
                for i, ti in enumerate(tiles):
                    o = oh[i % 3]
                    S.op("dve", lambda e: e.tensor_scalar(o[:], iot[:, sb * P:(sb + 1) * P], posm[:, ex, ti:ti + 1], None, op0=ALU.is_equal), r=[iot, posm], w=[o])
                    S.op("pe", lambda e: e.matmul(pi[:, 0:5], o[:], rv5[:, ex, ti, :], start=(i == 0), stop=(i == len(tiles) - 1)), r=[o, rv5], w=[pi])
                si = sidxp[par][sb]; hgt = hg2[sb % 2]
                S.op("dve", lambda e: e.tensor_copy(pis[:, 0:5], pi[:, 0:5]), r=[pi], w=[pis])
                S.op("dve", lambda e: e.scalar_tensor_tensor(out=sif[:], in0=pis[:, 0:1], scalar=128.0, in1=pis[:, 1:2], op0=ALU.mult, op1=ALU.add), r=[pis], w=[sif])
                S.op("dve", lambda e: e.tensor_tensor(out=dif[:], in0=sif[:], in1=pbase[:], op=ALU.subtract), r=[sif, pbase], w=[dif])
                S.op("dve", lambda e: e.scalar_tensor_tensor(out=sif[:], in0=dif[:], scalar=pis[:, 2:3], in1=pbase[:], op0=ALU.mult, op1=ALU.add), r=[dif, pis, pbase], w=[sif])
                S.op("dve", lambda e: e.tensor_copy(si[:], sif[:]), r=[sif], w=[si])
                for dh in range(NDH):
                    S.op("dve", lambda e: e.tensor_scalar(si2p[par][sb][dh][:], sif[:], float(NDH), float(dh), op0=ALU.mult, op1=ALU.add), r=[sif], w=[si2p[par][sb][dh]])
                S.op("dve", lambda e: e.tensor_tensor(out=gsp[par][sb][:], in0=pis[:, 3:4], in1=pis[:, 4:5], op=ALU.add), r=[pis], w=[gsp[par][sb]])
                S.dma_custom("pool", lambda e: e.indirect_dma_start(out=hgt[:], out_offset=None, in_=htm_d, in_offset=bass.IndirectOffsetOnAxis(ap=si[:, 0:1], axis=0)),
                             r=[si, "htm_d"], w_=[hgt])

            def a_tr(ex, sb):
                hgt = hg2[sb % 2]
                for k in range(KD):
                    pt = ptr[k % 2]
                    S.op("pe", lambda e: e.transpose(pt[:], hgt[:, k * P:(k + 1) * P], ident_b[:]), r=[hgt, ident_b], w=[pt])
                    if k % 2 == 0:
                        S.op("act", lambda e: e.activation(hsT[:, k, sb * P:(sb + 1) * P], pt[:], AF.Copy), r=[pt], w=[(hsT, (k, sb))])
                    else:
                        S.op("dve", lambda e: e.tensor_copy(hsT[:, k, sb * P:(sb + 1) * P], pt[:]), r=[pt], w=[(hsT, (k, sb))])

            def gate_up(ex):
                for fc in range(FF // FH):
                    sl = gcn[0] % 2; gcn[0] += 1
                    wgu = wgus[sl]
                    for which, wsrc in ((0, w_gate), (1, w_up)):
                        for kk in range(0, KD, KS):
                            st = stg[nst[0] % 2]; nst[0] += 1
                            S.dma("sp", st[:], wsrc[l, ex][kk * P:(kk + KS) * P, fc * FH:(fc + 1) * FH].rearrange("(k p) c -> p k c", p=P), w_=[st])
                            S.op("pool", lambda e: e.tensor_copy(wgu[:, kk:kk + KS, which, :], st[:]), r=[st], w=[(wbuf, sl)])
                    for s0 in range(0, NS, 512):
                        ns = min(512, NS - s0)
                        for k in range(KD):
                            S.op("pe", lambda e: e.matmul(pg[:, :ns], wgu[:, k, 0, :], hsT[:, k, s0:s0 + ns], start=(k == 0), stop=(k == KD - 1)), r=[(wbuf, sl), hsT], w=[pg], sig=(k == KD - 1))
                        for k in range(KD):
                            S.op("pe", lambda e: e.matmul(pu[:, :ns], wgu[:, k, 1, :], hsT[:, k, s0:s0 + ns], start=(k == 0), stop=(k == KD - 1)), r=[(wbuf, sl), hsT], w=[pu], sig=(k == KD - 1))
                        S.op("act", lambda e: e.activation(sgt[:, :ns], pg[:, :ns], AF.Silu), r=[pg], w=[sgt])
                        S.op("dve", lambda e: e.tensor_tensor(out=aTt[:, fc, s0:s0 + ns], in0=sgt[:, :ns], in1=pu[:, :ns], op=ALU.mult), r=[sgt, pu], w=[(aTt, (fc, s0))])

            def load_wd(ex, dh):
                for f in range(FC):
                    for d0 in range(0, DH, DS):
                        st = stgd[nst[0] % 2]; nst[0] += 1
                        S.dma("sp", st[:], w_down[l, ex][f * P:(f + 1) * P, dh * DH + d0: dh * DH + d0 + DS], w_=[st])
                        S.op("pool", lambda e: e.tensor_copy(wd[:, f, d0:d0 + DS], st[:]), r=[st], w=[(wbuf, sl_) for sl_ in wd_slots(f)])

            def gather_x(ex, dh, sb):
                x_ = xg[sb % 2]; ixt = si2p[ex % 2][sb][dh]
                prev_keys = [("M_d", (dh, (ex - 1) % 2, sbp)) for sbp in range(NSB)]
                S.dma_custom("pool", lambda e: e.indirect_dma_start(out=x_[:], out_offset=None, in_=M2, in_offset=bass.IndirectOffsetOnAxis(ap=ixt[:, 0:1], axis=0)),
                             r=[ixt] + prev_keys, w_=[x_])

            def down_block(ex, dh, sb):
                par = ex % 2
                if sb == 0:
                    load_wd(ex, dh)
                    gather_x(ex, dh, 0)
                if sb + 1 < NSB:
                    gather_x(ex, dh, sb + 1)
                x_ = xg[sb % 2]; ixt = si2p[par][sb][dh]
                for cb in range(DH // 512):
                    pp = py[cb % 2]
                    for f in range(FC):
                        S.op("pe", lambda e: e.matmul(pp[:], aTt[:, f, sb * P:(sb + 1) * P], wd[:, f, cb * 512:(cb + 1) * 512], start=(f == 0), stop=(f == FC - 1)), r=[aTt, (wbuf, 0), (wbuf, 1)], w=[pp], sig=(f == FC - 1))
                    S.op("dve", lambda e: e.scalar_tensor_tensor(out=x_[:, cb * 512:(cb + 1) * 512], in0=pp[:], scalar=gsp[par][sb][:, 0:1], in1=x_[:, cb * 512:(cb + 1) * 512], op0=ALU.mult, op1=ALU.add),
                         r=[pp, gsp[par][sb], x_], w=[x_])
                S.dma_custom("pool", lambda e: e.indirect_dma_start(out=M2, out_offset=bass.IndirectOffsetOnAxis(ap=ixt[:, 0:1], axis=0), in_=x_[:], in_offset=None),
                             r=[ixt, x_], w_=[("M_d", (dh, par, sb))])

            for sb in range(NSB):
                a_idx(0, sb)
                if sb >= 1:
                    a_tr(0, sb - 1)
            a_tr(0, NSB - 1)
            for ex in range(E):
                gate_up(ex)
                blocks = [(dh, sb) for dh in range(NDH) for sb in range(NSB)]
                nxt = ex + 1 < E
                ia = 0; it_ = 0
                for bi, (dh, sb) in enumerate(blocks):
                    if nxt and ia < NSB and (bi * NSB) // len(blocks) >= ia:
                        a_idx(ex + 1, ia); ia += 1
                    down_block(ex, dh, sb)
                    if nxt and it_ < ia - 1:
                        a_tr(ex + 1, it_); it_ += 1
                if nxt:
                    while ia < NSB:
                        a_idx(ex + 1, ia); ia += 1
                    while it_ < NSB:
                        a_tr(ex + 1, it_); it_ += 1
            S.pop()

        def phase_combine(l, upd, last):
            S.push()
            tiles = list(range(NTL)) + (list(range(NTL, NT)) if upd else [])
            g2 = {v: S.sb("g2", [P, D], F32) for v in ((0, 1) if upd else (0,))}
            for v in g2:
                S.dma("sp", g2[v][:], bc_row(mods_v[v, 5], D), r=["mods_d"], w_=[g2[v]])
            xts = [S.sb("xtc", [P, D], F32) for _ in range(2)]
            mts = [S.sb("mtc", [P, D], F32) for _ in range(2)]
            for i, ti in enumerate(tiles):
                v = 0 if ti < NTL else 1
                xt = xts[i % 2]; mt = mts[i % 2]
                S.dma("sp", xt[:], x_d[ti * P:(ti + 1) * P, :], r=[("x_d", ti)], w_=[xt])
                S.dma("act", mt[:], M_d[ti * P:(ti + 1) * P, :], r=["M_d"], w_=[mt])
                S.op("dve", lambda e: e.tensor_tensor(out=mt[:], in0=mt[:], in1=g2[v][:], op=ALU.mult), r=[mt, g2[v]], w=[mt])
                S.op("pool", lambda e: e.tensor_tensor(out=mt[:], in0=mt[:], in1=xt[:], op=ALU.add), r=[mt, xt], w=[mt])
                if last:
                    S.dma("act", out[ti * P:(ti + 1) * P, :], mt[:], r=[mt], w_=[("out", ti)], out_dram=True)
                else:
                    S.dma("act", x_d[ti * P:(ti + 1) * P, :], mt[:], r=[mt], w_=[("x_d", ti)])
            S.pop()

        all_tiles = list(range(NT))
        lat_tiles = list(range(NTL))
        stop_after = cfg.get("stop_after")
        plist = []
        for l in range(DEPTH):
            upd = l < DEPTH - 1
            plist += [lambda l=l: phase_ada(l),
                      lambda l=l: phase_norm(l, all_tiles, norm1, 0, 1, router=False),
                      lambda l=l: phase_inproj(l),
                      lambda l=l, upd=upd: phase_attn(l, upd),
                      lambda l=l, upd=upd: phase_conv_sg(l, upd),
                      lambda l=l, upd=upd: phase_outproj(l, upd),
                      lambda l=l, upd=upd: phase_norm(l, all_tiles if upd else lat_tiles, norm2, 3, 4, router=True),
                      lambda l=l, upd=upd: phase_topk(upd),
                      lambda l=l, upd=upd: phase_moe(l, upd),
                      lambda l=l, upd=upd: phase_combine(l, upd, last=(l == DEPTH - 1))]
        import os as _os
        nstop = int(_os.environ.get("MK_STOP", "1000"))
        for i, ph in enumerate(plist):
            if i >= nstop:
                break
            ph()
        S.finish()
    cfg["ninst"] = S.ninst; cfg["nwait"] = S.nwait; cfg["nsem"] = getattr(S, "nsem", 0)
    return nc


def host_prep(cfg, inputs, b):
    D, SEQ, CTX, E, FF, DEPTH, H, NG, KD = (cfg[k] for k in ("D", "SEQ", "CTX", "E", "FF", "DEPTH", "H", "NG", "KD"))
    f = lambda a: np.ascontiguousarray(np.asarray(a, dtype=np.float32))
    NAW = H * 128; CW = NG * 128
    m = {}
    m["x"] = f(inputs["x"][b]); m["ctx"] = f(inputs["ctx"][b])
    m["cvec"] = f(np.stack([np.asarray(inputs["c"])[b], np.asarray(inputs["c_ctx"])], 0).reshape(2 * KD, 128))
    m["w_ada"] = f(inputs["w_ada"]); m["b_ada"] = f(np.asarray(inputs["b_ada"]).reshape(DEPTH, 1, 6 * D))
    m["norm1"] = f(inputs["norm1"]); m["norm2"] = f(inputs["norm2"])
    w_in = np.asarray(inputs["w_in"])
    cols = []
    for g in range(NG):
        for base in (0, NAW):
            for hh in (2 * g, 2 * g + 1):
                cols.append(np.arange(base + hh * 128, base + (hh + 1) * 128))
        o = 3 * NAW
        for j in range(3):
            cols.append(np.arange(o + j * CW + g * 128, o + j * CW + (g + 1) * 128))
        o2 = 3 * NAW + 3 * CW
        cols.append(np.arange(o2 + g * 128, o2 + (g + 1) * 128))
        for hh in (2 * g, 2 * g + 1):
            cols.append(np.arange(2 * NAW + hh * 128, 2 * NAW + (hh + 1) * 128))
        cols.append(np.arange(o2 + CW + g * 128, o2 + CW + (g + 1) * 128))
    cols = np.concatenate(cols)
    m["w_in"] = f(w_in[:, :, cols])
    m["q_norm"] = f(inputs["q_norm"]); m["k_norm"] = f(inputs["k_norm"])
    rpb = np.asarray(inputs["rpb"])
    ck = np.arange(64)[:, None]; cq = np.arange(64)[None, :]
    dc = np.clip(ck - cq + 15, 0, 30)
    t = rpb[:, :, :, dc]
    top = t.transpose(0, 1, 3, 2, 4)
    bot = np.concatenate([top[:, :, :, 1:, :], top[:, :, :, 14:15, :]], axis=3)
    m["rpbT"] = f(np.concatenate([top, bot], axis=2).reshape(DEPTH, H, 128, 15 * 64))
    cs = np.clip(np.arange(64) - 8, 0, 48)
    valid = (ck >= cs[None, :]) & (ck < cs[None, :] + 16)
    mk = np.where(valid, 0.0, -30000.0).astype(np.float32)
    mk = np.broadcast_to(np.concatenate([mk, mk], 0)[:, None, :], (128, 15, 64))
    m["maskT"] = f(mk.reshape(128, 15 * 64))
    cwv = np.asarray(inputs["conv_w"])
    m["conv_w"] = f(cwv.transpose(0, 2, 1).reshape(DEPTH, NG, 128, 3))
    m["sg_norm"] = f(inputs["sg_norm"])
    sgw = np.asarray(inputs["sg_w"])
    m["sg_wT"] = f(sgw.transpose(0, 3, 1, 2).reshape(DEPTH, 128, NG * 128))
    m["sg_b"] = f(np.asarray(inputs["sg_b"]).reshape(DEPTH, NG * 128))
    m["w_out"] = f(inputs["w_out"]); m["w_router"] = f(inputs["w_router"])
    m["w_gate"] = f(inputs["w_gate"]); m["w_up"] = f(inputs["w_up"]); m["w_down"] = f(inputs["w_down"])
    return m


_CACHE = {}


def kernel(**inputs):
    x = np.asarray(inputs["x"])
    B, SEQ, D = x.shape
    cfg = make_cfg(D=D, SEQ=SEQ, CTX=np.asarray(inputs["ctx"]).shape[1], E=np.asarray(inputs["w_router"]).shape[-1],
                   FF=np.asarray(inputs["w_gate"]).shape[-1], DEPTH=np.asarray(inputs["w_ada"]).shape[0])
    key = tuple(sorted((k, v) for k, v in cfg.items() if isinstance(v, int)))
    if key not in _CACHE:
        _CACHE[key] = build(cfg)
    nc = _CACHE[key]
    in_maps = [host_prep(cfg, inputs, b) for b in range(B)]
    res = run_bass_kernel_spmd(nc, in_maps, core_ids=list(range(B)))
    return np.stack([res.results[b]["out"] for b in range(B)], 0).astype(np.float32)
```
